# Optimizing a Trainium2 kernel written in Bass

```python
import math
import numpy as np
import jax
import jax.numpy as jnp
from jax import lax

D_MODEL = 1024
BATCH = 1
SEQ = 16384
DEPTH = 4

EPS = 1e-6
N_BRANCH = 3
BRANCH_WIDTH = 512
SSM_HEADS = 8
SSM_HEAD_DIM = 64
SSM_INNER = 512
SSM_GROUPS = 2
SSM_STATE = 128
SSM_CONV = 4
SSD_CHUNK = 128
SSM_XBC = SSM_INNER + 2 * SSM_GROUPS * SSM_STATE
SB_HEADS = 4
SB_HEAD_DIM = 128
SB_INNER = 512
SB_BLOCK = 128
GDN_HEADS = 4
GDN_HEAD_DIM = 128
GDN_INNER = 512
GDN_CONV = 4
GDN_CHUNK = 64
MOE_GROUPS = 4
EXPERTS_PER_GROUP = 8
N_EXPERTS = 32
MOE_TOP_K = 2
EXPERT_FF = 512
MOE_BLOCK = 128
IN_SPLITS = (SSM_INNER, SSM_XBC, SSM_HEADS, 3 * SB_INNER, 3 * GDN_INNER, GDN_HEADS, GDN_HEADS, GDN_INNER, N_BRANCH * D_MODEL)
IN_COLS = SSM_INNER + SSM_XBC + SSM_HEADS + 3 * SB_INNER + 3 * GDN_INNER + 2 * GDN_HEADS + GDN_INNER + N_BRANCH * D_MODEL

kernel_name = "hybrid_ssd_stickbreak_gdn_hmoe"


def rms_norm(x, g):
    xf = x.astype(jnp.float32)
    y = xf * lax.rsqrt(jnp.mean(xf * xf, axis=-1, keepdims=True) + EPS)
    return (y * g.astype(jnp.float32)).astype(x.dtype)


def l2_norm(x):
    xf = x.astype(jnp.float32)
    return xf * lax.rsqrt(jnp.sum(xf * xf, axis=-1, keepdims=True) + EPS)


def causal_conv(x, w, b=None):
    k, ch = w.shape
    y = lax.conv_general_dilated(x, w[:, None, :].astype(x.dtype), window_strides=(1,), padding=[(k - 1, 0)],
                                 dimension_numbers=("NWC", "WIO", "NWC"), feature_group_count=ch)
    if b is not None:
        y = y + b.astype(x.dtype)
    return y


def ssd_chunked(xs, dt, a, bm, cm):
    bsz, s, h, p = xs.shape
    n = bm.shape[-1]
    nc = s // SSD_CHUNK
    xdt = (xs * dt[..., None]).reshape(bsz, nc, SSD_CHUNK, h, p)
    adt = (dt * a).reshape(bsz, nc, SSD_CHUNK, h).transpose(0, 3, 1, 2)
    bm = bm.reshape(bsz, nc, SSD_CHUNK, h, n)
    cm = cm.reshape(bsz, nc, SSD_CHUNK, h, n)
    a_cum = jnp.cumsum(adt, axis=-1)
    tril = jnp.tril(jnp.ones((SSD_CHUNK, SSD_CHUNK), bool))
    seg = jnp.exp(jnp.where(tril, a_cum[..., :, None] - a_cum[..., None, :], -jnp.inf))
    scores = jnp.einsum("bclhn,bcshn->bhcls", cm, bm) * seg
    y_diag = jnp.einsum("bhcls,bcshp->bclhp", scores, xdt)
    decay_states = jnp.exp(a_cum[..., -1:] - a_cum).transpose(0, 2, 3, 1)
    states = jnp.einsum("bclhn,bclhp->bchpn", bm, xdt * decay_states[..., None])
    chunk_decay = jnp.exp(a_cum[..., -1])

    def step(hstate, inp):
        st, dec = inp
        return hstate * dec[..., None, None] + st, hstate

    h0 = jnp.zeros((bsz, h, p, n), xs.dtype)
    _, states_in = lax.scan(step, h0, (states.transpose(1, 0, 2, 3, 4), chunk_decay.transpose(2, 0, 1)))
    states_in = states_in.transpose(1, 0, 2, 3, 4)
    c_dec = cm * jnp.exp(a_cum).transpose(0, 2, 3, 1)[..., None]
    y_off = jnp.einsum("bclhn,bchpn->bclhp", c_dec, states_in)
    return (y_diag + y_off).reshape(bsz, s, h, p)


def mamba2_branch(z, xbc, dt_raw, conv_w, conv_b, dt_bias, a_log, d_skip, norm_g):
    bsz, s, _ = z.shape
    xbc = jax.nn.silu(causal_conv(xbc, conv_w, conv_b)).astype(jnp.float32)
    xs, bm, cm = jnp.split(xbc, [SSM_INNER, SSM_INNER + SSM_GROUPS * SSM_STATE], axis=-1)
    xs = xs.reshape(bsz, s, SSM_HEADS, SSM_HEAD_DIM)
    rep = SSM_HEADS // SSM_GROUPS
    bm = jnp.repeat(bm.reshape(bsz, s, SSM_GROUPS, SSM_STATE), rep, axis=2)
    cm = jnp.repeat(cm.reshape(bsz, s, SSM_GROUPS, SSM_STATE), rep, axis=2)
    dt = jax.nn.softplus(dt_raw.astype(jnp.float32) + dt_bias.astype(jnp.float32))
    a = -jnp.exp(a_log.astype(jnp.float32))
    y = ssd_chunked(xs, dt, a, bm, cm) + xs * d_skip.astype(jnp.float32)[:, None]
    gsz = SSM_INNER // SSM_GROUPS
    y = y.reshape(bsz, s, SSM_GROUPS, gsz) * jax.nn.silu(z.astype(jnp.float32)).reshape(bsz, s, SSM_GROUPS, gsz)
    y = rms_norm(y, norm_g.reshape(SSM_GROUPS, gsz))
    return y.reshape(bsz, s, SSM_INNER)


def stick_breaking_attention(q, k, v):
    bsz, s, h, d = q.shape
    nblk = s // SB_BLOCK
    blk = SB_BLOCK

    def to_blocks(t):
        return t.astype(jnp.float32).reshape(bsz, nblk, blk, h, d).transpose(0, 3, 1, 2, 4)

    qb = to_blocks(q) * (d ** -0.5)
    kb = to_blocks(k)
    vb = to_blocks(v)
    idx = jnp.arange(blk)
    suffix = (idx[:, None] > idx[None, :]).astype(jnp.float32)
    diag_mask = idx[None, :] < idx[:, None]
    acc = jnp.zeros((bsz, h, nblk, blk), jnp.float32)
    out = jnp.zeros((bsz, h, nblk, blk, d), jnp.float32)
    for o in range(nblk):
        n = nblk - o
        z = jnp.einsum("bhnqd,bhnkd->bhnqk", qb[:, :, o:], kb[:, :, :n])
        log_beta = jax.nn.log_sigmoid(z)
        log_keep = log_beta - z
        if o == 0:
            log_keep = jnp.where(diag_mask, log_keep, 0.0)
        after = jnp.einsum("bhnqj,js->bhnqs", log_keep, suffix) + acc[:, :, o:, :, None]
        att = jnp.exp(log_beta + after)
        if o == 0:
            att = jnp.where(diag_mask, att, 0.0)
        out = out.at[:, :, o:].add(jnp.einsum("bhnqk,bhnkd->bhnqd", att, vb[:, :, :n]))
        acc = acc.at[:, :, o:].add(jnp.sum(log_keep, axis=-1))
    return out.transpose(0, 2, 3, 1, 4).reshape(bsz, s, h, d)


def stick_breaking_branch(qkv, q_norm_g, k_norm_g):
    bsz, s, _ = qkv.shape
    q, k, v = [t.reshape(bsz, s, SB_HEADS, SB_HEAD_DIM) for t in jnp.split(qkv, 3, axis=-1)]
    q = rms_norm(q, q_norm_g)
    k = rms_norm(k, k_norm_g)
    return stick_breaking_attention(q, k, v).reshape(bsz, s, SB_INNER)


def gated_delta_chunked(q, k, v, g, beta):
    bsz, s, h, dk = q.shape
    dv = v.shape[-1]
    nc = s // GDN_CHUNK
    cs = GDN_CHUNK

    def to_chunks(t):
        return t.reshape(bsz, nc, cs, h, t.shape[-1]).transpose(0, 3, 1, 2, 4)

    q = to_chunks(q * dk ** -0.5)
    k = to_chunks(k)
    v = to_chunks(v)
    g = g.reshape(bsz, nc, cs, h).transpose(0, 3, 1, 2)
    beta = beta.reshape(bsz, nc, cs, h).transpose(0, 3, 1, 2)
    g_cum = jnp.cumsum(g, axis=-1)
    tril_incl = jnp.tril(jnp.ones((cs, cs), bool))
    strict = jnp.tril(jnp.ones((cs, cs), bool), k=-1)
    decay = jnp.exp(jnp.where(tril_incl, g_cum[..., :, None] - g_cum[..., None, :], -jnp.inf))
    k_beta = k * beta[..., None]
    v_beta = v * beta[..., None]
    m = jnp.where(strict, jnp.einsum("bhncd,bhnsd->bhncs", k_beta, k) * decay, 0.0)
    t_mat = m + jnp.eye(cs, dtype=m.dtype)
    rhs = jnp.concatenate([v_beta, k_beta * jnp.exp(g_cum)[..., None]], axis=-1)
    sol = lax.linalg.triangular_solve(t_mat, rhs, left_side=True, lower=True, unit_diagonal=True)
    u, w = sol[..., :dv], sol[..., dv:]
    attn = jnp.where(tril_incl, jnp.einsum("bhncd,bhnsd->bhncs", q, k) * decay, 0.0)
    q_dec = q * jnp.exp(g_cum)[..., None]
    k_dec = k * jnp.exp(g_cum[..., -1:] - g_cum)[..., None]
    chunk_dec = jnp.exp(g_cum[..., -1])

    def step(state, inp):
        qd, kd, u_c, w_c, a_c, dec = inp
        v_new = u_c - jnp.einsum("bhcd,bhde->bhce", w_c, state)
        o = jnp.einsum("bhcd,bhde->bhce", qd, state) + jnp.einsum("bhcs,bhse->bhce", a_c, v_new)
        state = state * dec[..., None, None] + jnp.einsum("bhcd,bhce->bhde", kd, v_new)
        return state, o

    front = lambda t: t.transpose(2, 0, 1, 3, 4)
    s0 = jnp.zeros((bsz, h, dk, dv), q.dtype)
    _, o = lax.scan(step, s0, (front(q_dec), front(k_dec), front(u), front(w), front(attn), chunk_dec.transpose(2, 0, 1)))
    return o.transpose(1, 0, 3, 2, 4).reshape(bsz, s, h, dv)


def gated_deltanet_branch(qkv, a_raw, b_raw, gate, conv_w, a_log, dt_bias, norm_g):
    bsz, s, _ = qkv.shape
    qkv = jax.nn.silu(causal_conv(qkv, conv_w)).astype(jnp.float32)
    q, k, v = [t.reshape(bsz, s, GDN_HEADS, GDN_HEAD_DIM) for t in jnp.split(qkv, 3, axis=-1)]
    q = l2_norm(q)
    k = l2_norm(k)
    beta = jax.nn.sigmoid(b_raw.astype(jnp.float32))
    g = -jnp.exp(a_log.astype(jnp.float32)) * jax.nn.softplus(a_raw.astype(jnp.float32) + dt_bias.astype(jnp.float32))
    o = gated_delta_chunked(q, k, v, g, beta)
    o = rms_norm(o, norm_g) * jax.nn.silu(gate.astype(jnp.float32).reshape(bsz, s, GDN_HEADS, GDN_HEAD_DIM))
    return o.reshape(bsz, s, GDN_INNER)


def hier_moe(h, w_group, b_group, w_router, b_router, w_gate, w_up, w_down):
    n, d = h.shape
    hf = h.astype(jnp.float32)
    group_logits = hf @ w_group.astype(jnp.float32) + b_group.astype(jnp.float32)
    group_prob = jax.nn.softmax(group_logits, axis=-1)
    g_sel = jnp.argmax(group_logits, axis=-1)
    p_group = jnp.take_along_axis(group_prob, g_sel[:, None], axis=1)[:, 0]
    exp_logits = (hf @ w_router.astype(jnp.float32) + b_router.astype(jnp.float32)).reshape(n, MOE_GROUPS, EXPERTS_PER_GROUP)
    in_group = jnp.take_along_axis(exp_logits, g_sel[:, None, None], axis=1)[:, 0]
    top_p, top_i = lax.top_k(jax.nn.softmax(in_group, axis=-1), MOE_TOP_K)
    weights = p_group[:, None] * top_p / jnp.sum(top_p, axis=-1, keepdims=True)
    expert_ids = (g_sel[:, None] * EXPERTS_PER_GROUP + top_i).astype(jnp.int32)
    m = n * MOE_TOP_K
    flat_e = expert_ids.reshape(m)
    flat_tok = jnp.repeat(jnp.arange(n, dtype=jnp.int32), MOE_TOP_K)
    flat_w = weights.reshape(m)
    order = jnp.argsort(flat_e)
    sorted_e = flat_e[order]
    counts = jnp.zeros((N_EXPERTS,), jnp.int32).at[flat_e].add(1)
    padded = (counts + MOE_BLOCK - 1) // MOE_BLOCK * MOE_BLOCK
    start = jnp.cumsum(counts) - counts
    pend = jnp.cumsum(padded)
    pstart = pend - padded
    dest = pstart[sorted_e] + (jnp.arange(m, dtype=jnp.int32) - start[sorted_e])
    n_blocks = -(-m // MOE_BLOCK) + N_EXPERTS
    slot_tok = jnp.full((n_blocks * MOE_BLOCK,), n, jnp.int32).at[dest].set(flat_tok[order])
    slot_w = jnp.zeros((n_blocks * MOE_BLOCK,), jnp.float32).at[dest].set(flat_w[order])
    block_expert = jnp.minimum(jnp.searchsorted(pend, jnp.arange(n_blocks, dtype=jnp.int32) * MOE_BLOCK, side="right"), N_EXPERTS - 1)
    h_pad = jnp.concatenate([h, jnp.zeros((1, d), h.dtype)], axis=0)
    xb = h_pad[slot_tok].reshape(n_blocks, MOE_BLOCK, d)

    def expert_block(args):
        xi, e = args
        return (jax.nn.silu(xi @ w_gate[e]) * (xi @ w_up[e])) @ w_down[e]

    yb = lax.map(expert_block, (xb, block_expert)).reshape(n_blocks * MOE_BLOCK, d)
    y = jax.ops.segment_sum(yb * slot_w[:, None].astype(yb.dtype), slot_tok, num_segments=n + 1)
    return y[:n]


def setup_inputs(seed: int = 0) -> dict:
    key = jax.random.key(seed)
    ks = jax.random.split(key, 32)
    f32 = jnp.float32
    L, D = DEPTH, D_MODEL

    def nrm(k, shape, scale):
        return jax.random.normal(k, shape, f32) * scale

    def gain(k, shape):
        return 1.0 + 0.02 * jax.random.normal(k, shape, f32)

    def dt_bias_init(k, shape):
        dt = jnp.exp(jax.random.uniform(k, shape, f32, math.log(1e-3), math.log(1e-1)))
        return dt + jnp.log(-jnp.expm1(-dt))

    return {
        "x": nrm(ks[0], (BATCH, SEQ, D), 1.0),
        "c": nrm(ks[1], (BATCH, D), 1.0),
        "w_ada": nrm(ks[2], (L, D, 6 * D), 0.5 * D ** -0.5),
        "b_ada": nrm(ks[3], (L, 6 * D), 0.02),
        "norm_mix": gain(ks[4], (L, D)),
        "norm_ffn": gain(ks[5], (L, D)),
        "w_in": nrm(ks[6], (L, D, IN_COLS), D ** -0.5),
        "ssm_conv_w": nrm(ks[7], (L, SSM_CONV, SSM_XBC), SSM_CONV ** -0.5),
        "ssm_conv_b": nrm(ks[8], (L, SSM_XBC), 0.01),
        "ssm_dt_bias": dt_bias_init(ks[9], (L, SSM_HEADS)),
        "ssm_a_log": jnp.log(jax.random.uniform(ks[10], (L, SSM_HEADS), f32, 1.0, 16.0)),
        "ssm_d": gain(ks[11], (L, SSM_HEADS)),
        "ssm_norm": gain(ks[12], (L, SSM_INNER)),
        "sb_q_norm": gain(ks[13], (L, SB_HEAD_DIM)),
        "sb_k_norm": gain(ks[14], (L, SB_HEAD_DIM)),
        "gdn_conv_w": nrm(ks[15], (L, GDN_CONV, 3 * GDN_INNER), GDN_CONV ** -0.5),
        "gdn_a_log": jnp.log(jax.random.uniform(ks[16], (L, GDN_HEADS), f32, 1.0, 16.0)),
        "gdn_dt_bias": dt_bias_init(ks[17], (L, GDN_HEADS)),
        "gdn_norm": gain(ks[18], (L, GDN_HEAD_DIM)),
        "w_branch": nrm(ks[19], (L, N_BRANCH, BRANCH_WIDTH, D), BRANCH_WIDTH ** -0.5),
        "w_out": nrm(ks[20], (L, D, D), D ** -0.5),
        "w_group": nrm(ks[21], (L, D, MOE_GROUPS), D ** -0.5),
        "b_group": nrm(ks[22], (L, MOE_GROUPS), 0.01),
        "w_router": nrm(ks[23], (L, D, N_EXPERTS), D ** -0.5),
        "b_router": nrm(ks[24], (L, N_EXPERTS), 0.01),
        "w_gate": nrm(ks[25], (L, N_EXPERTS, D, EXPERT_FF), D ** -0.5),
        "w_up": nrm(ks[26], (L, N_EXPERTS, D, EXPERT_FF), D ** -0.5),
        "w_down": nrm(ks[27], (L, N_EXPERTS, EXPERT_FF, D), EXPERT_FF ** -0.5),
    }


def reference(x, c, w_ada, b_ada, norm_mix, norm_ffn, w_in, ssm_conv_w, ssm_conv_b, ssm_dt_bias, ssm_a_log, ssm_d,
              ssm_norm, sb_q_norm, sb_k_norm, gdn_conv_w, gdn_a_log, gdn_dt_bias, gdn_norm, w_branch, w_out,
              w_group, b_group, w_router, b_router, w_gate, w_up, w_down):
    bsz, s, d = x.shape
    split_points = np.cumsum(IN_SPLITS)[:-1].tolist()
    c_act = jax.nn.silu(c)
    for l in range(DEPTH):
        mod = c_act @ w_ada[l] + b_ada[l]
        shift_m, scale_m, gate_m, shift_f, scale_f, gate_f = [t[:, None, :] for t in jnp.split(mod, 6, axis=-1)]
        h = rms_norm(x, norm_mix[l]) * (1.0 + scale_m) + shift_m
        proj = h @ w_in[l]
        m_z, m_xbc, m_dt, sb_qkv, g_qkv, g_a, g_b, g_gate, br_gate = jnp.split(proj, split_points, axis=-1)
        y_a = mamba2_branch(m_z, m_xbc, m_dt, ssm_conv_w[l], ssm_conv_b[l], ssm_dt_bias[l], ssm_a_log[l], ssm_d[l], ssm_norm[l])
        y_b = stick_breaking_branch(sb_qkv, sb_q_norm[l], sb_k_norm[l])
        y_c = gated_deltanet_branch(g_qkv, g_a, g_b, g_gate, gdn_conv_w[l], gdn_a_log[l], gdn_dt_bias[l], gdn_norm[l])
        branches = jnp.stack([y_a.astype(x.dtype), y_b.astype(x.dtype), y_c.astype(x.dtype)], axis=2)
        branch_proj = jnp.einsum("bsie,ied->bsid", branches, w_branch[l])
        gates = jax.nn.sigmoid(br_gate.astype(jnp.float32)).astype(x.dtype).reshape(bsz, s, N_BRANCH, d)
        merged = jnp.sum(gates * branch_proj, axis=2)
        x = x + gate_m * (merged @ w_out[l])
        h = rms_norm(x, norm_ffn[l]) * (1.0 + scale_f) + shift_f
        y = hier_moe(h.reshape(bsz * s, d), w_group[l], b_group[l], w_router[l], b_router[l], w_gate[l], w_up[l], w_down[l])
        x = x + gate_f * y.reshape(bsz, s, d)
    return x
```

```python
import numpy as np
import concourse.bass as bass
import concourse.mybir as mybir
from concourse.bass_utils import run_bass_kernel_spmd

F32 = mybir.dt.float32
BF16 = mybir.dt.bfloat16
I32 = mybir.dt.int32
U32 = mybir.dt.uint32
AF = mybir.ActivationFunctionType
ALU = mybir.AluOpType
AX = mybir.AxisListType

NDS = 12


class Buf:
    __slots__ = ("name", "w", "r")

    def __init__(self, name):
        self.name = name
        self.w = None
        self.r = {}


class V:
    __slots__ = ("ap", "bufs")

    def __init__(self, ap, bufs):
        self.ap = ap
        self.bufs = bufs


class T:
    def __init__(self, h, name, nbuf_axis=None, nbuf=1):
        self.h = h
        self.name = name
        self.buf = Buf(name)

    def __getitem__(self, idx):
        return V(self.h[idx], (self.buf,))

    def v(self, ap):
        return V(ap, (self.buf,))


class P:
    def __init__(self, nc, same_eng_sync=True):
        self.nc = nc
        self.engs = {"pe": nc.tensor, "dve": nc.vector, "act": nc.scalar, "pool": nc.gpsimd, "sp": nc.sync}
        self.sem = {k: nc.alloc_semaphore(name=f"s_{k}") for k in self.engs}
        self.cnt = {k: 0 for k in self.engs}
        self.seen = {k: {} for k in self.engs}
        self.dsem = {}
        self.dcnt = {}
        self.dnext = {}
        for q in ("sp", "pool", "act"):
            self.dsem[q] = [nc.alloc_semaphore(name=f"d_{q}{i}") for i in range(NDS)]
            self.dcnt[q] = [0] * NDS
            self.dnext[q] = 0
        self.same_eng_sync = same_eng_sync
        self.n_wait = 0
        self.n_ins = 0
        self._n = 0

    def sb(self, shape, dt=F32, name=None):
        self._n += 1
        name = name or f"sb{self._n}"
        h = self.nc.alloc_sbuf_tensor("S_" + name, list(shape), dt)
        return T(h, name)

    def ps(self, shape, dt=F32, name=None):
        self._n += 1
        name = name or f"ps{self._n}"
        h = self.nc.alloc_psum_tensor("P_" + name, list(shape), dt)
        return T(h, name)

    def dram(self, name, shape, dt=F32, kind="Internal"):
        h = self.nc.dram_tensor(name, list(shape), dt, kind=kind)
        return T(h, name)

    def rot(self, key, shape, dt, n, psum=False):
        if not hasattr(self, "_rot"):
            self._rot = {}
        if key not in self._rot:
            tiles = [(self.ps if psum else self.sb)(shape, dt, f"{key}_{i}") for i in range(n)]
            self._rot[key] = [tiles, 0]
        ent = self._rot[key]
        t = ent[0][ent[1] % len(ent[0])]
        ent[1] += 1
        return t

    def _semof(self, src):
        if src[0] == "e":
            return self.sem[src[1]]
        return self.dsem[src[1]][src[2]]

    def _wait(self, e, deps):
        best = {}
        for d in deps:
            if d is None:
                continue
            key = d[:-1]
            if key not in best or best[key] < d[-1]:
                best[key] = d[-1]
        for key, c in best.items():
            if key[0] == "e" and key[1] == e and not (self.same_eng_sync and e not in ("pe",)):
                continue
            if self.seen[e].get(key, 0) >= c:
                continue
            self.seen[e][key] = c
            self.engs[e].wait_ge(self._semof(key), c)
            self.n_wait += 1

    def _deps(self, reads, writes):
        deps = set()
        for v in reads:
            for b in v.bufs:
                if b.w is not None:
                    deps.add(b.w)
        for v in writes:
            for b in v.bufs:
                if b.w is not None:
                    deps.add(b.w)
                deps.update(b.r.values())
        return deps

    def _commit(self, tok, reads, writes):
        for v in writes:
            for b in v.bufs:
                b.w = tok
                b.r = {}
        wb = set()
        for v in writes:
            wb.update(id(b) for b in v.bufs)
        for v in reads:
            for b in v.bufs:
                if id(b) not in wb:
                    b.r[tok[:-1]] = tok

    def op(self, e, fn, reads=(), writes=()):
        self._wait(e, self._deps(reads, writes))
        ins = fn(self.engs[e])
        self.cnt[e] += 1
        ins.then_inc(self.sem[e], 1)
        self.n_ins += 1
        self._commit(("e", e, self.cnt[e]), reads, writes)
        return ins

    def dma(self, out, in_, q="sp", **kw):
        i = self.dnext[q]
        self.dnext[q] = (i + 1) % NDS
        deps = self._deps([in_], [out])
        if self.dcnt[q][i] > 0:
            deps.add(("d", q, i, 16 * self.dcnt[q][i]))
        self._wait(q, deps)
        ins = self.engs[q].dma_start(out=out.ap, in_=in_.ap, **kw)
        self.dcnt[q][i] += 1
        ins.then_inc(self.dsem[q][i], 16)
        self.n_ins += 1
        self._commit(("d", q, i, 16 * self.dcnt[q][i]), [in_], [out])
        return ins

    def finish(self, outs, e="sp"):
        deps = set()
        for t in outs:
            if t.buf.w is not None:
                deps.add(t.buf.w)
        for k in self.engs:
            if self.cnt[k] > 0:
                deps.add(("e", k, self.cnt[k]))
        for q in self.dsem:
            for i in range(NDS):
                if self.dcnt[q][i] > 0:
                    deps.add(("d", q, i, 16 * self.dcnt[q][i]))
        self._wait(e, deps)

    def mm(self, out, lhsT, rhs, start=True, stop=True):
        rd = [lhsT, rhs]
        return self.op("pe", lambda e: e.matmul(out.ap, lhsT.ap, rhs.ap, start=start, stop=stop), rd, [out])

    def tr(self, out, in_, ident):
        return self.op("pe", lambda e: e.transpose(out.ap, in_.ap, ident.ap), [in_, ident], [out])

    def act(self, out, in_, func, bias=None, scale=None, accum=None, e="act"):
        kw = {}
        rd = [in_]
        wr = [out]
        if bias is not None:
            if isinstance(bias, V):
                kw["bias"] = bias.ap
                rd.append(bias)
            else:
                kw["bias"] = bias
        if scale is not None:
            if isinstance(scale, V):
                kw["scale"] = scale.ap
                rd.append(scale)
            else:
                kw["scale"] = scale
        if accum is not None:
            kw["accum_out"] = accum.ap
            wr.append(accum)
        return self.op("act", lambda en: en.activation(out.ap, in_.ap, func, **kw), rd, wr)

    def tt(self, out, a, b, op, e="dve"):
        return self.op(e, lambda en: en.tensor_tensor(out.ap, a.ap, b.ap, op), [a, b], [out])

    def ts(self, out, a, s1, op0, s2=None, op1=None, accum=None, e="dve"):
        rd = [a]
        wr = [out]
        s1a = s1.ap if isinstance(s1, V) else s1
        s2a = s2.ap if isinstance(s2, V) else s2
        if isinstance(s1, V):
            rd.append(s1)
        if isinstance(s2, V):
            rd.append(s2)
        kw = {}
        if op1 is not None:
            kw["op1"] = op1
        if accum is not None:
            kw["accum_out"] = accum.ap
            wr.append(accum)
        return self.op(e, lambda en: en.tensor_scalar(out.ap, a.ap, s1a, s2a, op0, **kw), rd, wr)

    def stt(self, out, a, s, b, op0, op1, e="dve"):
        rd = [a, b]
        sa = s.ap if isinstance(s, V) else s
        if isinstance(s, V):
            rd.append(s)
        return self.op(e, lambda en: en.scalar_tensor_tensor(out.ap, a.ap, sa, b.ap, op0, op1), rd, [out])

    def copy(self, out, in_, e="dve"):
        if e == "act":
            return self.op("act", lambda en: en.copy(out.ap, in_.ap), [in_], [out])
        return self.op(e, lambda en: en.tensor_copy(out.ap, in_.ap), [in_], [out])

    def memset(self, out, val, e="dve"):
        return self.op(e, lambda en: en.memset(out.ap, val), [], [out])

    def reduce(self, out, in_, op=ALU.add, axis=AX.X, e="dve"):
        return self.op(e, lambda en: en.tensor_reduce(out.ap, in_.ap, axis, op), [in_], [out])

    def recip(self, out, in_):
        return self.op("dve", lambda en: en.reciprocal(out.ap, in_.ap), [in_], [out])


D = 1024
SEQ = 16384
NCORE = 8
TOK = SEQ // NCORE
NT = TOK // 128
IN_COLS = 8208
EPS = 1e-6
DEBUG = False
SAME_ENG = False


def bcast_row(t, n, parts=128):
    return t.v(t.h[0:1, 0:n].to_broadcast([parts, n]))


def emit_mod(p, c_bc, wada, bada, col0, ncols, out_tile, ones1, plus_one=False):
    nblk = (ncols + 511) // 512
    for b in range(nblk):
        n = min(512, ncols - b * 512)
        ws = p.rot("modw", [128, 8, 512], F32, 1)
        p.dma(ws[:, :, 0:n], wada.v(wada.h[:, col0 + b * 512: col0 + b * 512 + n].rearrange("(k p) n -> p k n", p=128)))
        bs = p.rot("modb", [1, 512], F32, 2)
        p.dma(bs[0:1, 0:n], bada[0:1, col0 + b * 512: col0 + b * 512 + n])
        ps = p.rot("ps_mm", [128, 512], F32, 4, psum=True)
        for k in range(8):
            p.mm(ps[:, 0:n], c_bc[:, k, :], ws[:, k, 0:n], start=(k == 0), stop=False)
        p.mm(ps[:, 0:n], ones1[0:1, :], bs[0:1, 0:n], start=False, stop=True)
        if plus_one:
            p.ts(out_tile[:, b * 512: b * 512 + n], ps[:, 0:n], 1.0, ALU.add)
        else:
            p.copy(out_tile[:, b * 512: b * 512 + n], ps[:, 0:n])


def emit_cbc(p, c_in, ones_f):
    c_col = p.sb([128, 8], F32, "c_col")
    p.dma(c_col[:], c_in[:])
    c_act = p.sb([128, 8], F32, "c_act")
    p.act(c_act[:], c_col[:], AF.Silu)
    c_bc = p.sb([128, 8, 128], F32, "c_bc")
    for k in range(8):
        p.ts(c_bc[:, k, :], ones_f[:], c_act[:, k:k + 1], ALU.mult)
    return c_bc


def emit_rmsnorm_mod(p, x_t, gmod, shift, h_out, width=D):
    junk = p.rot("rn_junk", [128, width], F32, 1)
    ssq = p.rot("rn_ssq", [128, 1], F32, 2)
    p.act(junk[:], x_t, AF.Square, accum=ssq[:])
    rstd = p.rot("rn_rstd", [128, 1], F32, 2)
    p.act(rstd[:], ssq[:], AF.Sqrt, bias=EPS, scale=1.0 / width)
    p.recip(rstd[:], rstd[:])
    tmp = p.rot("rn_tmp", [128, width], F32, 1)
    p.stt(tmp[:], x_t, rstd[:], gmod, ALU.mult, ALU.mult)
    p.tt(h_out, tmp[:], shift, ALU.add)


def build_k1(ntiles=NT, ncols=IN_COLS):
    nc = bass.Bass("TRN2", target_bir_lowering=False)
    p = P(nc)
    ntok = ntiles * 128
    x = p.dram("x", [ntok, D], F32, kind="ExternalInput")
    c_in = p.dram("c_col", [128, 8], F32, kind="ExternalInput")
    wada = p.dram("wada", [D, 2 * D], F32, kind="ExternalInput")
    bada = p.dram("bada", [1, 2 * D], F32, kind="ExternalInput")
    ng = p.dram("norm_g", [1, D], F32, kind="ExternalInput")
    w_in = p.dram("w_in", [D, ncols], F32, kind="ExternalInput")
    ident_d = p.dram("ident_in", [128, 128], F32, kind="ExternalInput")
    out = p.dram("proj", [ntok, ncols], F32, kind="ExternalOutput")

    ident = p.sb([128, 128], F32, "ident")
    p.dma(ident[:], ident_d[:])
    identb = p.sb([128, 128], BF16, "identb")
    p.copy(identb[:], ident[:])
    ones_f = p.sb([128, 128], F32, "ones_f")
    p.memset(ones_f[:], 1.0)
    c_bc = emit_cbc(p, c_in, ones_f)
    shift = p.sb([128, D], F32, "shift")
    gmod = p.sb([128, D], F32, "gmod")
    emit_mod(p, c_bc, wada, bada, 0, D, shift, ones_f)
    emit_mod(p, c_bc, wada, bada, D, D, gmod, ones_f, plus_one=True)
    ngb = p.sb([128, D], F32, "ngb")
    p.dma(ngb[:], bcast_row(ng, D))
    p.tt(gmod[:], gmod[:], ngb[:], ALU.mult)

    HALF = (ncols + 1) // 2
    Wb = p.sb([128, 8, HALF], BF16, "Wb")
    PIECE = 2052
    cv = 0
    for half in range(2):
        h0 = half * HALF
        hn = min(HALF, ncols - h0)
        npiece = (hn + PIECE - 1) // PIECE
        for k in range(8):
            for pc in range(npiece):
                c0 = pc * PIECE
                n = min(PIECE, hn - c0)
                st = p.rot("wstage", [128, PIECE], F32, 2)
                p.dma(st[:, 0:n], w_in[k * 128:(k + 1) * 128, h0 + c0:h0 + c0 + n], q=("sp" if cv % 2 == 0 else "pool"))
                eng = ("dve", "act", "pool")[cv % 3]
                p.copy(Wb[:, k, c0:c0 + n], st[:, 0:n], e=eng)
                cv += 1
        for t in range(ntiles):
            xt = p.rot("xt", [128, D], F32, 2)
            p.dma(xt[:], x[t * 128:(t + 1) * 128, :])
            hb = p.rot("hb", [128, D], BF16, 2)
            emit_rmsnorm_mod(p, xt[:], gmod[:], shift[:], hb[:])
            psT = p.rot("psT", [128, 8, 128], BF16, 1, psum=True)
            for k in range(8):
                p.tr(psT[:, k, :], hb[:, k * 128:(k + 1) * 128], identb[:])
            hT = p.rot("hT", [128, 8, 128], BF16, 2)
            p.copy(hT[:], psT[:], e="act")
            ot = p.rot("ot", [128, HALF], F32, 2)
            nb = (hn + 511) // 512
            for b in range(nb):
                n = min(512, hn - b * 512)
                ps = p.rot("ps_mm", [128, 512], F32, 4, psum=True)
                for k in range(8):
                    p.mm(ps[:, 0:n], hT[:, k, :], Wb[:, k, b * 512: b * 512 + n], start=(k == 0), stop=(k == 7))
                p.copy(ot[:, b * 512:b * 512 + n], ps[:, 0:n], e=("dve" if b % 2 == 0 else "act"))
            p.dma(out[t * 128:(t + 1) * 128, h0:h0 + hn], ot[:, 0:hn], q=("sp" if t % 2 == 0 else "pool"))
    if DEBUG:
        dbg = p.dram("dbg", [128, 4 * D], F32, kind="ExternalOutput")
        p.dma(dbg[:, 0:D], gmod[:])
        p.dma(dbg[:, D:2 * D], shift[:])
        hf = p.sb([128, D], F32, "hf")
        p.copy(hf[:], hb[:])
        p.dma(dbg[:, 2 * D:3 * D], hf[:])
        hf2 = p.sb([128, D], F32, "hf2")
        p.copy(hf2[:], hT[:])
        p.dma(dbg[:, 3 * D:4 * D], hf2[:])
        p.finish([out, dbg])
        return nc, p
    p.finish([out])
    return nc, p


def emit_qk_norm(p, src, nblk, gain_bc, identb, dstT, scale, neg_dstT=None):
    for b0 in range(0, nblk, 4):
        t = p.rot("qk_in", [128, 4, 128], F32, 2)
        p.dma(t[:], src.v(src.h[b0 * 128:(b0 + 4) * 128, :].rearrange("(b p) d -> p b d", p=128)), q=("sp" if (b0 // 4) % 2 == 0 else "pool"))
        sq = p.rot("qk_sq", [128, 4, 128], F32, 1)
        p.act(sq[:], t[:], AF.Square)
        ssq = p.rot("qk_ssq", [128, 4], F32, 2)
        p.reduce(ssq[:], sq[:])
        rstd = p.rot("qk_rstd", [128, 4], F32, 2)
        p.act(rstd[:], ssq[:], AF.Sqrt, bias=EPS, scale=1.0 / 128)
        p.recip(rstd[:], rstd[:])
        nb = p.rot("qk_nb", [128, 4, 128], BF16, 2)
        for i in range(4):
            p.stt(nb[:, i, :], t[:, i, :], rstd[:, i:i + 1], gain_bc[:], ALU.mult, ALU.mult)
        pt = p.rot("qk_pt", [128, 512], BF16, 2, psum=True)
        for i in range(4):
            p.tr(pt[:, i * 128:(i + 1) * 128], nb[:, i, :], identb[:])
        p.ts(dstT[:, b0 * 128:(b0 + 4) * 128], pt[:], scale, ALU.mult, e="pool" if False else "dve")
        if neg_dstT is not None:
            p.ts(neg_dstT[:, b0 * 128:(b0 + 4) * 128], pt[:], -scale, ALU.mult)


def build_sb(nblk=128):
    nc = bass.Bass("TRN2", target_bir_lowering=False)
    p = P(nc)
    S = nblk * 128
    ngrp = nblk // 8
    q_d = p.dram("q", [ngrp * 512, 128], F32, kind="ExternalInput")
    k_d = p.dram("k", [S, 128], F32, kind="ExternalInput")
    v_d = p.dram("v", [S, 128], F32, kind="ExternalInput")
    qg_d = p.dram("qg", [1, 128], F32, kind="ExternalInput")
    kg_d = p.dram("kg", [1, 128], F32, kind="ExternalInput")
    mask_d = p.dram("mask", [128, 8, 512], F32, kind="ExternalInput")
    mincl_d = p.dram("mincl", [128, 128], F32, kind="ExternalInput")
    ident_d = p.dram("ident_in", [128, 128], F32, kind="ExternalInput")
    out = p.dram("oT", [128, ngrp * 512], F32, kind="ExternalOutput")

    ident = p.sb([128, 128], F32, "ident")
    p.dma(ident[:], ident_d[:])
    identb = p.sb([128, 128], BF16, "identb")
    p.copy(identb[:], ident[:])
    mincl_f = p.sb([128, 128], F32, "mincl_f")
    p.dma(mincl_f[:], mincl_d[:])
    mincl = p.sb([128, 128], BF16, "mincl")
    p.copy(mincl[:], mincl_f[:])
    onesb = p.sb([128, 128], BF16, "onesb")
    p.memset(onesb[:], 1.0)
    maskb = p.sb([128, 8, 512], BF16, "maskb")
    for o in range(8):
        mt = p.rot("mask_st", [128, 512], F32, 2)
        p.dma(mt[:], mask_d[:, o, :])
        p.copy(maskb[:, o, :], mt[:])
    qg = p.sb([128, 128], F32, "qg")
    p.dma(qg[:], bcast_row(qg_d, 128))
    kg = p.sb([128, 128], F32, "kg")
    p.dma(kg[:], bcast_row(kg_d, 128))

    knT = p.sb([128, S], BF16, "knT")
    nknT = p.sb([128, S], BF16, "nknT")
    qnT = p.sb([128, ngrp * 512], BF16, "qnT")
    vb = p.sb([128, nblk, 128], BF16, "vb")
    emit_qk_norm(p, k_d, nblk, kg, identb, knT, 1.0, nknT)
    emit_qk_norm(p, q_d, ngrp * 4, qg, identb, qnT, 128 ** -0.5)
    for b0 in range(0, nblk, 4):
        t = p.rot("qk_in", [128, 4, 128], F32, 2)
        p.dma(t[:], v_d.v(v_d.h[b0 * 128:(b0 + 4) * 128, :].rearrange("(b p) d -> p b d", p=128)))
        p.copy(vb[:, b0:b0 + 4, :], t[:], e="pool")

    import os
    STOP = int(os.environ.get("SB_STOP", "0"))
    if STOP == 1:
        for m in range(ngrp):
            ot = p.rot("sbOT", [128, 512], F32, 2)
            p.copy(ot[:], qnT[:, m * 512:(m + 1) * 512])
            p.dma(out[:, m * 512:(m + 1) * 512], ot[:])
        p.finish([out])
        return nc, p
    for m in range(ngrp):
        O = p.rot("sbO", [128, 512], F32, 1, psum=True)
        CS = p.rot("sbCS", [128, 512], F32, 1, psum=True)
        accb = None
        qs = qnT[:, m * 512:(m + 1) * 512]
        kbs = list(range(8 * m + 7, -1, -1))
        for it, kb in enumerate(kbs):
            first = it == 0
            last = it == len(kbs) - 1
            off = kb - 8 * m
            Z = p.rot("sbZ", [128, 512], F32, 2, psum=True)
            U = p.rot("sbU", [128, 512], F32, 2, psum=True)
            p.mm(Z[:], knT[:, kb * 128:(kb + 1) * 128], qs)
            p.mm(U[:], nknT[:, kb * 128:(kb + 1) * 128], qs, start=True, stop=False)
            e = p.rot("sbE", [128, 512], F32, 2)
            p.act(e[:], Z[:], AF.Exp)
            spb = p.rot("sbSP", [128, 512], BF16, 2)
            p.act(spb[:], e[:], AF.Ln, bias=1.0)
            if off >= 0:
                p.tt(spb[:], spb[:], maskb[:, off, :], ALU.mult)
            p.mm(U[:], mincl[:], spb[:], start=False, stop=first)
            if not first:
                p.mm(U[:], identb[:], accb[:], start=False, stop=True)
            p.mm(CS[:], onesb[:], spb[:], start=first, stop=last)
            att = p.rot("sbATT", [128, 512], BF16, 2)
            p.act(att[:], U[:], AF.Exp, scale=-1.0)
            if off >= 0:
                p.tt(att[:], att[:], maskb[:, off, :], ALU.mult)
            if not last:
                accb = p.rot("sbACC", [128, 512], BF16, 2)
                p.copy(accb[:], CS[:])
            p.mm(O[:], vb[:, kb, :], att[:], start=first, stop=last)
        ot = p.rot("sbOT", [128, 512], F32, 2)
        p.copy(ot[:], O[:])
        p.dma(out[:, m * 512:(m + 1) * 512], ot[:])
    p.finish([out])
    return nc, p


def emit_conv_silu(p, src_d, nch, S, w_d, b_d, dst, dst_dt, name, CH=2048, bias=True):
    w = p.sb([nch, 4], F32, name + "_w")
    p.dma(w[:], w_d[:])
    if bias:
        b = p.sb([nch, 1], F32, name + "_b")
        p.dma(b[:], b_d[:])
    for c0 in range(0, S, CH):
        t = p.rot("cv_in", [128, CH + 3], F32, 2)
        p.dma(t[0:nch, :], src_d[:, c0:c0 + CH + 3])
        acc = p.rot("cv_acc", [128, CH], F32, 2)
        p.ts(acc[0:nch, :], t[0:nch, 0:CH], w[:, 0:1], ALU.mult)
        for i in range(1, 4):
            p.stt(acc[0:nch, :], t[0:nch, i:i + CH], w[:, i:i + 1], acc[0:nch, :], ALU.mult, ALU.add)
        if bias:
            p.act(dst[0:nch, c0:c0 + CH], acc[0:nch, :], AF.Silu, bias=b[:, 0:1])
        else:
            p.act(dst[0:nch, c0:c0 + CH], acc[0:nch, :], AF.Silu)


def build_ssd(nchunk=128):
    nc = bass.Bass("TRN2", target_bir_lowering=False)
    p = P(nc)
    S = nchunk * 128
    CH = min(2048, S)
    xT_d = p.dram("xT", [64, S + 3], F32, kind="ExternalInput")
    BT_d = p.dram("BT", [128, S + 3], F32, kind="ExternalInput")
    CT_d = p.dram("CT", [128, S + 3], F32, kind="ExternalInput")
    wx_d = p.dram("wx", [64, 4], F32, kind="ExternalInput")
    bx_d = p.dram("bx", [64, 1], F32, kind="ExternalInput")
    wB_d = p.dram("wB", [128, 4], F32, kind="ExternalInput")
    bB_d = p.dram("bB", [128, 1], F32, kind="ExternalInput")
    wC_d = p.dram("wC", [128, 4], F32, kind="ExternalInput")
    bC_d = p.dram("bC", [128, 1], F32, kind="ExternalInput")
    dt_d = p.dram("dt_col", [128, nchunk], F32, kind="ExternalInput")
    sc_d = p.dram("scal", [128, 3], F32, kind="ExternalInput")
    triu_d = p.dram("triu", [128, 128], F32, kind="ExternalInput")
    mneg_d = p.dram("mneg", [128, 128], F32, kind="ExternalInput")
    ident_d = p.dram("ident_in", [128, 128], F32, kind="ExternalInput")
    out = p.dram("y", [S, 64], F32, kind="ExternalOutput")

    ident = p.sb([128, 128], F32, "ident")
    p.dma(ident[:], ident_d[:])
    identb = p.sb([128, 128], BF16, "identb")
    p.copy(identb[:], ident[:])
    triu = p.sb([128, 128], F32, "triu")
    p.dma(triu[:], triu_d[:])
    mneg = p.sb([128, 128], F32, "mneg")
    p.dma(mneg[:], mneg_d[:])
    ones_f = p.sb([128, 128], F32, "ones_f")
    p.memset(ones_f[:], 1.0)
    scal = p.sb([128, 3], F32, "scal")
    p.dma(scal[:], sc_d[:])
    dtc = p.sb([128, nchunk], F32, "dtc")
    p.dma(dtc[:], dt_d[:])
    p.act(dtc[:], dtc[:], AF.Exp, bias=scal[:, 0:1])
    p.act(dtc[:], dtc[:], AF.Ln, bias=1.0)
    aneg = p.sb([128, 1], F32, "aneg")
    p.act(aneg[:], scal[:, 1:2], AF.Exp)
    p.ts(aneg[:], aneg[:], -1.0, ALU.mult)
    adt = p.sb([128, nchunk], F32, "adt")
    p.ts(adt[:], dtc[:], aneg[:, 0:1], ALU.mult)

    xT = p.sb([64, S], F32, "xTs")
    BT = p.sb([128, S], BF16, "BTs")
    CT = p.sb([128, S], BF16, "CTs")
    emit_conv_silu(p, xT_d, 64, S, wx_d, bx_d, xT, F32, "cx", CH)
    emit_conv_silu(p, BT_d, 128, S, wB_d, bB_d, BT, BF16, "cB", CH)
    emit_conv_silu(p, CT_d, 128, S, wC_d, bC_d, CT, BF16, "cC", CH)

    state_f = p.sb([128, 64], F32, "state_f")
    state_b = p.sb([128, 64], BF16, "state_b")
    p.memset(state_f[:], 0.0)
    p.memset(state_b[:], 0.0)
    OB = 8
    for c in range(nchunk):
        sl = slice(c * 128, (c + 1) * 128)
        px = p.rot("ssd_px", [128, 64], F32, 1, psum=True)
        p.tr(px[:], xT[0:64, sl], ident[0:64, 0:64])
        xtok = p.rot("ssd_xtok", [128, 64], F32, 2)
        p.copy(xtok[:], px[:])
        pB = p.rot("ssd_pB", [128, 128], BF16, 1, psum=True)
        p.tr(pB[:], BT[:, sl], identb[:])
        Btok = p.rot("ssd_Btok", [128, 128], BF16, 2)
        p.copy(Btok[:], pB[:], e="act")
        adt_c = adt[:, c:c + 1]
        pcol = p.rot("ssd_pcol", [128, 1], F32, 1, psum=True)
        p.mm(pcol[:], triu[:], adt_c)
        acol = p.rot("ssd_acol", [128, 1], F32, 2)
        p.copy(acol[:], pcol[:])
        adt_bc = p.rot("ssd_adtbc", [128, 128], F32, 2)
        p.ts(adt_bc[:], ones_f[:], adt_c, ALU.mult)
        prow = p.rot("ssd_prow", [128, 128], F32, 1, psum=True)
        p.mm(prow[:], adt_bc[:], triu[:])
        dsg = p.rot("ssd_dsg", [128, 128], F32, 2)
        p.stt(dsg[:], prow[:], acol[:, 0:1], mneg[:], ALU.subtract, ALU.add)
        segT = p.rot("ssd_segT", [128, 128], F32, 2)
        p.act(segT[:], dsg[:], AF.Exp)
        ea = p.rot("ssd_ea", [128, 128], F32, 2)
        p.act(ea[:], prow[:], AF.Exp)
        alast = p.rot("ssd_alast", [128, 1], F32, 2)
        p.copy(alast[:], prow[:, 127:128])
        dte = p.rot("ssd_dte", [128, 1], F32, 2)
        p.act(dte[:], acol[:], AF.Exp, bias=alast[:, 0:1], scale=-1.0)
        cdec = p.rot("ssd_cdec", [128, 1], F32, 2)
        p.act(cdec[:], alast[:], AF.Exp)
        pcb = p.rot("ssd_pcb", [128, 128], F32, 1, psum=True)
        p.mm(pcb[:], BT[:, sl], CT[:, sl])
        scT = p.rot("ssd_scT", [128, 128], BF16, 2)
        p.tt(scT[:], pcb[:], segT[:], ALU.mult)
        xdt = p.rot("ssd_xdt", [128, 64], BF16, 2)
        p.ts(xdt[:], xtok[:], dtc[:, c:c + 1], ALU.mult)
        cdT = p.rot("ssd_cdT", [128, 128], BF16, 2)
        p.tt(cdT[:], CT[:, sl], ea[:], ALU.mult)
        py = p.rot("ssd_py", [128, 64], F32, 2, psum=True)
        p.mm(py[:], scT[:], xdt[:], start=True, stop=False)
        p.mm(py[:], cdT[:], state_b[:], start=False, stop=True)
        if c % OB == 0:
            yo = p.rot("ssd_yo", [128, OB, 64], F32, 2)
        p.stt(yo[:, c % OB, :], xtok[:], scal[:, 2:3], py[:], ALU.mult, ALU.add)
        if c % OB == OB - 1:
            c0 = (c - OB + 1) * 128
            p.dma(out.v(out.h[c0:c0 + OB * 128, :].rearrange("(b p) d -> p b d", p=128)), yo[:])
        sc2 = p.rot("ssd_sc2", [128, 1], F32, 2)
        p.tt(sc2[:], dtc[:, c:c + 1], dte[:], ALU.mult)
        xdd = p.rot("ssd_xdd", [128, 64], BF16, 2)
        p.ts(xdd[:], xtok[:], sc2[:, 0:1], ALU.mult)
        pS = p.rot("ssd_pS", [128, 64], F32, 1, psum=True)
        p.mm(pS[:], Btok[:], xdd[:])
        p.stt(state_f[:], state_f[:], cdec[:, 0:1], pS[:], ALU.mult, ALU.add)
        p.copy(state_b[:], state_f[:])
    p.finish([out])
    return nc, p


def build_gdn(nchunk=256):
    nc = bass.Bass("TRN2", target_bir_lowering=False)
    p = P(nc)
    S = nchunk * 64
    CH = min(2048, S)
    CPS = CH // 64
    qT_d = p.dram("qT", [128, S + 3], F32, kind="ExternalInput")
    kT_d = p.dram("kT", [128, S + 3], F32, kind="ExternalInput")
    vT_d = p.dram("vT", [64, S + 3], F32, kind="ExternalInput")
    wq_d = p.dram("wq", [128, 4], F32, kind="ExternalInput")
    wk_d = p.dram("wk", [128, 4], F32, kind="ExternalInput")
    wv_d = p.dram("wv", [64, 4], F32, kind="ExternalInput")
    a_d = p.dram("a_col", [64, nchunk], F32, kind="ExternalInput")
    b_d = p.dram("b_col", [64, nchunk], F32, kind="ExternalInput")
    sc_d = p.dram("scal", [128, 2], F32, kind="ExternalInput")
    triu_d = p.dram("triu", [64, 64], F32, kind="ExternalInput")
    mpos_d = p.dram("mpos", [64, 64], F32, kind="ExternalInput")
    mneg_d = p.dram("mneg", [64, 64], F32, kind="ExternalInput")
    st01_d = p.dram("st01", [64, 64], F32, kind="ExternalInput")
    ident_d = p.dram("ident_in", [128, 128], F32, kind="ExternalInput")
    out = p.dram("o", [S, 64], F32, kind="ExternalOutput")

    def ld(d, shape, name):
        t = p.sb(shape, F32, name)
        p.dma(t[:], d[:])
        return t
    ident = ld(ident_d, [128, 128], "ident")
    triu = ld(triu_d, [64, 64], "triu")
    mpos = ld(mpos_d, [64, 64], "mpos")
    mneg = ld(mneg_d, [64, 64], "mneg")
    st01 = ld(st01_d, [64, 64], "st01")
    scal = ld(sc_d, [128, 2], "scal")
    ones_f = p.sb([64, 128], F32, "ones_f")
    p.memset(ones_f[:], 1.0)
    beta = ld(b_d, [64, nchunk], "beta")
    p.act(beta[:], beta[:], AF.Sigmoid)
    gall = ld(a_d, [64, nchunk], "gall")
    p.act(gall[:], gall[:], AF.Exp, bias=scal[0:64, 1:2])
    p.act(gall[:], gall[:], AF.Ln, bias=1.0)
    aneg = p.sb([64, 1], F32, "aneg")
    p.act(aneg[:], scal[0:64, 0:1], AF.Exp)
    p.ts(aneg[:], aneg[:], -1.0, ALU.mult)
    p.ts(gall[:], gall[:], aneg[:, 0:1], ALU.mult)

    wq = ld(wq_d, [128, 4], "wq")
    wk = ld(wk_d, [128, 4], "wk")
    wv = ld(wv_d, [64, 4], "wv")
    state = p.sb([128, 64], F32, "state")
    p.memset(state[:], 0.0)
    OB = 8
    RSQ = 128 ** -0.5

    def conv(src_d, nch, w, c0, key):
        t = p.rot(key + "_in", [128, CH + 3], F32, 1)
        p.dma(t[0:nch, :], src_d[:, c0:c0 + CH + 3])
        acc = p.rot(key + "_acc", [128, CH], F32, 1)
        p.ts(acc[0:nch, :], t[0:nch, 0:CH], w[:, 0:1], ALU.mult)
        for i in range(1, 4):
            p.stt(acc[0:nch, :], t[0:nch, i:i + CH], w[:, i:i + 1], acc[0:nch, :], ALU.mult, ALU.add)
        o = p.rot(key + "_o", [128, CH], F32, 1)
        p.act(o[0:nch, :], acc[0:nch, :], AF.Silu)
        return o

    import os
    LVL = int(os.environ.get('GDN_LVL', '9'))
    for sc in range(S // CH):
        qTs = conv(qT_d, 128, wq, sc * CH, "gq")
        kTs = conv(kT_d, 128, wk, sc * CH, "gk")
        vTs = conv(vT_d, 64, wv, sc * CH, "gv")
        for ci in range(CPS):
            c = sc * CPS + ci
            sl = slice(ci * 64, (ci + 1) * 64)
            PA = p.rot("g_PA", [128, 512], F32, 1, psum=True)
            PC = p.rot("g_PC", [64, 512], F32, 1, psum=True)
            PD = p.rot("g_PD", [128, 256], F32, 1, psum=True)
            ptq = PA[0:64, 0:128]
            p.tr(ptq, qTs[:, sl], ident[:])
            ptk = PA[0:64, 128:256]
            p.tr(ptk, kTs[:, sl], ident[:])
            ptv = PA[0:64, 256:320]
            p.tr(ptv, vTs[0:64, sl], ident[0:64, 0:64])
            junk = p.rot("g_junk", [64, 128], F32, 2)
            ssq = p.rot("g_ssq", [64, 2], F32, 2)
            p.act(junk[:], ptq, AF.Square, accum=ssq[:, 0:1])
            p.act(junk[:], ptk, AF.Square, accum=ssq[:, 1:2])
            rinv = p.rot("g_rinv", [64, 2], F32, 2)
            p.act(rinv[:], ssq[:], AF.Sqrt, bias=EPS)
            p.recip(rinv[:], rinv[:])
            if LVL < 1:
                continue
            g_c = gall[:, c:c + 1]
            b_c = beta[:, c:c + 1]
            pcol = PA[0:64, 320:321]
            p.mm(pcol, triu[:], g_c)
            gcol = p.rot("g_gcol", [64, 1], F32, 2)
            p.copy(gcol[:], pcol)
            g_bc = p.rot("g_gbc", [64, 128], F32, 2)
            p.ts(g_bc[:], ones_f[:], g_c, ALU.mult)
            prow = PA[:, 384:448]
            prow64 = PA[0:64, 384:448]
            p.mm(prow, g_bc[:], triu[:])
            nd = p.rot("g_nd", [64, 64], F32, 2)
            p.stt(nd[:], prow64, gcol[:, 0:1], mpos[:], ALU.subtract, ALU.add)
            dec_s = p.rot("g_decs", [64, 64], F32, 2)
            p.act(dec_s[:], nd[:], AF.Exp, scale=-1.0)
            d2 = p.rot("g_d2", [64, 64], F32, 2)
            p.stt(d2[:], prow64, gcol[:, 0:1], mneg[:], ALU.subtract, ALU.add)
            decT = p.rot("g_decT", [64, 64], F32, 2)
            p.act(decT[:], d2[:], AF.Exp)
            decTs = p.rot("g_decTs", [64, 64], F32, 2)
            p.tt(decTs[:], decT[:], st01[:], ALU.mult)
            egc = p.rot("g_egc", [64, 1], F32, 2)
            p.act(egc[:], gcol[:], AF.Exp)
            glast = p.rot("g_glast", [128, 1], F32, 2)
            p.copy(glast[:], PA[:, 447:448])
            dlast = p.rot("g_dlast", [64, 1], F32, 2)
            p.act(dlast[:], gcol[:], AF.Exp, bias=glast[0:64, 0:1], scale=-1.0)
            cdec = p.rot("g_cdec", [128, 1], F32, 2)
            p.act(cdec[:], glast[:], AF.Exp)
            if LVL < 2:
                continue
            k_n = p.rot("g_kn", [64, 128], F32, 2)
            p.ts(k_n[:], ptk, rinv[:, 1:2], ALU.mult)
            q_n = p.rot("g_qn", [64, 128], F32, 2)
            p.ts(q_n[:], ptq, rinv[:, 0:1], ALU.mult, RSQ, ALU.mult)
            kb = p.rot("g_kb", [64, 128], F32, 2)
            p.ts(kb[:], k_n[:], b_c, ALU.mult)
            R = p.rot("g_R", [64, 192], F32, 3)
            p.ts(R[:, 0:64], ptv, b_c, ALU.mult)
            p.ts(R[:, 64:192], kb[:], egc[:, 0:1], ALU.mult)
            qd = p.rot("g_qd", [64, 128], F32, 2)
            p.ts(qd[:], q_n[:], egc[:, 0:1], ALU.mult)
            kd = p.rot("g_kd", [64, 128], F32, 2)
            p.ts(kd[:], k_n[:], dlast[:, 0:1], ALU.mult)
            if LVL < 3:
                continue
            pT = p.rot("g_pT", [128, 4, 64], F32, 1, psum=True)
            p.tr(pT[:, 0, :], k_n[:], ident[0:64, 0:64])
            p.tr(pT[:, 1, :], kb[:], ident[0:64, 0:64])
            p.tr(pT[:, 2, :], q_n[:], ident[0:64, 0:64])
            p.tr(pT[:, 3, :], qd[:], ident[0:64, 0:64])
            fT = p.rot("g_fT", [128, 4, 64], F32, 2)
            p.copy(fT[:], pT[:])
            knT, kbT, qnT, qdT = fT[:, 0, :], fT[:, 1, :], fT[:, 2, :], fT[:, 3, :]
            if os.environ.get('GDN_SUB') == '1':
                continue
            pG0, pG1, pG2 = PC[:, 0:64], PC[:, 64:128], PC[:, 128:192]
            p.mm(pG0, kbT, knT)
            p.mm(pG1, knT, kbT)
            p.mm(pG2, knT, qnT)
            if os.environ.get('GDN_SUB') == '2':
                continue
            A = p.rot("g_A", [64, 64], F32, 2)
            p.tt(A[:], pG0, dec_s[:], ALU.mult)
            At = p.rot("g_At", [64, 64], F32, 2)
            p.tt(At[:], pG1, decTs[:], ALU.mult)
            attnT = p.rot("g_attnT", [64, 64], F32, 2)
            p.tt(attnT[:], pG2, decT[:], ALU.mult)
            if LVL < 4:
                continue
            for lvl in range(6):
                pR = PC[:, 192:384]
                p.mm(pR, At[:], R[:])
                Rn = p.rot("g_R", [64, 192], F32, 3)
                if lvl == 0:
                    p.tt(Rn[:], R[:], pR, ALU.subtract)
                else:
                    p.tt(Rn[:], R[:], pR, ALU.add)
                R = Rn
                if os.environ.get('GDN_SUB') == '3':
                    break
                if lvl < 5:
                    pP0, pP1 = PC[:, 384:448], PC[:, 448:512]
                    p.mm(pP0, At[:], A[:])
                    p.mm(pP1, A[:], At[:])
                    A2 = p.rot("g_A", [64, 64], F32, 2)
                    At2 = p.rot("g_At", [64, 64], F32, 2)
                    p.copy(A2[:], pP0)
                    p.copy(At2[:], pP1)
                    A, At = A2, At2
            if LVL < 5:
                continue
            pW = PD[:, 192:256]
            p.tr(pW, R[:, 64:192], ident[0:64, 0:64])
            wT = p.rot("g_wT", [128, 64], F32, 2)
            p.copy(wT[:], pW)
            if LVL < 6:
                continue
            pv = PD[0:64, 0:64]
            p.mm(pv, wT[:], state[:])
            vnew = p.rot("g_vnew", [64, 64], F32, 2)
            p.tt(vnew[:], R[:, 0:64], pv, ALU.subtract)
            po = PD[0:64, 64:128]
            p.mm(po, qdT, state[:], start=True, stop=False)
            p.mm(po, attnT[:], vnew[:], start=False, stop=True)
            if c % OB == 0:
                oo = p.rot("g_oo", [64, OB, 64], F32, 2)
            p.copy(oo[:, c % OB, :], po, e="act")
            if c % OB == OB - 1:
                c0 = (c - OB + 1) * 64
                p.dma(out.v(out.h[c0:c0 + OB * 64, :].rearrange("(b p) d -> p b d", p=64)), oo[:])
            pS = PD[:, 128:192]
            p.mm(pS, kd[:], vnew[:])
            p.stt(state[:], state[:], cdec[:, 0:1], pS, ALU.mult, ALU.add)
    p.finish([out])
    return nc, p


def load_w_bf16(p, w_d, rows, cols, dst, r0=0):
    cv = 0
    for k in range(rows // 128):
        for c0 in range(0, cols, 2048):
            n = min(2048, cols - c0)
            st = p.rot("wstage", [128, 2048], F32, 2)
            p.dma(st[:, 0:n], w_d[r0 + k * 128:r0 + (k + 1) * 128, c0:c0 + n], q=("sp" if cv % 2 == 0 else "pool"))
            p.copy(dst[:, k, c0:c0 + n], st[:, 0:n], e=("dve" if cv % 2 == 0 else "pool"))
            cv += 1


def build_k3a(ntiles=NT):
    nc = bass.Bass("TRN2", target_bir_lowering=False)
    p = P(nc)
    ntok = ntiles * 128
    di = lambda n, s: p.dram(n, s, F32, kind="ExternalInput")
    x = di("x", [ntok, D]); z_d = di("z", [ntok, 512]); gg_d = di("ggate", [ntok, 512]); brg_d = di("brg", [ntok, 3072])
    ya_d = di("ya", [ntok, 512]); yb_d = di("yb", [ntok, 512]); yc_d = di("yc", [ntok, 512])
    c_in = di("c_col", [128, 8]); wada = di("wada", [D, 4 * D]); bada = di("bada", [1, 4 * D])
    ssmg_d = di("ssm_g", [1, 512]); gdng_d = di("gdn_g", [1, 128]); ffng_d = di("ffn_g", [1, D])
    wbr_d = di("w_branch", [1536, D]); wout_d = di("w_out", [D, D]); wgr_d = di("wgr", [D, 36]); bgr_d = di("bgr", [1, 36])
    ident_d = di("ident_in", [128, 128])
    x1_o = p.dram("x1", [ntok, D], F32, kind="ExternalOutput")
    h2_o = p.dram("h2", [ntok, D], F32, kind="ExternalOutput")
    wt_o = p.dram("wt", [ntok, 32], F32, kind="ExternalOutput")
    gf_o = p.dram("gf", [128, D], F32, kind="ExternalOutput")

    ident = p.sb([128, 128], F32, "ident")
    p.dma(ident[:], ident_d[:])
    identb = p.sb([128, 128], BF16, "identb")
    p.copy(identb[:], ident[:])
    ones_f = p.sb([128, 128], F32, "ones_f")
    p.memset(ones_f[:], 1.0)
    c_bc = emit_cbc(p, c_in, ones_f)
    gate_m = p.sb([128, D], F32, "gate_m"); shift_f = p.sb([128, D], F32, "shift_f")
    gmod_f = p.sb([128, D], F32, "gmod_f"); gate_f = p.sb([128, D], F32, "gate_f")
    emit_mod(p, c_bc, wada, bada, 0, D, gate_m, ones_f)
    emit_mod(p, c_bc, wada, bada, D, D, shift_f, ones_f)
    emit_mod(p, c_bc, wada, bada, 2 * D, D, gmod_f, ones_f, plus_one=True)
    emit_mod(p, c_bc, wada, bada, 3 * D, D, gate_f, ones_f)
    p.dma(gf_o[:], gate_f[:])
    ffng = p.sb([128, D], F32, "ffng")
    p.dma(ffng[:], bcast_row(ffng_d, D))
    p.tt(gmod_f[:], gmod_f[:], ffng[:], ALU.mult)
    ssmg = p.sb([128, 512], F32, "ssmg")
    p.dma(ssmg[:], bcast_row(ssmg_d, 512))
    gdng = p.sb([128, 128], F32, "gdng")
    p.dma(gdng[:], bcast_row(gdng_d, 128))
    wgr = p.sb([128, 8, 36], F32, "wgr")
    p.dma(wgr[:], wgr_d.v(wgr_d.h[:, :].rearrange("(k p) n -> p k n", p=128)))
    bgr = p.sb([1, 36], F32, "bgr")
    p.dma(bgr[:], bgr_d[:])
    Wbr = p.sb([128, 12, D], BF16, "Wbr")
    load_w_bf16(p, wbr_d, 1536, D, Wbr)
    Wout = p.sb([128, 8, D], BF16, "Wout")
    load_w_bf16(p, wout_d, D, D, Wout)

    for t in range(ntiles):
        rs = slice(t * 128, (t + 1) * 128)
        xt = p.rot("xt", [128, D], F32, 2)
        p.dma(xt[:], x[rs, :])
        zt = p.rot("zt", [128, 512], F32, 2); p.dma(zt[:], z_d[rs, :], q="pool")
        gt = p.rot("gt", [128, 512], F32, 2); p.dma(gt[:], gg_d[rs, :])
        bt = p.rot("bt", [128, 3072], F32, 1); p.dma(bt[:], brg_d[rs, :], q="pool")
        ya = p.rot("ya", [128, 512], F32, 2); p.dma(ya[:], ya_d[rs, :])
        yb = p.rot("yb", [128, 512], F32, 2); p.dma(yb[:], yb_d[rs, :], q="pool")
        yc = p.rot("yc", [128, 512], F32, 2); p.dma(yc[:], yc_d[rs, :])
        brn = p.rot("brn", [128, 1536], BF16, 2)
        p.act(zt[:], zt[:], AF.Silu)
        p.tt(ya[:], ya[:], zt[:], ALU.mult)
        junk = p.rot("junk", [128, 512], F32, 1)
        ssq = p.rot("ssq", [128, 8], F32, 2)
        for g in range(2):
            p.act(junk[:, 0:256], ya[:, g * 256:(g + 1) * 256], AF.Square, accum=ssq[:, g:g + 1])
        for hd in range(4):
            p.act(junk[:, 0:128], yc[:, hd * 128:(hd + 1) * 128], AF.Square, accum=ssq[:, 2 + hd:3 + hd])
        rstd = p.rot("rstd", [128, 8], F32, 2)
        p.act(rstd[:, 0:2], ssq[:, 0:2], AF.Sqrt, bias=EPS, scale=1.0 / 256)
        p.act(rstd[:, 2:6], ssq[:, 2:6], AF.Sqrt, bias=EPS, scale=1.0 / 128)
        p.recip(rstd[:, 0:6], rstd[:, 0:6])
        for g in range(2):
            sl = slice(g * 256, (g + 1) * 256)
            p.stt(brn[:, sl], ya[:, sl], rstd[:, g:g + 1], ssmg[:, sl], ALU.mult, ALU.mult)
        p.copy(brn[:, 512:1024], yb[:], e="pool")
        p.act(gt[:], gt[:], AF.Silu)
        for hd in range(4):
            sl = slice(hd * 128, (hd + 1) * 128)
            p.stt(yc[:, sl], yc[:, sl], rstd[:, 2 + hd:3 + hd], gdng[:], ALU.mult, ALU.mult)
        p.tt(brn[:, 1024:1536], yc[:], gt[:], ALU.mult)
        brT = p.rot("brT", [128, 12, 128], BF16, 2)
        for q4 in range(3):
            pt = p.rot("psT", [128, 4, 128], BF16, 2, psum=True)
            for i in range(4):
                kk = q4 * 4 + i
                p.tr(pt[:, i, :], brn[:, kk * 128:(kk + 1) * 128], identb[:])
            p.copy(brT[:, q4 * 4:q4 * 4 + 4, :], pt[:])
        p.act(bt[:], bt[:], AF.Sigmoid)
        merged = p.rot("merged", [128, D], F32, 1)
        for nb in range(2):
            cs = slice(nb * 512, (nb + 1) * 512)
            for i in range(3):
                ps = p.rot("ps_mm", [128, 512], F32, 4, psum=True)
                for k in range(4):
                    p.mm(ps[:], brT[:, 4 * i + k, :], Wbr[:, 4 * i + k, cs], start=(k == 0), stop=(k == 3))
                if i == 0:
                    p.tt(merged[:, cs], ps[:], bt[:, i * D + nb * 512: i * D + (nb + 1) * 512], ALU.mult)
                else:
                    tmp = p.rot("mtmp", [128, 512], F32, 2)
                    p.tt(tmp[:], ps[:], bt[:, i * D + nb * 512: i * D + (nb + 1) * 512], ALU.mult)
                    p.tt(merged[:, cs], merged[:, cs], tmp[:], ALU.add)
        mb = p.rot("mb", [128, D], BF16, 1)
        p.copy(mb[:], merged[:], e="pool")
        mT = p.rot("mT", [128, 8, 128], BF16, 1)
        for q4 in range(2):
            pt = p.rot("psT", [128, 4, 128], BF16, 2, psum=True)
            for i in range(4):
                kk = q4 * 4 + i
                p.tr(pt[:, i, :], mb[:, kk * 128:(kk + 1) * 128], identb[:])
            p.copy(mT[:, q4 * 4:q4 * 4 + 4, :], pt[:])
        x1 = p.rot("x1", [128, D], F32, 2)
        for nb in range(2):
            cs = slice(nb * 512, (nb + 1) * 512)
            ps = p.rot("ps_mm", [128, 512], F32, 4, psum=True)
            for k in range(8):
                p.mm(ps[:], mT[:, k, :], Wout[:, k, cs], start=(k == 0), stop=(k == 7))
            tmp = p.rot("mtmp", [128, 512], F32, 2)
            p.tt(tmp[:], ps[:], gate_m[:, cs], ALU.mult)
            p.tt(x1[:, cs], tmp[:], xt[:, cs], ALU.add)
        p.dma(x1_o[rs, :], x1[:])
        h2 = p.rot("h2", [128, D], F32, 2)
        emit_rmsnorm_mod(p, x1[:], gmod_f[:], shift_f[:], h2[:])
        p.dma(h2_o[rs, :], h2[:], q="pool")
        h2T = p.rot("h2Tf", [128, 8, 128], F32, 1)
        for q4 in range(2):
            pt = p.rot("psTf", [128, 4, 128], F32, 1, psum=True)
            for i in range(4):
                kk = q4 * 4 + i
                p.tr(pt[:, i, :], h2[:, kk * 128:(kk + 1) * 128], ident[:])
            p.copy(h2T[:, q4 * 4:q4 * 4 + 4, :], pt[:])
        pr = p.rot("ps_r", [128, 36], F32, 1, psum=True)
        for k in range(8):
            p.mm(pr[:], h2T[:, k, :], wgr[:, k, :], start=(k == 0), stop=False)
        p.mm(pr[:], ones_f[0:1, :], bgr[0:1, :], start=False, stop=True)
        lg = p.rot("lg", [128, 36], F32, 2)
        p.copy(lg[:], pr[:])
        sm = p.rot("rt_sm", [128, 16], F32, 2)
        gmax, ngmax, sg, pgrp, m1, m2, e2, rden, w1, w2 = [sm[:, i:i + 1] for i in range(10)]
        p.reduce(gmax, lg[:, 0:4], op=ALU.max)
        oh = p.rot("rt_oh", [128, 4], F32, 2)
        p.ts(oh[:], lg[:, 0:4], gmax, ALU.is_equal)
        p.ts(ngmax, gmax, -1.0, ALU.mult)
        eg = p.rot("rt_eg", [128, 4], F32, 2)
        p.act(eg[:], lg[:, 0:4], AF.Exp, bias=ngmax)
        p.reduce(sg, eg[:])
        p.recip(pgrp, sg)
        el = p.rot("rt_el", [128, 8], F32, 2)
        p.ts(el[:], lg[:, 4:12], oh[:, 0:1], ALU.mult)
        for g in range(1, 4):
            p.stt(el[:], lg[:, 4 + 8 * g:12 + 8 * g], oh[:, g:g + 1], el[:], ALU.mult, ALU.add)
        p.reduce(m1, el[:], op=ALU.max)
        mk1 = p.rot("rt_mk1", [128, 8], F32, 2)
        p.ts(mk1[:], el[:], m1, ALU.is_equal)
        el2 = p.rot("rt_el2", [128, 8], F32, 2)
        p.ts(el2[:], mk1[:], -1e30, ALU.mult)
        p.tt(el2[:], el2[:], el[:], ALU.add)
        p.reduce(m2, el2[:], op=ALU.max)
        mk2 = p.rot("rt_mk2", [128, 8], F32, 2)
        p.ts(mk2[:], el2[:], m2, ALU.is_equal)
        p.tt(e2, m2, m1, ALU.subtract)
        p.act(e2, e2, AF.Exp)
        p.ts(rden, e2, 1.0, ALU.add)
        p.recip(rden, rden)
        p.tt(w1, rden, pgrp, ALU.mult)
        p.tt(w2, w1, e2, ALU.mult)
        wexp = p.rot("rt_wexp", [128, 8], F32, 2)
        p.ts(wexp[:], mk1[:], w1, ALU.mult)
        p.stt(wexp[:], mk2[:], w2, wexp[:], ALU.mult, ALU.add)
        wt = p.rot("rt_wt", [128, 32], F32, 2)
        for g in range(4):
            p.ts(wt[:, 8 * g:8 * g + 8], wexp[:], oh[:, g:g + 1], ALU.mult)
        p.dma(wt_o[rs, :], wt[:])
    p.finish([x1_o, h2_o, wt_o, gf_o])
    return nc, p


def build_k3b(ntiles=NT, nexp=32):
    nc = bass.Bass("TRN2", target_bir_lowering=False)
    p = P(nc)
    ntok = ntiles * 128
    di = lambda n, s: p.dram(n, s, F32, kind="ExternalInput")
    x1_d = di("x1", [ntok, D]); h2_d = di("h2", [ntok, D]); wt_d = di("wt", [ntok, 32]); gf_d = di("gf", [128, D])
    wg_d = di("w_gate", [nexp * D, 512]); wu_d = di("w_up", [nexp * D, 512]); wd_d = di("w_down", [nexp * 512, D])
    ident_d = di("ident_in", [128, 128])
    xo = p.dram("xo", [ntok, D], F32, kind="ExternalOutput")
    ident = p.sb([128, 128], F32, "ident")
    p.dma(ident[:], ident_d[:])
    identb = p.sb([128, 128], BF16, "identb")
    p.copy(identb[:], ident[:])
    gf = p.sb([128, D], F32, "gf")
    p.dma(gf[:], gf_d[:])
    x1 = p.sb([128, ntiles, D], F32, "x1")
    h2T = p.sb([128, ntiles, 8, 128], BF16, "h2T")
    wt = p.sb([128, ntiles, 32], F32, "wt")
    for t in range(ntiles):
        rs = slice(t * 128, (t + 1) * 128)
        p.dma(x1[:, t, :], x1_d[rs, :])
        p.dma(wt[:, t, :], wt_d[rs, :], q="pool")
        ht = p.rot("ht", [128, D], F32, 2)
        p.dma(ht[:], h2_d[rs, :], q="pool")
        hb = p.rot("hb", [128, D], BF16, 2)
        p.copy(hb[:], ht[:])
        for q4 in range(2):
            pt = p.rot("psT", [128, 4, 128], BF16, 2, psum=True)
            for i in range(4):
                kk = q4 * 4 + i
                p.tr(pt[:, i, :], hb[:, kk * 128:(kk + 1) * 128], identb[:])
            p.copy(h2T[:, t, q4 * 4:q4 * 4 + 4, :], pt[:])
    for e in range(nexp):
        Wg = p.rot("Wg", [128, 8, 512], BF16, 2)
        Wu = p.rot("Wu", [128, 8, 512], BF16, 2)
        Wd = p.rot("Wd", [128, 4, D], BF16, 2)
        load_w_bf16(p, wg_d, D, 512, Wg, r0=e * D)
        load_w_bf16(p, wu_d, D, 512, Wu, r0=e * D)
        for k in range(4):
            st = p.rot("wstage", [128, 2048], F32, 2)
            p.dma(st[:, 0:D], wd_d[e * 512 + k * 128:e * 512 + (k + 1) * 128, :])
            p.tt(Wd[:, k, :], st[:, 0:D], gf[:], ALU.mult)
        for t in range(ntiles):
            pg = p.rot("ps_mm", [128, 512], F32, 4, psum=True)
            for k in range(8):
                p.mm(pg[:], h2T[:, t, k, :], Wg[:, k, :], start=(k == 0), stop=(k == 7))
            pu = p.rot("ps_mm", [128, 512], F32, 4, psum=True)
            for k in range(8):
                p.mm(pu[:], h2T[:, t, k, :], Wu[:, k, :], start=(k == 0), stop=(k == 7))
            sg = p.rot("sg", [128, 512], F32, 2)
            p.act(sg[:], pg[:], AF.Silu)
            hid = p.rot("hid", [128, 512], BF16, 2)
            p.stt(hid[:], sg[:], wt[:, t, e:e + 1], pu[:], ALU.mult, ALU.mult)
            pt = p.rot("psT", [128, 4, 128], BF16, 2, psum=True)
            for i in range(4):
                p.tr(pt[:, i, :], hid[:, i * 128:(i + 1) * 128], identb[:])
            hT = p.rot("hidT", [128, 4, 128], BF16, 2)
            p.copy(hT[:], pt[:], e="pool" if False else "dve")
            for nb in range(2):
                cs = slice(nb * 512, (nb + 1) * 512)
                py = p.rot("ps_mm", [128, 512], F32, 4, psum=True)
                for k in range(4):
                    p.mm(py[:], hT[:, k, :], Wd[:, k, cs], start=(k == 0), stop=(k == 3))
                p.tt(x1[:, t, cs], x1[:, t, cs], py[:], ALU.add)
    for t in range(ntiles):
        p.dma(xo[t * 128:(t + 1) * 128, :], x1[:, t, :], q=("sp" if t % 2 == 0 else "pool"))
    p.finish([xo])
    return nc, p


_PROGS = {}


def _prog(name, fn):
    if name not in _PROGS:
        _PROGS[name] = fn()[0]
    return _PROGS[name]


def _run(nc, in_maps):
    in_maps = [{k: np.ascontiguousarray(v, dtype=np.float32) for k, v in m.items()} for m in in_maps]
    return run_bass_kernel_spmd(nc, in_maps, core_ids=list(range(NCORE))).results


def _padT(a):
    return np.ascontiguousarray(np.concatenate([np.zeros((a.shape[1], 3), np.float32), a.T], 1))


def _sb_masks(j):
    kk = (np.arange(8)[:, None, None] * 128 + np.arange(128)[None, :, None])
    qq = j * 512 + np.arange(512)[None, None, :]
    return np.ascontiguousarray((kk < qq).astype(np.float32).transpose(1, 0, 2))


def kernel(x, c, w_ada, b_ada, norm_mix, norm_ffn, w_in, ssm_conv_w, ssm_conv_b, ssm_dt_bias, ssm_a_log, ssm_d,
           ssm_norm, sb_q_norm, sb_k_norm, gdn_conv_w, gdn_a_log, gdn_dt_bias, gdn_norm, w_branch, w_out,
           w_group, b_group, w_router, b_router, w_gate, w_up, w_down):
    f32 = lambda a: np.asarray(a, dtype=np.float32)
    x = f32(x)[0]
    c_col = np.ascontiguousarray(f32(c)[0].reshape(8, 128).T)
    I = np.eye(128, dtype=np.float32)
    i128 = np.arange(128)
    i64 = np.arange(64)
    triu128 = (i128[:, None] <= i128[None, :]).astype(np.float32)
    mneg128 = np.where(i128[:, None] <= i128[None, :], 0.0, -30000.0).astype(np.float32)
    mincl = (i128[:, None] >= i128[None, :]).astype(np.float32)
    triu64 = (i64[:, None] <= i64[None, :]).astype(np.float32)
    mpos64 = np.where(i64[:, None] > i64[None, :], 0.0, 30000.0).astype(np.float32)
    mneg64 = np.where(i64[:, None] <= i64[None, :], 0.0, -30000.0).astype(np.float32)
    st01 = (i64[:, None] < i64[None, :]).astype(np.float32)
    sbm = [_sb_masks(0), _sb_masks(1)]
    k1 = _prog("k1", build_k1)
    kssd = _prog("ssd", build_ssd)
    ksb = _prog("sb", build_sb)
    kgdn = _prog("gdn", build_gdn)
    k3a = _prog("k3a", build_k3a)
    k3b = _prog("k3b", build_k3b)
    for l in range(4):
        wa = f32(w_ada[l]); ba = f32(b_ada[l])
        r = _run(k1, [{"x": x[TOK * i:TOK * (i + 1)], "c_col": c_col, "wada": wa[:, 0:2 * D], "bada": ba[None, 0:2 * D],
                       "norm_g": f32(norm_mix[l])[None], "w_in": f32(w_in[l]), "ident_in": I} for i in range(NCORE)])
        proj = np.concatenate([r[i]["proj"] for i in range(NCORE)], 0)
        m_z = proj[:, 0:512]; m_xbc = proj[:, 512:1536]; m_dt = proj[:, 1536:1544]
        sbq = proj[:, 1544:3080]; gq = proj[:, 3080:4616]; g_a = proj[:, 4616:4620]; g_b = proj[:, 4620:4624]
        g_gate = proj[:, 4624:5136]; br_gate = proj[:, 5136:8208]
        cw = f32(ssm_conv_w[l]); cb = f32(ssm_conv_b[l])
        ims = []
        for i in range(NCORE):
            g = i // 4
            xs = slice(64 * i, 64 * i + 64); bs = slice(512 + 128 * g, 640 + 128 * g); cs = slice(768 + 128 * g, 896 + 128 * g)
            ims.append({"xT": _padT(m_xbc[:, xs]), "BT": _padT(m_xbc[:, bs]), "CT": _padT(m_xbc[:, cs]),
                        "wx": cw[:, xs].T, "bx": cb[xs, None], "wB": cw[:, bs].T, "bB": cb[bs, None], "wC": cw[:, cs].T, "bC": cb[cs, None],
                        "dt_col": m_dt[:, i].reshape(-1, 128).T,
                        "scal": np.tile(np.array([[f32(ssm_dt_bias[l])[i], f32(ssm_a_log[l])[i], f32(ssm_d[l])[i]]], np.float32), (128, 1)),
                        "triu": triu128, "mneg": mneg128, "ident_in": I})
        r = _run(kssd, ims)
        y_ssd = np.concatenate([r[i]["y"] for i in range(NCORE)], 1)
        ims = []
        for i in range(NCORE):
            h, j = i // 2, i % 2
            q = sbq[:, 128 * h:128 * h + 128]
            qsel = np.concatenate([q[(2 * m + j) * 512:(2 * m + j + 1) * 512] for m in range(16)], 0)
            ims.append({"q": qsel, "k": sbq[:, 512 + 128 * h:640 + 128 * h], "v": sbq[:, 1024 + 128 * h:1152 + 128 * h],
                        "qg": f32(sb_q_norm[l])[None], "kg": f32(sb_k_norm[l])[None], "mask": sbm[j], "mincl": mincl, "ident_in": I})
        r = _run(ksb, ims)
        y_sb = np.zeros((SEQ, 512), np.float32)
        for i in range(NCORE):
            h, j = i // 2, i % 2
            oT = r[i]["oT"]
            for m in range(16):
                g = 2 * m + j
                y_sb[g * 512:(g + 1) * 512, 128 * h:128 * h + 128] = oT[:, m * 512:(m + 1) * 512].T
        gw = f32(gdn_conv_w[l])
        ims = []
        for i in range(NCORE):
            h, e = i // 2, i % 2
            qs = slice(128 * h, 128 * h + 128); ks = slice(512 + 128 * h, 640 + 128 * h); vs = slice(1024 + 128 * h + 64 * e, 1024 + 128 * h + 64 * e + 64)
            ims.append({"qT": _padT(gq[:, qs]), "kT": _padT(gq[:, ks]), "vT": _padT(gq[:, vs]),
                        "wq": gw[:, qs].T, "wk": gw[:, ks].T, "wv": gw[:, vs].T,
                        "a_col": g_a[:, h].reshape(-1, 64).T, "b_col": g_b[:, h].reshape(-1, 64).T,
                        "scal": np.tile(np.array([[f32(gdn_a_log[l])[h], f32(gdn_dt_bias[l])[h]]], np.float32), (128, 1)),
                        "triu": triu64, "mpos": mpos64, "mneg": mneg64, "st01": st01, "ident_in": I})
        r = _run(kgdn, ims)
        o_gdn = np.zeros((SEQ, 512), np.float32)
        for i in range(NCORE):
            h, e = i // 2, i % 2
            o_gdn[:, 128 * h + 64 * e:128 * h + 64 * e + 64] = r[i]["o"]
        wgr = np.concatenate([f32(w_group[l]), f32(w_router[l])], 1)
        bgr = np.concatenate([f32(b_group[l]), f32(b_router[l])])[None]
        ims = []
        for i in range(NCORE):
            ts_ = slice(TOK * i, TOK * (i + 1))
            ims.append({"x": x[ts_], "z": m_z[ts_], "ggate": g_gate[ts_], "brg": br_gate[ts_], "ya": y_ssd[ts_], "yb": y_sb[ts_], "yc": o_gdn[ts_],
                        "c_col": c_col, "wada": wa[:, 2 * D:6 * D], "bada": ba[None, 2 * D:6 * D],
                        "ssm_g": f32(ssm_norm[l])[None], "gdn_g": f32(gdn_norm[l])[None], "ffn_g": f32(norm_ffn[l])[None],
                        "w_branch": f32(w_branch[l]).reshape(1536, D), "w_out": f32(w_out[l]), "wgr": wgr, "bgr": bgr, "ident_in": I})
        ra = _run(k3a, ims)
        wg = f32(w_gate[l]).reshape(-1, 512); wu = f32(w_up[l]).reshape(-1, 512); wd = f32(w_down[l]).reshape(-1, D)
        ims = [{"x1": ra[i]["x1"], "h2": ra[i]["h2"], "wt": ra[i]["wt"], "gf": ra[i]["gf"], "w_gate": wg, "w_up": wu, "w_down": wd, "ident_in": I}
               for i in range(NCORE)]
        rb = _run(k3b, ims)
        x = np.concatenate([rb[i]["xo"] for i in range(NCORE)], 0)
    return x[None].astype(np.float32)
```

```python
import numpy as np
import concourse.bass as bass
import concourse.mybir as mybir
from concourse.bass_utils import run_bass_kernel_spmd

F32 = mybir.dt.float32
BF16 = mybir.dt.bfloat16
I32 = mybir.dt.int32
U32 = mybir.dt.uint32
AF = mybir.ActivationFunctionType
ALU = mybir.AluOpType
AX = mybir.AxisListType

NDS = 12


class Buf:
    __slots__ = ("name", "w", "r")

    def __init__(self, name):
        self.name = name
        self.w = None
        self.r = {}


class V:
    __slots__ = ("ap", "bufs")

    def __init__(self, ap, bufs):
        self.ap = ap
        self.bufs = bufs


class T:
    def __init__(self, h, name, nbuf_axis=None, nbuf=1):
        self.h = h
        self.name = name
        self.buf = Buf(name)

    def __getitem__(self, idx):
        return V(self.h[idx], (self.buf,))

    def v(self, ap):
        return V(ap, (self.buf,))


class P:
    def __init__(self, nc, same_eng_sync=True):
        self.nc = nc
        self.engs = {"pe": nc.tensor, "dve": nc.vector, "act": nc.scalar, "pool": nc.gpsimd, "sp": nc.sync}
        self.sem = {k: nc.alloc_semaphore(name=f"s_{k}") for k in self.engs}
        self.cnt = {k: 0 for k in self.engs}
        self.seen = {k: {} for k in self.engs}
        self.dsem = {}
        self.dcnt = {}
        self.dnext = {}
        for q in ("sp", "pool", "act"):
            self.dsem[q] = [nc.alloc_semaphore(name=f"d_{q}{i}") for i in range(NDS)]
            self.dcnt[q] = [0] * NDS
            self.dnext[q] = 0
        self.same_eng_sync = same_eng_sync
        self.n_wait = 0
        self.n_ins = 0
        self._n = 0

    def sb(self, shape, dt=F32, name=None):
        self._n += 1
        name = name or f"sb{self._n}"
        h = self.nc.alloc_sbuf_tensor("S_" + name, list(shape), dt)
        return T(h, name)

    def ps(self, shape, dt=F32, name=None):
        self._n += 1
        name = name or f"ps{self._n}"
        h = self.nc.alloc_psum_tensor("P_" + name, list(shape), dt)
        return T(h, name)

    def dram(self, name, shape, dt=F32, kind="Internal"):
        h = self.nc.dram_tensor(name, list(shape), dt, kind=kind)
        return T(h, name)

    def rot(self, key, shape, dt, n, psum=False):
        if not hasattr(self, "_rot"):
            self._rot = {}
        if key not in self._rot:
            tiles = [(self.ps if psum else self.sb)(shape, dt, f"{key}_{i}") for i in range(n)]
            self._rot[key] = [tiles, 0]
        ent = self._rot[key]
        t = ent[0][ent[1] % len(ent[0])]
        ent[1] += 1
        return t

    def _semof(self, src):
        if src[0] == "e":
            return self.sem[src[1]]
        return self.dsem[src[1]][src[2]]

    def _wait(self, e, deps):
        best = {}
        for d in deps:
            if d is None:
                continue
            key = d[:-1]
            if key not in best or best[key] < d[-1]:
                best[key] = d[-1]
        for key, c in best.items():
            if key[0] == "e" and key[1] == e and not (self.same_eng_sync and e not in ("pe",)):
                continue
            if self.seen[e].get(key, 0) >= c:
                continue
            self.seen[e][key] = c
            self.engs[e].wait_ge(self._semof(key), c)
            self.n_wait += 1

    def _deps(self, reads, writes):
        deps = set()
        for v in reads:
            for b in v.bufs:
                if b.w is not None:
                    deps.add(b.w)
        for v in writes:
            for b in v.bufs:
                if b.w is not None:
                    deps.add(b.w)
                deps.update(b.r.values())
        return deps

    def _commit(self, tok, reads, writes):
        for v in writes:
            for b in v.bufs:
                b.w = tok
                b.r = {}
        wb = set()
        for v in writes:
            wb.update(id(b) for b in v.bufs)
        for v in reads:
            for b in v.bufs:
                if id(b) not in wb:
                    b.r[tok[:-1]] = tok

    def op(self, e, fn, reads=(), writes=()):
        self._wait(e, self._deps(reads, writes))
        ins = fn(self.engs[e])
        self.cnt[e] += 1
        ins.then_inc(self.sem[e], 1)
        self.n_ins += 1
        self._commit(("e", e, self.cnt[e]), reads, writes)
        return ins

    def dma(self, out, in_, q="sp", **kw):
        i = self.dnext[q]
        self.dnext[q] = (i + 1) % NDS
        deps = self._deps([in_], [out])
        if self.dcnt[q][i] > 0:
            deps.add(("d", q, i, 16 * self.dcnt[q][i]))
        self._wait(q, deps)
        ins = self.engs[q].dma_start(out=out.ap, in_=in_.ap, **kw)
        self.dcnt[q][i] += 1
        ins.then_inc(self.dsem[q][i], 16)
        self.n_ins += 1
        self._commit(("d", q, i, 16 * self.dcnt[q][i]), [in_], [out])
        return ins

    def finish(self, outs, e="sp"):
        deps = set()
        for t in outs:
            if t.buf.w is not None:
                deps.add(t.buf.w)
        for k in self.engs:
            if self.cnt[k] > 0:
                deps.add(("e", k, self.cnt[k]))
        for q in self.dsem:
            for i in range(NDS):
                if self.dcnt[q][i] > 0:
                    deps.add(("d", q, i, 16 * self.dcnt[q][i]))
        self._wait(e, deps)

    def mm(self, out, lhsT, rhs, start=True, stop=True):
        rd = [lhsT, rhs]
        return self.op("pe", lambda e: e.matmul(out.ap, lhsT.ap, rhs.ap, start=start, stop=stop), rd, [out])

    def tr(self, out, in_, ident):
        return self.op("pe", lambda e: e.transpose(out.ap, in_.ap, ident.ap), [in_, ident], [out])

    def act(self, out, in_, func, bias=None, scale=None, accum=None, e="act"):
        kw = {}
        rd = [in_]
        wr = [out]
        if bias is not None:
            if isinstance(bias, V):
                kw["bias"] = bias.ap
                rd.append(bias)
            else:
                kw["bias"] = bias
        if scale is not None:
            if isinstance(scale, V):
                kw["scale"] = scale.ap
                rd.append(scale)
            else:
                kw["scale"] = scale
        if accum is not None:
            kw["accum_out"] = accum.ap
            wr.append(accum)
        return self.op("act", lambda en: en.activation(out.ap, in_.ap, func, **kw), rd, wr)

    def tt(self, out, a, b, op, e="dve"):
        return self.op(e, lambda en: en.tensor_tensor(out.ap, a.ap, b.ap, op), [a, b], [out])

    def ts(self, out, a, s1, op0, s2=None, op1=None, accum=None, e="dve"):
        rd = [a]
        wr = [out]
        s1a = s1.ap if isinstance(s1, V) else s1
        s2a = s2.ap if isinstance(s2, V) else s2
        if isinstance(s1, V):
            rd.append(s1)
        if isinstance(s2, V):
            rd.append(s2)
        kw = {}
        if op1 is not None:
            kw["op1"] = op1
        if accum is not None:
            kw["accum_out"] = accum.ap
            wr.append(accum)
        return self.op(e, lambda en: en.tensor_scalar(out.ap, a.ap, s1a, s2a, op0, **kw), rd, wr)

    def stt(self, out, a, s, b, op0, op1, e="dve"):
        rd = [a, b]
        sa = s.ap if isinstance(s, V) else s
        if isinstance(s, V):
            rd.append(s)
        return self.op(e, lambda en: en.scalar_tensor_tensor(out.ap, a.ap, sa, b.ap, op0, op1), rd, [out])

    def copy(self, out, in_, e="dve"):
        if e == "act":
            return self.op("act", lambda en: en.copy(out.ap, in_.ap), [in_], [out])
        return self.op(e, lambda en: en.tensor_copy(out.ap, in_.ap), [in_], [out])

    def memset(self, out, val, e="dve"):
        return self.op(e, lambda en: en.memset(out.ap, val), [], [out])

    def reduce(self, out, in_, op=ALU.add, axis=AX.X, e="dve"):
        return self.op(e, lambda en: en.tensor_reduce(out.ap, in_.ap, axis, op), [in_], [out])

    def recip(self, out, in_):
        return self.op("dve", lambda en: en.reciprocal(out.ap, in_.ap), [in_], [out])


D = 1024
SEQ = 16384
NCORE = 8
TOK = SEQ // NCORE
NT = TOK // 128
IN_COLS = 8208
EPS = 1e-6
DEBUG = False
SAME_ENG = False


def bcast_row(t, n, parts=128):
    return t.v(t.h[0:1, 0:n].to_broadcast([parts, n]))


def emit_mod(p, c_bc, wada, bada, col0, ncols, out_tile, ones1, plus_one=False):
    nblk = (ncols + 511) // 512
    for b in range(nblk):
        n = min(512, ncols - b * 512)
        ws = p.rot("modw", [128, 8, 512], F32, 1)
        p.dma(ws[:, :, 0:n], wada.v(wada.h[:, col0 + b * 512: col0 + b * 512 + n].rearrange("(k p) n -> p k n", p=128)))
        bs = p.rot("modb", [1, 512], F32, 2)
        p.dma(bs[0:1, 0:n], bada[0:1, col0 + b * 512: col0 + b * 512 + n])
        ps = p.rot("ps_mm", [128, 512], F32, 4, psum=True)
        for k in range(8):
            p.mm(ps[:, 0:n], c_bc[:, k, :], ws[:, k, 0:n], start=(k == 0), stop=False)
        p.mm(ps[:, 0:n], ones1[0:1, :], bs[0:1, 0:n], start=False, stop=True)
        if plus_one:
            p.ts(out_tile[:, b * 512: b * 512 + n], ps[:, 0:n], 1.0, ALU.add)
        else:
            p.copy(out_tile[:, b * 512: b * 512 + n], ps[:, 0:n])


def emit_cbc(p, c_in, ones_f):
    c_col = p.sb([128, 8], F32, "c_col")
    p.dma(c_col[:], c_in[:])
    c_act = p.sb([128, 8], F32, "c_act")
    p.act(c_act[:], c_col[:], AF.Silu)
    c_bc = p.sb([128, 8, 128], F32, "c_bc")
    for k in range(8):
        p.ts(c_bc[:, k, :], ones_f[:], c_act[:, k:k + 1], ALU.mult)
    return c_bc


def emit_rmsnorm_mod(p, x_t, gmod, shift, h_out, width=D):
    junk = p.rot("rn_junk", [128, width], F32, 1)
    ssq = p.rot("rn_ssq", [128, 1], F32, 2)
    p.act(junk[:], x_t, AF.Square, accum=ssq[:])
    rstd = p.rot("rn_rstd", [128, 1], F32, 2)
    p.act(rstd[:], ssq[:], AF.Sqrt, bias=EPS, scale=1.0 / width)
    p.recip(rstd[:], rstd[:])
    tmp = p.rot("rn_tmp", [128, width], F32, 1)
    p.stt(tmp[:], x_t, rstd[:], gmod, ALU.mult, ALU.mult)
    p.tt(h_out, tmp[:], shift, ALU.add)


def build_k1(ntiles=NT, ncols=IN_COLS):
    nc = bass.Bass("TRN2", target_bir_lowering=False)
    p = P(nc)
    ntok = ntiles * 128
    x = p.dram("x", [ntok, D], F32, kind="ExternalInput")
    c_in = p.dram("c_col", [128, 8], F32, kind="ExternalInput")
    wada = p.dram("wada", [D, 2 * D], F32, kind="ExternalInput")
    bada = p.dram("bada", [1, 2 * D], F32, kind="ExternalInput")
    ng = p.dram("norm_g", [1, D], F32, kind="ExternalInput")
    w_in = p.dram("w_in", [D, ncols], F32, kind="ExternalInput")
    ident_d = p.dram("ident_in", [128, 128], F32, kind="ExternalInput")
    out = p.dram("proj", [ntok, ncols], F32, kind="ExternalOutput")

    ident = p.sb([128, 128], F32, "ident")
    p.dma(ident[:], ident_d[:])
    identb = p.sb([128, 128], BF16, "identb")
    p.copy(identb[:], ident[:])
    ones_f = p.sb([128, 128], F32, "ones_f")
    p.memset(ones_f[:], 1.0)
    c_bc = emit_cbc(p, c_in, ones_f)
    shift = p.sb([128, D], F32, "shift")
    gmod = p.sb([128, D], F32, "gmod")
    emit_mod(p, c_bc, wada, bada, 0, D, shift, ones_f)
    emit_mod(p, c_bc, wada, bada, D, D, gmod, ones_f, plus_one=True)
    ngb = p.sb([128, D], F32, "ngb")
    p.dma(ngb[:], bcast_row(ng, D))
    p.tt(gmod[:], gmod[:], ngb[:], ALU.mult)

    HALF = (ncols + 1) // 2
    Wb = p.sb([128, 8, HALF], BF16, "Wb")
    PIECE = 2052
    cv = 0
    for half in range(2):
        h0 = half * HALF
        hn = min(HALF, ncols - h0)
        npiece = (hn + PIECE - 1) // PIECE
        for k in range(8):
            for pc in range(npiece):
                c0 = pc * PIECE
                n = min(PIECE, hn - c0)
                st = p.rot("wstage", [128, PIECE], F32, 2)
                p.dma(st[:, 0:n], w_in[k * 128:(k + 1) * 128, h0 + c0:h0 + c0 + n], q=("sp" if cv % 2 == 0 else "pool"))
                eng = ("dve", "act", "pool")[cv % 3]
                p.copy(Wb[:, k, c0:c0 + n], st[:, 0:n], e=eng)
                cv += 1
        for t in range(ntiles):
            xt = p.rot("xt", [128, D], F32, 2)
            p.dma(xt[:], x[t * 128:(t + 1) * 128, :])
            hb = p.rot("hb", [128, D], BF16, 2)
            emit_rmsnorm_mod(p, xt[:], gmod[:], shift[:], hb[:])
            psT = p.rot("psT", [128, 8, 128], BF16, 1, psum=True)
            for k in range(8):
                p.tr(psT[:, k, :], hb[:, k * 128:(k + 1) * 128], identb[:])
            hT = p.rot("hT", [128, 8, 128], BF16, 2)
            p.copy(hT[:], psT[:], e="act")
            ot = p.rot("ot", [128, HALF], F32, 2)
            nb = (hn + 511) // 512
            for b in range(nb):
                n = min(512, hn - b * 512)
                ps = p.rot("ps_mm", [128, 512], F32, 4, psum=True)
                for k in range(8):
                    p.mm(ps[:, 0:n], hT[:, k, :], Wb[:, k, b * 512: b * 512 + n], start=(k == 0), stop=(k == 7))
                p.copy(ot[:, b * 512:b * 512 + n], ps[:, 0:n], e=("dve" if b % 2 == 0 else "act"))
            p.dma(out[t * 128:(t + 1) * 128, h0:h0 + hn], ot[:, 0:hn], q=("sp" if t % 2 == 0 else "pool"))
    if DEBUG:
        dbg = p.dram("dbg", [128, 4 * D], F32, kind="ExternalOutput")
        p.dma(dbg[:, 0:D], gmod[:])
        p.dma(dbg[:, D:2 * D], shift[:])
        hf = p.sb([128, D], F32, "hf")
        p.copy(hf[:], hb[:])
        p.dma(dbg[:, 2 * D:3 * D], hf[:])
        hf2 = p.sb([128, D], F32, "hf2")
        p.copy(hf2[:], hT[:])
        p.dma(dbg[:, 3 * D:4 * D], hf2[:])
        p.finish([out, dbg])
        return nc, p
    p.finish([out])
    return nc, p


def emit_qk_norm(p, src, nblk, gain_bc, identb, dstT, scale, neg_dstT=None):
    for b0 in range(0, nblk, 4):
        t = p.rot("qk_in", [128, 4, 128], F32, 2)
        p.dma(t[:], src.v(src.h[b0 * 128:(b0 + 4) * 128, :].rearrange("(b p) d -> p b d", p=128)), q=("sp" if (b0 // 4) % 2 == 0 else "pool"))
        sq = p.rot("qk_sq", [128, 4, 128], F32, 1)
        p.act(sq[:], t[:], AF.Square)
        ssq = p.rot("qk_ssq", [128, 4], F32, 2)
        p.reduce(ssq[:], sq[:])
        rstd = p.rot("qk_rstd", [128, 4], F32, 2)
        p.act(rstd[:], ssq[:], AF.Sqrt, bias=EPS, scale=1.0 / 128)
        p.recip(rstd[:], rstd[:])
        nb = p.rot("qk_nb", [128, 4, 128], BF16, 2)
        for i in range(4):
            p.stt(nb[:, i, :], t[:, i, :], rstd[:, i:i + 1], gain_bc[:], ALU.mult, ALU.mult)
        pt = p.rot("qk_pt", [128, 512], BF16, 2, psum=True)
        for i in range(4):
            p.tr(pt[:, i * 128:(i + 1) * 128], nb[:, i, :], identb[:])
        p.ts(dstT[:, b0 * 128:(b0 + 4) * 128], pt[:], scale, ALU.mult, e="pool" if False else "dve")
        if neg_dstT is not None:
            p.ts(neg_dstT[:, b0 * 128:(b0 + 4) * 128], pt[:], -scale, ALU.mult)


def build_sb(nblk=128):
    nc = bass.Bass("TRN2", target_bir_lowering=False)
    p = P(nc)
    S = nblk * 128
    ngrp = nblk // 8
    q_d = p.dram("q", [ngrp * 512, 128], F32, kind="ExternalInput")
    k_d = p.dram("k", [S, 128], F32, kind="ExternalInput")
    v_d = p.dram("v", [S, 128], F32, kind="ExternalInput")
    qg_d = p.dram("qg", [1, 128], F32, kind="ExternalInput")
    kg_d = p.dram("kg", [1, 128], F32, kind="ExternalInput")
    mask_d = p.dram("mask", [128, 8, 512], F32, kind="ExternalInput")
    mincl_d = p.dram("mincl", [128, 128], F32, kind="ExternalInput")
    ident_d = p.dram("ident_in", [128, 128], F32, kind="ExternalInput")
    out = p.dram("oT", [128, ngrp * 512], F32, kind="ExternalOutput")

    ident = p.sb([128, 128], F32, "ident")
    p.dma(ident[:], ident_d[:])
    identb = p.sb([128, 128], BF16, "identb")
    p.copy(identb[:], ident[:])
    mincl_f = p.sb([128, 128], F32, "mincl_f")
    p.dma(mincl_f[:], mincl_d[:])
    mincl = p.sb([128, 128], BF16, "mincl")
    p.copy(mincl[:], mincl_f[:])
    onesb = p.sb([128, 128], BF16, "onesb")
    p.memset(onesb[:], 1.0)
    maskb = p.sb([128, 8, 512], BF16, "maskb")
    for o in range(8):
        mt = p.rot("mask_st", [128, 512], F32, 2)
        p.dma(mt[:], mask_d[:, o, :])
        p.copy(maskb[:, o, :], mt[:])
    qg = p.sb([128, 128], F32, "qg")
    p.dma(qg[:], bcast_row(qg_d, 128))
    kg = p.sb([128, 128], F32, "kg")
    p.dma(kg[:], bcast_row(kg_d, 128))

    knT = p.sb([128, S], BF16, "knT")
    nknT = p.sb([128, S], BF16, "nknT")
    qnT = p.sb([128, ngrp * 512], BF16, "qnT")
    vb = p.sb([128, nblk, 128], BF16, "vb")
    emit_qk_norm(p, k_d, nblk, kg, identb, knT, 1.0, nknT)
    emit_qk_norm(p, q_d, ngrp * 4, qg, identb, qnT, 128 ** -0.5)
    for b0 in range(0, nblk, 4):
        t = p.rot("qk_in", [128, 4, 128], F32, 2)
        p.dma(t[:], v_d.v(v_d.h[b0 * 128:(b0 + 4) * 128, :].rearrange("(b p) d -> p b d", p=128)))
        p.copy(vb[:, b0:b0 + 4, :], t[:], e="pool")

    import os
    STOP = int(os.environ.get("SB_STOP", "0"))
    if STOP == 1:
        for m in range(ngrp):
            ot = p.rot("sbOT", [128, 512], F32, 2)
            p.copy(ot[:], qnT[:, m * 512:(m + 1) * 512])
            p.dma(out[:, m * 512:(m + 1) * 512], ot[:])
        p.finish([out])
        return nc, p
    for m in range(ngrp):
        O = p.rot("sbO", [128, 512], F32, 1, psum=True)
        CS = p.rot("sbCS", [128, 512], F32, 1, psum=True)
        qs = qnT[:, m * 512:(m + 1) * 512]
        kbs = list(range(8 * m + 7, -1, -1))
        nit = len(kbs)
        st = {}

        def phaseA(it):
            kb = kbs[it]
            off = kb - 8 * m
            Z = p.rot("sbZ", [128, 512], F32, 2, psum=True)
            U = p.rot("sbU", [128, 512], F32, 2, psum=True)
            p.mm(Z[:], knT[:, kb * 128:(kb + 1) * 128], qs)
            p.mm(U[:], nknT[:, kb * 128:(kb + 1) * 128], qs, start=True, stop=False)
            e = p.rot("sbE", [128, 512], F32, 2)
            p.act(e[:], Z[:], AF.Exp)
            spb = p.rot("sbSP", [128, 512], BF16, 3)
            p.act(spb[:], e[:], AF.Ln, bias=1.0)
            if off >= 0:
                p.tt(spb[:], spb[:], maskb[:, off, :], ALU.mult)
            st[it] = (U, spb)

        def phaseB(it):
            kb = kbs[it]
            off = kb - 8 * m
            first = it == 0
            last = it == nit - 1
            U, spb = st.pop(it)
            p.mm(U[:], mincl[:], spb[:], start=False, stop=first)
            if not first:
                p.mm(U[:], identb[:], st["accb"][:], start=False, stop=True)
            p.mm(CS[:], onesb[:], spb[:], start=first, stop=last)
            att = p.rot("sbATT", [128, 512], BF16, 2)
            p.act(att[:], U[:], AF.Exp, scale=-1.0)
            if off >= 0:
                p.tt(att[:], att[:], maskb[:, off, :], ALU.mult)
            if not last:
                accb = p.rot("sbACC", [128, 512], BF16, 2)
                p.copy(accb[:], CS[:])
                st["accb"] = accb
            p.mm(O[:], vb[:, kb, :], att[:], start=first, stop=last)

        phaseA(0)
        for it in range(nit):
            if it + 1 < nit:
                phaseA(it + 1)
            phaseB(it)
        ot = p.rot("sbOT", [128, 512], F32, 2)
        p.copy(ot[:], O[:])
        p.dma(out[:, m * 512:(m + 1) * 512], ot[:])
    p.finish([out])
    return nc, p


def emit_conv_silu(p, src_d, nch, S, w_d, b_d, dst, dst_dt, name, CH=2048, bias=True):
    w = p.sb([nch, 4], F32, name + "_w")
    p.dma(w[:], w_d[:])
    if bias:
        b = p.sb([nch, 1], F32, name + "_b")
        p.dma(b[:], b_d[:])
    for c0 in range(0, S, CH):
        t = p.rot("cv_in", [128, CH + 3], F32, 2)
        p.dma(t[0:nch, :], src_d[:, c0:c0 + CH + 3])
        acc = p.rot("cv_acc", [128, CH], F32, 2)
        p.ts(acc[0:nch, :], t[0:nch, 0:CH], w[:, 0:1], ALU.mult)
        for i in range(1, 4):
            p.stt(acc[0:nch, :], t[0:nch, i:i + CH], w[:, i:i + 1], acc[0:nch, :], ALU.mult, ALU.add)
        if bias:
            p.act(dst[0:nch, c0:c0 + CH], acc[0:nch, :], AF.Silu, bias=b[:, 0:1])
        else:
            p.act(dst[0:nch, c0:c0 + CH], acc[0:nch, :], AF.Silu)


def build_ssd(nchunk=128):
    nc = bass.Bass("TRN2", target_bir_lowering=False)
    p = P(nc)
    S = nchunk * 128
    CH = min(2048, S)
    xT_d = p.dram("xT", [64, S + 3], F32, kind="ExternalInput")
    BT_d = p.dram("BT", [128, S + 3], F32, kind="ExternalInput")
    CT_d = p.dram("CT", [128, S + 3], F32, kind="ExternalInput")
    wx_d = p.dram("wx", [64, 4], F32, kind="ExternalInput")
    bx_d = p.dram("bx", [64, 1], F32, kind="ExternalInput")
    wB_d = p.dram("wB", [128, 4], F32, kind="ExternalInput")
    bB_d = p.dram("bB", [128, 1], F32, kind="ExternalInput")
    wC_d = p.dram("wC", [128, 4], F32, kind="ExternalInput")
    bC_d = p.dram("bC", [128, 1], F32, kind="ExternalInput")
    dt_d = p.dram("dt_col", [128, nchunk], F32, kind="ExternalInput")
    sc_d = p.dram("scal", [128, 3], F32, kind="ExternalInput")
    triu_d = p.dram("triu", [128, 128], F32, kind="ExternalInput")
    mneg_d = p.dram("mneg", [128, 128], F32, kind="ExternalInput")
    ident_d = p.dram("ident_in", [128, 128], F32, kind="ExternalInput")
    out = p.dram("y", [S, 64], F32, kind="ExternalOutput")

    ident = p.sb([128, 128], F32, "ident")
    p.dma(ident[:], ident_d[:])
    identb = p.sb([128, 128], BF16, "identb")
    p.copy(identb[:], ident[:])
    triu = p.sb([128, 128], F32, "triu")
    p.dma(triu[:], triu_d[:])
    mneg = p.sb([128, 128], F32, "mneg")
    p.dma(mneg[:], mneg_d[:])
    ones_f = p.sb([128, 128], F32, "ones_f")
    p.memset(ones_f[:], 1.0)
    scal = p.sb([128, 3], F32, "scal")
    p.dma(scal[:], sc_d[:])
    dtc = p.sb([128, nchunk], F32, "dtc")
    p.dma(dtc[:], dt_d[:])
    p.act(dtc[:], dtc[:], AF.Exp, bias=scal[:, 0:1])
    p.act(dtc[:], dtc[:], AF.Ln, bias=1.0)
    aneg = p.sb([128, 1], F32, "aneg")
    p.act(aneg[:], scal[:, 1:2], AF.Exp)
    p.ts(aneg[:], aneg[:], -1.0, ALU.mult)
    adt = p.sb([128, nchunk], F32, "adt")
    p.ts(adt[:], dtc[:], aneg[:, 0:1], ALU.mult)

    xT = p.sb([64, S], F32, "xTs")
    BT = p.sb([128, S], BF16, "BTs")
    CT = p.sb([128, S], BF16, "CTs")
    emit_conv_silu(p, xT_d, 64, S, wx_d, bx_d, xT, F32, "cx", CH)
    emit_conv_silu(p, BT_d, 128, S, wB_d, bB_d, BT, BF16, "cB", CH)
    emit_conv_silu(p, CT_d, 128, S, wC_d, bC_d, CT, BF16, "cC", CH)

    state_f = p.sb([128, 64], F32, "state_f")
    state_b = p.sb([128, 64], BF16, "state_b")
    p.memset(state_f[:], 0.0)
    p.memset(state_b[:], 0.0)
    OB = 8
    for c in range(nchunk):
        sl = slice(c * 128, (c + 1) * 128)
        px = p.rot("ssd_px", [128, 64], F32, 1, psum=True)
        p.tr(px[:], xT[0:64, sl], ident[0:64, 0:64])
        xtok = p.rot("ssd_xtok", [128, 64], F32, 2)
        p.copy(xtok[:], px[:])
        pB = p.rot("ssd_pB", [128, 128], BF16, 1, psum=True)
        p.tr(pB[:], BT[:, sl], identb[:])
        Btok = p.rot("ssd_Btok", [128, 128], BF16, 2)
        p.copy(Btok[:], pB[:], e="act")
        adt_c = adt[:, c:c + 1]
        pcol = p.rot("ssd_pcol", [128, 1], F32, 1, psum=True)
        p.mm(pcol[:], triu[:], adt_c)
        acol = p.rot("ssd_acol", [128, 1], F32, 2)
        p.copy(acol[:], pcol[:])
        adt_bc = p.rot("ssd_adtbc", [128, 128], F32, 2)
        p.ts(adt_bc[:], ones_f[:], adt_c, ALU.mult)
        prow = p.rot("ssd_prow", [128, 128], F32, 1, psum=True)
        p.mm(prow[:], adt_bc[:], triu[:])
        dsg = p.rot("ssd_dsg", [128, 128], F32, 2)
        p.stt(dsg[:], prow[:], acol[:, 0:1], mneg[:], ALU.subtract, ALU.add)
        segT = p.rot("ssd_segT", [128, 128], F32, 2)
        p.act(segT[:], dsg[:], AF.Exp)
        ea = p.rot("ssd_ea", [128, 128], F32, 2)
        p.act(ea[:], prow[:], AF.Exp)
        alast = p.rot("ssd_alast", [128, 1], F32, 2)
        p.copy(alast[:], prow[:, 127:128])
        dte = p.rot("ssd_dte", [128, 1], F32, 2)
        p.act(dte[:], acol[:], AF.Exp, bias=alast[:, 0:1], scale=-1.0)
        cdec = p.rot("ssd_cdec", [128, 1], F32, 2)
        p.act(cdec[:], alast[:], AF.Exp)
        pcb = p.rot("ssd_pcb", [128, 128], F32, 1, psum=True)
        p.mm(pcb[:], BT[:, sl], CT[:, sl])
        scT = p.rot("ssd_scT", [128, 128], BF16, 2)
        p.tt(scT[:], pcb[:], segT[:], ALU.mult)
        xdt = p.rot("ssd_xdt", [128, 64], BF16, 2)
        p.ts(xdt[:], xtok[:], dtc[:, c:c + 1], ALU.mult)
        cdT = p.rot("ssd_cdT", [128, 128], BF16, 2)
        p.tt(cdT[:], CT[:, sl], ea[:], ALU.mult)
        py = p.rot("ssd_py", [128, 64], F32, 2, psum=True)
        p.mm(py[:], scT[:], xdt[:], start=True, stop=False)
        p.mm(py[:], cdT[:], state_b[:], start=False, stop=True)
        if c % OB == 0:
            yo = p.rot("ssd_yo", [128, OB, 64], F32, 2)
        p.stt(yo[:, c % OB, :], xtok[:], scal[:, 2:3], py[:], ALU.mult, ALU.add)
        if c % OB == OB - 1:
            c0 = (c - OB + 1) * 128
            p.dma(out.v(out.h[c0:c0 + OB * 128, :].rearrange("(b p) d -> p b d", p=128)), yo[:])
        sc2 = p.rot("ssd_sc2", [128, 1], F32, 2)
        p.tt(sc2[:], dtc[:, c:c + 1], dte[:], ALU.mult)
        xdd = p.rot("ssd_xdd", [128, 64], BF16, 2)
        p.ts(xdd[:], xtok[:], sc2[:, 0:1], ALU.mult)
        pS = p.rot("ssd_pS", [128, 64], F32, 1, psum=True)
        p.mm(pS[:], Btok[:], xdd[:])
        p.stt(state_f[:], state_f[:], cdec[:, 0:1], pS[:], ALU.mult, ALU.add)
        p.copy(state_b[:], state_f[:])
    p.finish([out])
    return nc, p


def build_gdn(nchunk=256, NB=4):
    import os
    nc = bass.Bass("TRN2", target_bir_lowering=False)
    p = P(nc)
    S = nchunk * 64
    CH = min(1024, S)
    CPS = CH // 64
    qT_d = p.dram("qT", [128, S + 3], F32, kind="ExternalInput")
    kT_d = p.dram("kT", [128, S + 3], F32, kind="ExternalInput")
    vT_d = p.dram("vT", [64, S + 3], F32, kind="ExternalInput")
    wq_d = p.dram("wq", [128, 4], F32, kind="ExternalInput")
    wk_d = p.dram("wk", [128, 4], F32, kind="ExternalInput")
    wv_d = p.dram("wv", [64, 4], F32, kind="ExternalInput")
    a_d = p.dram("a_col", [64, nchunk], F32, kind="ExternalInput")
    b_d = p.dram("b_col", [64, nchunk], F32, kind="ExternalInput")
    sc_d = p.dram("scal", [128, 2], F32, kind="ExternalInput")
    triu_d = p.dram("triu", [64, 64], F32, kind="ExternalInput")
    mpos_d = p.dram("mpos", [64, 64], F32, kind="ExternalInput")
    mneg_d = p.dram("mneg", [64, 64], F32, kind="ExternalInput")
    st01_d = p.dram("st01", [64, 64], F32, kind="ExternalInput")
    ident_d = p.dram("ident_in", [128, 128], F32, kind="ExternalInput")
    out = p.dram("o", [S, 64], F32, kind="ExternalOutput")

    def ld(d, shape, name):
        t = p.sb(shape, F32, name)
        p.dma(t[:], d[:])
        return t
    ident = ld(ident_d, [128, 128], "ident")
    triu = ld(triu_d, [64, 64], "triu")
    mpos = ld(mpos_d, [64, 64], "mpos")
    mneg = ld(mneg_d, [64, 64], "mneg")
    st01 = ld(st01_d, [64, 64], "st01")
    scal = ld(sc_d, [128, 2], "scal")
    ones_f = p.sb([64, 128], F32, "ones_f")
    p.memset(ones_f[:], 1.0)
    beta = ld(b_d, [64, nchunk], "beta")
    p.act(beta[:], beta[:], AF.Sigmoid)
    gall = ld(a_d, [64, nchunk], "gall")
    p.act(gall[:], gall[:], AF.Exp, bias=scal[0:64, 1:2])
    p.act(gall[:], gall[:], AF.Ln, bias=1.0)
    aneg = p.sb([64, 1], F32, "aneg")
    p.act(aneg[:], scal[0:64, 0:1], AF.Exp)
    p.ts(aneg[:], aneg[:], -1.0, ALU.mult)
    p.ts(gall[:], gall[:], aneg[:, 0:1], ALU.mult)

    wq = ld(wq_d, [128, 4], "wq")
    wk = ld(wk_d, [128, 4], "wk")
    wv = ld(wv_d, [64, 4], "wv")
    state = p.sb([128, 64], F32, "state")
    p.memset(state[:], 0.0)
    OB = 8
    RSQ = 128 ** -0.5
    D1 = NB + 1
    D2 = 2 * NB + 1

    def conv(src_d, nch, w, c0, key):
        t = p.rot(key + "_in", [128, CH + 3], F32, 1)
        p.dma(t[0:nch, :], src_d[:, c0:c0 + CH + 3])
        acc = p.rot(key + "_acc", [128, CH], F32, 1)
        p.ts(acc[0:nch, :], t[0:nch, 0:CH], w[:, 0:1], ALU.mult)
        for i in range(1, 4):
            p.stt(acc[0:nch, :], t[0:nch, i:i + CH], w[:, i:i + 1], acc[0:nch, :], ALU.mult, ALU.add)
        o = p.rot(key + "_o", [128, CH], F32, 2)
        p.act(o[0:nch, :], acc[0:nch, :], AF.Silu)
        return o

    oo_box = [None]

    def pre(c, ci, qTs, kTs, vTs, ctx):
        sl = slice(ci * 64, (ci + 1) * 64)
        PB = p.rot("g_PB", [128, 512], F32, 2 * NB, psum=True)
        ctx["PB"] = PB
        ptq, ptk, ptv = PB[0:64, 0:128], PB[0:64, 128:256], PB[0:64, 256:320]
        p.tr(ptq, qTs[:, sl], ident[:])
        p.tr(ptk, kTs[:, sl], ident[:])
        p.tr(ptv, vTs[0:64, sl], ident[0:64, 0:64])
        g_c = gall[:, c:c + 1]
        b_c = beta[:, c:c + 1]
        pcol = PB[0:64, 320:321]
        p.mm(pcol, triu[:], g_c)
        g_bc = p.rot("g_gbc", [64, 128], F32, D1)
        p.ts(g_bc[:], ones_f[:], g_c, ALU.mult)
        yield
        junk = p.rot("g_junk", [64, 128], F32, 2)
        ssq = p.rot("g_ssq", [64, 2], F32, D1)
        p.act(junk[:], ptq, AF.Square, accum=ssq[:, 0:1])
        p.act(junk[:], ptk, AF.Square, accum=ssq[:, 1:2])
        if os.environ.get('GDN_SUB') == 'a':
            yield
            return
        gcol = p.rot("g_gcol", [64, 1], F32, D1)
        p.copy(gcol[:], pcol, e="act")
        if os.environ.get('GDN_SUB') == 'b':
            yield
            return
        prow, prow64 = PB[:, 384:448], PB[0:64, 384:448]
        p.mm(prow, g_bc[:], triu[:])
        yield
        rinv = p.rot("g_rinv", [64, 2], F32, D1)
        p.act(rinv[:], ssq[:], AF.Sqrt, bias=EPS)
        nd = p.rot("g_nd", [64, 64], F32, D1)
        p.stt(nd[:], prow64, gcol[:, 0:1], mpos[:], ALU.subtract, ALU.add)
        d2 = p.rot("g_d2", [64, 64], F32, D1)
        p.stt(d2[:], prow64, gcol[:, 0:1], mneg[:], ALU.subtract, ALU.add)
        glast = p.rot("g_glast", [128, 1], F32, D1)
        p.copy(glast[:], PB[:, 447:448])
        yield
        p.recip(rinv[:], rinv[:])
        dec_s = p.rot("g_decs", [64, 64], F32, D1)
        p.act(dec_s[:], nd[:], AF.Exp, scale=-1.0)
        decT = p.rot("g_decT", [64, 64], F32, D1)
        p.act(decT[:], d2[:], AF.Exp)
        egc = p.rot("g_egc", [64, 1], F32, D1)
        p.act(egc[:], gcol[:], AF.Exp)
        dlast = p.rot("g_dlast", [64, 1], F32, D1)
        p.act(dlast[:], gcol[:], AF.Exp, bias=glast[0:64, 0:1], scale=-1.0)
        cdec = p.rot("g_cdec", [128, 1], F32, D2)
        p.act(cdec[:], glast[:], AF.Exp)
        ctx["cdec"] = cdec
        yield
        decTs = p.rot("g_decTs", [64, 64], F32, D1)
        p.tt(decTs[:], decT[:], st01[:], ALU.mult)
        k_n = p.rot("g_kn", [64, 128], F32, D1)
        p.ts(k_n[:], ptk, rinv[:, 1:2], ALU.mult)
        q_n = p.rot("g_qn", [64, 128], F32, D1)
        p.ts(q_n[:], ptq, rinv[:, 0:1], ALU.mult, RSQ, ALU.mult)
        R = p.rot("g_R0", [64, 192], F32, D1)
        p.ts(R[:, 0:64], ptv, b_c, ALU.mult)
        yield
        kb = p.rot("g_kb", [64, 128], F32, D1)
        p.ts(kb[:], k_n[:], b_c, ALU.mult)
        qd = p.rot("g_qd", [64, 128], F32, D1)
        p.ts(qd[:], q_n[:], egc[:, 0:1], ALU.mult)
        kd = p.rot("g_kd", [64, 128], F32, D2)
        p.ts(kd[:], k_n[:], dlast[:, 0:1], ALU.mult)
        ctx["kd"] = kd
        yield
        p.ts(R[:, 64:192], kb[:], egc[:, 0:1], ALU.mult)
        p.tr(PB[:, 0:64], k_n[:], ident[0:64, 0:64])
        p.tr(PB[:, 64:128], kb[:], ident[0:64, 0:64])
        p.tr(PB[:, 128:192], q_n[:], ident[0:64, 0:64])
        p.tr(PB[:, 192:256], qd[:], ident[0:64, 0:64])
        yield
        fT = p.rot("g_fT", [128, 256], F32, D2)
        p.copy(fT[:], PB[:, 0:256])
        knT, kbT, qnT, qdT = fT[:, 0:64], fT[:, 64:128], fT[:, 128:192], fT[:, 192:256]
        ctx["qdT"] = qdT
        yield
        pG0, pG1, pG2 = PB[0:64, 256:320], PB[0:64, 320:384], PB[0:64, 384:448]
        p.mm(pG0, kbT, knT)
        p.mm(pG1, knT, kbT)
        p.mm(pG2, knT, qnT)
        yield
        A = p.rot("g_A", [64, 64], F32, 2 * NB)
        p.tt(A[:], pG0, dec_s[:], ALU.mult)
        At = p.rot("g_At", [64, 64], F32, 2 * NB)
        p.tt(At[:], pG1, decTs[:], ALU.mult)
        attnT = p.rot("g_attnT", [64, 64], F32, D2)
        p.tt(attnT[:], pG2, decT[:], ALU.mult)
        ctx["attnT"] = attnT
        yield
        for lvl in range(6):
            pR = PB[0:64, 0:192]
            p.mm(pR, At[:], R[:])
            if lvl < 5:
                pP0, pP1 = PB[0:64, 192:256], PB[0:64, 256:320]
                p.mm(pP0, At[:], A[:])
                p.mm(pP1, A[:], At[:])
            yield
            Rn = p.rot("g_Rf", [64, 192], F32, D2) if lvl == 5 else p.rot("g_Ri", [64, 192], F32, 2 * NB)
            if lvl == 0:
                p.tt(Rn[:], R[:], pR, ALU.subtract)
            else:
                p.tt(Rn[:], R[:], pR, ALU.add)
            R = Rn
            if lvl < 5:
                A2 = p.rot("g_A", [64, 64], F32, 2 * NB)
                At2 = p.rot("g_At", [64, 64], F32, 2 * NB)
                p.copy(A2[:], pP0)
                p.copy(At2[:], pP1)
                A, At = A2, At2
            yield
        ctx["R"] = R
        pW = PB[:, 320:384]
        p.tr(pW, R[:, 64:192], ident[0:64, 0:64])
        yield
        wT = p.rot("g_wT", [128, 64], F32, D2)
        p.copy(wT[:], pW)
        ctx["wT"] = wT
        yield

    def scan(c, ctx):
        PB = ctx["PB"]
        R = ctx["R"]
        pv = PB[0:64, 384:448]
        p.mm(pv, ctx["wT"][:], state[:])
        vnew = p.rot("g_vnew", [64, 64], F32, 3)
        p.tt(vnew[:], R[:, 0:64], pv, ALU.subtract)
        po = PB[0:64, 448:512]
        p.mm(po, ctx["qdT"], state[:], start=True, stop=False)
        p.mm(po, ctx["attnT"][:], vnew[:], start=False, stop=True)
        pS = PB[:, 192:256]
        p.mm(pS, ctx["kd"][:], vnew[:])
        p.stt(state[:], state[:], ctx["cdec"][:, 0:1], pS, ALU.mult, ALU.add)
        if c % OB == 0:
            oo_box[0] = p.rot("g_oo", [64, OB, 64], F32, 2)
        oo = oo_box[0]
        p.copy(oo[:, c % OB, :], po)
        if c % OB == OB - 1:
            c0 = (c - OB + 1) * 64
            p.dma(out.v(out.h[c0:c0 + OB * 64, :].rearrange("(b p) d -> p b d", p=64)), oo[:])

    import os
    MAXST = int(os.environ.get('GDN_MAXST', '999'))
    pending = []
    for sc in range(S // CH):
        qTs = conv(qT_d, 128, wq, sc * CH, "gq")
        kTs = conv(kT_d, 128, wk, sc * CH, "gk")
        vTs = conv(vT_d, 64, wv, sc * CH, "gv")
        for b0 in range(0, CPS, NB):
            ctxs = [dict() for _ in range(NB)]
            gens = [pre(sc * CPS + b0 + i, b0 + i, qTs, kTs, vTs, ctxs[i]) for i in range(NB)]
            stage = 0
            alive = True
            while alive:
                alive = False
                for g in gens:
                    try:
                        next(g)
                        alive = True
                    except StopIteration:
                        pass
                stage += 1
                if stage >= MAXST:
                    break
                if pending and stage % 4 == 0:
                    cc, cx = pending.pop(0)
                    scan(cc, cx)
            while pending:
                cc, cx = pending.pop(0)
                scan(cc, cx)
            if MAXST < 99:
                continue
            pending = [(sc * CPS + b0 + i, ctxs[i]) for i in range(NB)]
    while pending:
        cc, cx = pending.pop(0)
        scan(cc, cx)
    p.finish([out])
    return nc, p


def load_w_bf16(p, w_d, rows, cols, dst, r0=0):
    cv = 0
    for k in range(rows // 128):
        for c0 in range(0, cols, 2048):
            n = min(2048, cols - c0)
            st = p.rot("wstage", [128, 2048], F32, 2)
            p.dma(st[:, 0:n], w_d[r0 + k * 128:r0 + (k + 1) * 128, c0:c0 + n], q=("sp" if cv % 2 == 0 else "pool"))
            p.copy(dst[:, k, c0:c0 + n], st[:, 0:n], e=("dve" if cv % 2 == 0 else "pool"))
            cv += 1


def build_k3a(ntiles=NT):
    nc = bass.Bass("TRN2", target_bir_lowering=False)
    p = P(nc)
    ntok = ntiles * 128
    di = lambda n, s: p.dram(n, s, F32, kind="ExternalInput")
    x = di("x", [ntok, D]); z_d = di("z", [ntok, 512]); gg_d = di("ggate", [ntok, 512]); brg_d = di("brg", [ntok, 3072])
    ya_d = di("ya", [ntok, 512]); yb_d = di("yb", [ntok, 512]); yc_d = di("yc", [ntok, 512])
    c_in = di("c_col", [128, 8]); wada = di("wada", [D, 4 * D]); bada = di("bada", [1, 4 * D])
    ssmg_d = di("ssm_g", [1, 512]); gdng_d = di("gdn_g", [1, 128]); ffng_d = di("ffn_g", [1, D])
    wbr_d = di("w_branch", [1536, D]); wout_d = di("w_out", [D, D]); wgr_d = di("wgr", [D, 36]); bgr_d = di("bgr", [1, 36])
    ident_d = di("ident_in", [128, 128])
    x1_o = p.dram("x1", [ntok, D], F32, kind="ExternalOutput")
    h2_o = p.dram("h2", [ntok, D], F32, kind="ExternalOutput")
    wt_o = p.dram("wt", [ntok, 32], F32, kind="ExternalOutput")
    gf_o = p.dram("gf", [128, D], F32, kind="ExternalOutput")

    ident = p.sb([128, 128], F32, "ident")
    p.dma(ident[:], ident_d[:])
    identb = p.sb([128, 128], BF16, "identb")
    p.copy(identb[:], ident[:])
    ones_f = p.sb([128, 128], F32, "ones_f")
    p.memset(ones_f[:], 1.0)
    c_bc = emit_cbc(p, c_in, ones_f)
    gate_m = p.sb([128, D], F32, "gate_m"); shift_f = p.sb([128, D], F32, "shift_f")
    gmod_f = p.sb([128, D], F32, "gmod_f"); gate_f = p.sb([128, D], F32, "gate_f")
    emit_mod(p, c_bc, wada, bada, 0, D, gate_m, ones_f)
    emit_mod(p, c_bc, wada, bada, D, D, shift_f, ones_f)
    emit_mod(p, c_bc, wada, bada, 2 * D, D, gmod_f, ones_f, plus_one=True)
    emit_mod(p, c_bc, wada, bada, 3 * D, D, gate_f, ones_f)
    p.dma(gf_o[:], gate_f[:])
    ffng = p.sb([128, D], F32, "ffng")
    p.dma(ffng[:], bcast_row(ffng_d, D))
    p.tt(gmod_f[:], gmod_f[:], ffng[:], ALU.mult)
    ssmg = p.sb([128, 512], F32, "ssmg")
    p.dma(ssmg[:], bcast_row(ssmg_d, 512))
    gdng = p.sb([128, 128], F32, "gdng")
    p.dma(gdng[:], bcast_row(gdng_d, 128))
    wgr = p.sb([128, 8, 36], F32, "wgr")
    p.dma(wgr[:], wgr_d.v(wgr_d.h[:, :].rearrange("(k p) n -> p k n", p=128)))
    bgr = p.sb([1, 36], F32, "bgr")
    p.dma(bgr[:], bgr_d[:])
    Wbr = p.sb([128, 12, D], BF16, "Wbr")
    load_w_bf16(p, wbr_d, 1536, D, Wbr)
    Wout = p.sb([128, 8, D], BF16, "Wout")
    load_w_bf16(p, wout_d, D, D, Wout)

    for t in range(ntiles):
        rs = slice(t * 128, (t + 1) * 128)
        xt = p.rot("xt", [128, D], F32, 2)
        p.dma(xt[:], x[rs, :])
        zt = p.rot("zt", [128, 512], F32, 2); p.dma(zt[:], z_d[rs, :], q="pool")
        gt = p.rot("gt", [128, 512], F32, 2); p.dma(gt[:], gg_d[rs, :])
        bt = p.rot("bt", [128, 3072], F32, 1); p.dma(bt[:], brg_d[rs, :], q="pool")
        ya = p.rot("ya", [128, 512], F32, 2); p.dma(ya[:], ya_d[rs, :])
        yb = p.rot("yb", [128, 512], F32, 2); p.dma(yb[:], yb_d[rs, :], q="pool")
        yc = p.rot("yc", [128, 512], F32, 2); p.dma(yc[:], yc_d[rs, :])
        brn = p.rot("brn", [128, 1536], BF16, 2)
        p.act(zt[:], zt[:], AF.Silu)
        p.tt(ya[:], ya[:], zt[:], ALU.mult)
        junk = p.rot("junk", [128, 512], F32, 1)
        ssq = p.rot("ssq", [128, 8], F32, 2)
        for g in range(2):
            p.act(junk[:, 0:256], ya[:, g * 256:(g + 1) * 256], AF.Square, accum=ssq[:, g:g + 1])
        for hd in range(4):
            p.act(junk[:, 0:128], yc[:, hd * 128:(hd + 1) * 128], AF.Square, accum=ssq[:, 2 + hd:3 + hd])
        rstd = p.rot("rstd", [128, 8], F32, 2)
        p.act(rstd[:, 0:2], ssq[:, 0:2], AF.Sqrt, bias=EPS, scale=1.0 / 256)
        p.act(rstd[:, 2:6], ssq[:, 2:6], AF.Sqrt, bias=EPS, scale=1.0 / 128)
        p.recip(rstd[:, 0:6], rstd[:, 0:6])
        for g in range(2):
            sl = slice(g * 256, (g + 1) * 256)
            p.stt(brn[:, sl], ya[:, sl], rstd[:, g:g + 1], ssmg[:, sl], ALU.mult, ALU.mult)
        p.copy(brn[:, 512:1024], yb[:], e="pool")
        p.act(gt[:], gt[:], AF.Silu)
        for hd in range(4):
            sl = slice(hd * 128, (hd + 1) * 128)
            p.stt(yc[:, sl], yc[:, sl], rstd[:, 2 + hd:3 + hd], gdng[:], ALU.mult, ALU.mult)
        p.tt(brn[:, 1024:1536], yc[:], gt[:], ALU.mult)
        brT = p.rot("brT", [128, 12, 128], BF16, 2)
        for q4 in range(3):
            pt = p.rot("psT", [128, 4, 128], BF16, 2, psum=True)
            for i in range(4):
                kk = q4 * 4 + i
                p.tr(pt[:, i, :], brn[:, kk * 128:(kk + 1) * 128], identb[:])
            p.copy(brT[:, q4 * 4:q4 * 4 + 4, :], pt[:])
        p.act(bt[:], bt[:], AF.Sigmoid)
        merged = p.rot("merged", [128, D], F32, 1)
        for nb in range(2):
            cs = slice(nb * 512, (nb + 1) * 512)
            for i in range(3):
                ps = p.rot("ps_mm", [128, 512], F32, 4, psum=True)
                for k in range(4):
                    p.mm(ps[:], brT[:, 4 * i + k, :], Wbr[:, 4 * i + k, cs], start=(k == 0), stop=(k == 3))
                if i == 0:
                    p.tt(merged[:, cs], ps[:], bt[:, i * D + nb * 512: i * D + (nb + 1) * 512], ALU.mult)
                else:
                    tmp = p.rot("mtmp", [128, 512], F32, 2)
                    p.tt(tmp[:], ps[:], bt[:, i * D + nb * 512: i * D + (nb + 1) * 512], ALU.mult)
                    p.tt(merged[:, cs], merged[:, cs], tmp[:], ALU.add)
        mb = p.rot("mb", [128, D], BF16, 1)
        p.copy(mb[:], merged[:], e="pool")
        mT = p.rot("mT", [128, 8, 128], BF16, 1)
        for q4 in range(2):
            pt = p.rot("psT", [128, 4, 128], BF16, 2, psum=True)
            for i in range(4):
                kk = q4 * 4 + i
                p.tr(pt[:, i, :], mb[:, kk * 128:(kk + 1) * 128], identb[:])
            p.copy(mT[:, q4 * 4:q4 * 4 + 4, :], pt[:])
        x1 = p.rot("x1", [128, D], F32, 2)
        for nb in range(2):
            cs = slice(nb * 512, (nb + 1) * 512)
            ps = p.rot("ps_mm", [128, 512], F32, 4, psum=True)
            for k in range(8):
                p.mm(ps[:], mT[:, k, :], Wout[:, k, cs], start=(k == 0), stop=(k == 7))
            tmp = p.rot("mtmp", [128, 512], F32, 2)
            p.tt(tmp[:], ps[:], gate_m[:, cs], ALU.mult)
            p.tt(x1[:, cs], tmp[:], xt[:, cs], ALU.add)
        p.dma(x1_o[rs, :], x1[:])
        h2 = p.rot("h2", [128, D], F32, 2)
        emit_rmsnorm_mod(p, x1[:], gmod_f[:], shift_f[:], h2[:])
        p.dma(h2_o[rs, :], h2[:], q="pool")
        h2T = p.rot("h2Tf", [128, 8, 128], F32, 1)
        for q4 in range(2):
            pt = p.rot("psTf", [128, 4, 128], F32, 1, psum=True)
            for i in range(4):
                kk = q4 * 4 + i
                p.tr(pt[:, i, :], h2[:, kk * 128:(kk + 1) * 128], ident[:])
            p.copy(h2T[:, q4 * 4:q4 * 4 + 4, :], pt[:])
        pr = p.rot("ps_r", [128, 36], F32, 1, psum=True)
        for k in range(8):
            p.mm(pr[:], h2T[:, k, :], wgr[:, k, :], start=(k == 0), stop=False)
        p.mm(pr[:], ones_f[0:1, :], bgr[0:1, :], start=False, stop=True)
        lg = p.rot("lg", [128, 36], F32, 2)
        p.copy(lg[:], pr[:])
        sm = p.rot("rt_sm", [128, 16], F32, 2)
        gmax, ngmax, sg, pgrp, m1, m2, e2, rden, w1, w2 = [sm[:, i:i + 1] for i in range(10)]
        p.reduce(gmax, lg[:, 0:4], op=ALU.max)
        oh = p.rot("rt_oh", [128, 4], F32, 2)
        p.ts(oh[:], lg[:, 0:4], gmax, ALU.is_equal)
        p.ts(ngmax, gmax, -1.0, ALU.mult)
        eg = p.rot("rt_eg", [128, 4], F32, 2)
        p.act(eg[:], lg[:, 0:4], AF.Exp, bias=ngmax)
        p.reduce(sg, eg[:])
        p.recip(pgrp, sg)
        el = p.rot("rt_el", [128, 8], F32, 2)
        p.ts(el[:], lg[:, 4:12], oh[:, 0:1], ALU.mult)
        for g in range(1, 4):
            p.stt(el[:], lg[:, 4 + 8 * g:12 + 8 * g], oh[:, g:g + 1], el[:], ALU.mult, ALU.add)
        p.reduce(m1, el[:], op=ALU.max)
        mk1 = p.rot("rt_mk1", [128, 8], F32, 2)
        p.ts(mk1[:], el[:], m1, ALU.is_equal)
        el2 = p.rot("rt_el2", [128, 8], F32, 2)
        p.ts(el2[:], mk1[:], -1e30, ALU.mult)
        p.tt(el2[:], el2[:], el[:], ALU.add)
        p.reduce(m2, el2[:], op=ALU.max)
        mk2 = p.rot("rt_mk2", [128, 8], F32, 2)
        p.ts(mk2[:], el2[:], m2, ALU.is_equal)
        p.tt(e2, m2, m1, ALU.subtract)
        p.act(e2, e2, AF.Exp)
        p.ts(rden, e2, 1.0, ALU.add)
        p.recip(rden, rden)
        p.tt(w1, rden, pgrp, ALU.mult)
        p.tt(w2, w1, e2, ALU.mult)
        wexp = p.rot("rt_wexp", [128, 8], F32, 2)
        p.ts(wexp[:], mk1[:], w1, ALU.mult)
        p.stt(wexp[:], mk2[:], w2, wexp[:], ALU.mult, ALU.add)
        wt = p.rot("rt_wt", [128, 32], F32, 2)
        for g in range(4):
            p.ts(wt[:, 8 * g:8 * g + 8], wexp[:], oh[:, g:g + 1], ALU.mult)
        p.dma(wt_o[rs, :], wt[:])
    p.finish([x1_o, h2_o, wt_o, gf_o])
    return nc, p


def build_k3b(ntiles=NT, nexp=32):
    nc = bass.Bass("TRN2", target_bir_lowering=False)
    p = P(nc)
    ntok = ntiles * 128
    di = lambda n, s: p.dram(n, s, F32, kind="ExternalInput")
    x1_d = di("x1", [ntok, D]); h2_d = di("h2", [ntok, D]); wt_d = di("wt", [ntok, 32]); gf_d = di("gf", [128, D])
    wg_d = di("w_gate", [nexp * D, 512]); wu_d = di("w_up", [nexp * D, 512]); wd_d = di("w_down", [nexp * 512, D])
    ident_d = di("ident_in", [128, 128])
    xo = p.dram("xo", [ntok, D], F32, kind="ExternalOutput")
    ident = p.sb([128, 128], F32, "ident")
    p.dma(ident[:], ident_d[:])
    identb = p.sb([128, 128], BF16, "identb")
    p.copy(identb[:], ident[:])
    gf = p.sb([128, D], F32, "gf")
    p.dma(gf[:], gf_d[:])
    x1 = p.sb([128, ntiles, D], F32, "x1")
    h2T = p.sb([128, ntiles, 8, 128], BF16, "h2T")
    wt = p.sb([128, ntiles, 32], F32, "wt")
    for t in range(ntiles):
        rs = slice(t * 128, (t + 1) * 128)
        p.dma(x1[:, t, :], x1_d[rs, :])
        p.dma(wt[:, t, :], wt_d[rs, :], q="pool")
        ht = p.rot("ht", [128, D], F32, 2)
        p.dma(ht[:], h2_d[rs, :], q="pool")
        hb = p.rot("hb", [128, D], BF16, 2)
        p.copy(hb[:], ht[:])
        for q4 in range(2):
            pt = p.rot("psT", [128, 4, 128], BF16, 2, psum=True)
            for i in range(4):
                kk = q4 * 4 + i
                p.tr(pt[:, i, :], hb[:, kk * 128:(kk + 1) * 128], identb[:])
            p.copy(h2T[:, t, q4 * 4:q4 * 4 + 4, :], pt[:])
    for e in range(nexp):
        Wg = p.rot("Wg", [128, 8, 512], BF16, 2)
        Wu = p.rot("Wu", [128, 8, 512], BF16, 2)
        Wd = p.rot("Wd", [128, 4, D], BF16, 2)
        load_w_bf16(p, wg_d, D, 512, Wg, r0=e * D)
        load_w_bf16(p, wu_d, D, 512, Wu, r0=e * D)
        for k in range(4):
            st = p.rot("wstage", [128, 2048], F32, 2)
            p.dma(st[:, 0:D], wd_d[e * 512 + k * 128:e * 512 + (k + 1) * 128, :])
            p.tt(Wd[:, k, :], st[:, 0:D], gf[:], ALU.mult)
        stb = {}

        def partA(t):
            pg = p.rot("ps_mm", [128, 512], F32, 4, psum=True)
            for k in range(8):
                p.mm(pg[:], h2T[:, t, k, :], Wg[:, k, :], start=(k == 0), stop=(k == 7))
            pu = p.rot("ps_mm", [128, 512], F32, 4, psum=True)
            for k in range(8):
                p.mm(pu[:], h2T[:, t, k, :], Wu[:, k, :], start=(k == 0), stop=(k == 7))
            sg = p.rot("sg", [128, 512], F32, 2)
            p.act(sg[:], pg[:], AF.Silu)
            hid = p.rot("hid", [128, 512], BF16, 3)
            p.stt(hid[:], sg[:], wt[:, t, e:e + 1], pu[:], ALU.mult, ALU.mult)
            stb[t] = hid

        def partB(t):
            hid = stb.pop(t)
            pt = p.rot("psT", [128, 4, 128], BF16, 2, psum=True)
            for i in range(4):
                p.tr(pt[:, i, :], hid[:, i * 128:(i + 1) * 128], identb[:])
            hT = p.rot("hidT", [128, 4, 128], BF16, 3)
            p.copy(hT[:], pt[:])
            stb[("T", t)] = hT

        def partC(t):
            hT = stb.pop(("T", t))
            for nb in range(2):
                cs = slice(nb * 512, (nb + 1) * 512)
                py = p.rot("ps_py", [128, 512], F32, 2, psum=True)
                for k in range(4):
                    p.mm(py[:], hT[:, k, :], Wd[:, k, cs], start=(k == 0), stop=(k == 3))
                p.tt(x1[:, t, cs], x1[:, t, cs], py[:], ALU.add)

        for step in range(ntiles + 2):
            if step < ntiles:
                partA(step)
            if 0 <= step - 1 < ntiles:
                partB(step - 1)
            if 0 <= step - 2 < ntiles:
                partC(step - 2)
    for t in range(ntiles):
        p.dma(xo[t * 128:(t + 1) * 128, :], x1[:, t, :], q=("sp" if t % 2 == 0 else "pool"))
    p.finish([xo])
    return nc, p


_PROGS = {}


def _prog(name, fn):
    if name not in _PROGS:
        _PROGS[name] = fn()[0]
    return _PROGS[name]


def _run(nc, in_maps):
    in_maps = [{k: np.ascontiguousarray(v, dtype=np.float32) for k, v in m.items()} for m in in_maps]
    return run_bass_kernel_spmd(nc, in_maps, core_ids=list(range(NCORE))).results


def _padT(a):
    return np.ascontiguousarray(np.concatenate([np.zeros((a.shape[1], 3), np.float32), a.T], 1))


def _sb_masks(j):
    kk = (np.arange(8)[:, None, None] * 128 + np.arange(128)[None, :, None])
    qq = j * 512 + np.arange(512)[None, None, :]
    return np.ascontiguousarray((kk < qq).astype(np.float32).transpose(1, 0, 2))


def kernel(x, c, w_ada, b_ada, norm_mix, norm_ffn, w_in, ssm_conv_w, ssm_conv_b, ssm_dt_bias, ssm_a_log, ssm_d,
           ssm_norm, sb_q_norm, sb_k_norm, gdn_conv_w, gdn_a_log, gdn_dt_bias, gdn_norm, w_branch, w_out,
           w_group, b_group, w_router, b_router, w_gate, w_up, w_down):
    f32 = lambda a: np.asarray(a, dtype=np.float32)
    x = f32(x)[0]
    c_col = np.ascontiguousarray(f32(c)[0].reshape(8, 128).T)
    I = np.eye(128, dtype=np.float32)
    i128 = np.arange(128)
    i64 = np.arange(64)
    triu128 = (i128[:, None] <= i128[None, :]).astype(np.float32)
    mneg128 = np.where(i128[:, None] <= i128[None, :], 0.0, -30000.0).astype(np.float32)
    mincl = (i128[:, None] >= i128[None, :]).astype(np.float32)
    triu64 = (i64[:, None] <= i64[None, :]).astype(np.float32)
    mpos64 = np.where(i64[:, None] > i64[None, :], 0.0, 30000.0).astype(np.float32)
    mneg64 = np.where(i64[:, None] <= i64[None, :], 0.0, -30000.0).astype(np.float32)
    st01 = (i64[:, None] < i64[None, :]).astype(np.float32)
    sbm = [_sb_masks(0), _sb_masks(1)]
    k1 = _prog("k1", build_k1)
    kssd = _prog("ssd", build_ssd)
    ksb = _prog("sb", build_sb)
    kgdn = _prog("gdn", build_gdn)
    k3a = _prog("k3a", build_k3a)
    k3b = _prog("k3b", build_k3b)
    for l in range(4):
        wa = f32(w_ada[l]); ba = f32(b_ada[l])
        r = _run(k1, [{"x": x[TOK * i:TOK * (i + 1)], "c_col": c_col, "wada": wa[:, 0:2 * D], "bada": ba[None, 0:2 * D],
                       "norm_g": f32(norm_mix[l])[None], "w_in": f32(w_in[l]), "ident_in": I} for i in range(NCORE)])
        proj = np.concatenate([r[i]["proj"] for i in range(NCORE)], 0)
        m_z = proj[:, 0:512]; m_xbc = proj[:, 512:1536]; m_dt = proj[:, 1536:1544]
        sbq = proj[:, 1544:3080]; gq = proj[:, 3080:4616]; g_a = proj[:, 4616:4620]; g_b = proj[:, 4620:4624]
        g_gate = proj[:, 4624:5136]; br_gate = proj[:, 5136:8208]
        cw = f32(ssm_conv_w[l]); cb = f32(ssm_conv_b[l])
        ims = []
        for i in range(NCORE):
            g = i // 4
            xs = slice(64 * i, 64 * i + 64); bs = slice(512 + 128 * g, 640 + 128 * g); cs = slice(768 + 128 * g, 896 + 128 * g)
            ims.append({"xT": _padT(m_xbc[:, xs]), "BT": _padT(m_xbc[:, bs]), "CT": _padT(m_xbc[:, cs]),
                        "wx": cw[:, xs].T, "bx": cb[xs, None], "wB": cw[:, bs].T, "bB": cb[bs, None], "wC": cw[:, cs].T, "bC": cb[cs, None],
                        "dt_col": m_dt[:, i].reshape(-1, 128).T,
                        "scal": np.tile(np.array([[f32(ssm_dt_bias[l])[i], f32(ssm_a_log[l])[i], f32(ssm_d[l])[i]]], np.float32), (128, 1)),
                        "triu": triu128, "mneg": mneg128, "ident_in": I})
        r = _run(kssd, ims)
        y_ssd = np.concatenate([r[i]["y"] for i in range(NCORE)], 1)
        ims = []
        for i in range(NCORE):
            h, j = i // 2, i % 2
            q = sbq[:, 128 * h:128 * h + 128]
            qsel = np.concatenate([q[(2 * m + j) * 512:(2 * m + j + 1) * 512] for m in range(16)], 0)
            ims.append({"q": qsel, "k": sbq[:, 512 + 128 * h:640 + 128 * h], "v": sbq[:, 1024 + 128 * h:1152 + 128 * h],
                        "qg": f32(sb_q_norm[l])[None], "kg": f32(sb_k_norm[l])[None], "mask": sbm[j], "mincl": mincl, "ident_in": I})
        r = _run(ksb, ims)
        y_sb = np.zeros((SEQ, 512), np.float32)
        for i in range(NCORE):
            h, j = i // 2, i % 2
            oT = r[i]["oT"]
            for m in range(16):
                g = 2 * m + j
                y_sb[g * 512:(g + 1) * 512, 128 * h:128 * h + 128] = oT[:, m * 512:(m + 1) * 512].T
        gw = f32(gdn_conv_w[l])
        ims = []
        for i in range(NCORE):
            h, e = i // 2, i % 2
            qs = slice(128 * h, 128 * h + 128); ks = slice(512 + 128 * h, 640 + 128 * h); vs = slice(1024 + 128 * h + 64 * e, 1024 + 128 * h + 64 * e + 64)
            ims.append({"qT": _padT(gq[:, qs]), "kT": _padT(gq[:, ks]), "vT": _padT(gq[:, vs]),
                        "wq": gw[:, qs].T, "wk": gw[:, ks].T, "wv": gw[:, vs].T,
                        "a_col": g_a[:, h].reshape(-1, 64).T, "b_col": g_b[:, h].reshape(-1, 64).T,
                        "scal": np.tile(np.array([[f32(gdn_a_log[l])[h], f32(gdn_dt_bias[l])[h]]], np.float32), (128, 1)),
                        "triu": triu64, "mpos": mpos64, "mneg": mneg64, "st01": st01, "ident_in": I})
        r = _run(kgdn, ims)
        o_gdn = np.zeros((SEQ, 512), np.float32)
        for i in range(NCORE):
            h, e = i // 2, i % 2
            o_gdn[:, 128 * h + 64 * e:128 * h + 64 * e + 64] = r[i]["o"]
        wgr = np.concatenate([f32(w_group[l]), f32(w_router[l])], 1)
        bgr = np.concatenate([f32(b_group[l]), f32(b_router[l])])[None]
        ims = []
        for i in range(NCORE):
            ts_ = slice(TOK * i, TOK * (i + 1))
            ims.append({"x": x[ts_], "z": m_z[ts_], "ggate": g_gate[ts_], "brg": br_gate[ts_], "ya": y_ssd[ts_], "yb": y_sb[ts_], "yc": o_gdn[ts_],
                        "c_col": c_col, "wada": wa[:, 2 * D:6 * D], "bada": ba[None, 2 * D:6 * D],
                        "ssm_g": f32(ssm_norm[l])[None], "gdn_g": f32(gdn_norm[l])[None], "ffn_g": f32(norm_ffn[l])[None],
                        "w_branch": f32(w_branch[l]).reshape(1536, D), "w_out": f32(w_out[l]), "wgr": wgr, "bgr": bgr, "ident_in": I})
        ra = _run(k3a, ims)
        wg = f32(w_gate[l]).reshape(-1, 512); wu = f32(w_up[l]).reshape(-1, 512); wd = f32(w_down[l]).reshape(-1, D)
        ims = [{"x1": ra[i]["x1"], "h2": ra[i]["h2"], "wt": ra[i]["wt"], "gf": ra[i]["gf"], "w_gate": wg, "w_up": wu, "w_down": wd, "ident_in": I}
               for i in range(NCORE)]
        rb = _run(k3b, ims)
        x = np.concatenate([rb[i]["xo"] for i in range(NCORE)], 0)
    return x[None].astype(np.float32)
```

```python
import numpy as np
import concourse.bass as bass
import concourse.mybir as mybir
from concourse.bass_utils import run_bass_kernel_spmd

F32 = mybir.dt.float32
BF16 = mybir.dt.bfloat16
I32 = mybir.dt.int32
U32 = mybir.dt.uint32
AF = mybir.ActivationFunctionType
ALU = mybir.AluOpType
AX = mybir.AxisListType

NDS = 12


class Buf:
    __slots__ = ("name", "w", "r")

    def __init__(self, name):
        self.name = name
        self.w = None
        self.r = {}


class V:
    __slots__ = ("ap", "bufs")

    def __init__(self, ap, bufs):
        self.ap = ap
        self.bufs = bufs


class T:
    def __init__(self, h, name, nbuf_axis=None, nbuf=1):
        self.h = h
        self.name = name
        self.buf = Buf(name)

    def __getitem__(self, idx):
        return V(self.h[idx], (self.buf,))

    def v(self, ap):
        return V(ap, (self.buf,))


class P:
    def __init__(self, nc, same_eng_sync=True):
        self.nc = nc
        self.engs = {"pe": nc.tensor, "dve": nc.vector, "act": nc.scalar, "pool": nc.gpsimd, "sp": nc.sync}
        self.sem = {k: nc.alloc_semaphore(name=f"s_{k}") for k in self.engs}
        self.cnt = {k: 0 for k in self.engs}
        self.seen = {k: {} for k in self.engs}
        self.dsem = {}
        self.dcnt = {}
        self.dnext = {}
        for q in ("sp", "pool", "act"):
            self.dsem[q] = [nc.alloc_semaphore(name=f"d_{q}{i}") for i in range(NDS)]
            self.dcnt[q] = [0] * NDS
            self.dnext[q] = 0
        self.same_eng_sync = same_eng_sync
        self.n_wait = 0
        self.n_ins = 0
        self._n = 0

    def sb(self, shape, dt=F32, name=None):
        self._n += 1
        name = name or f"sb{self._n}"
        h = self.nc.alloc_sbuf_tensor("S_" + name, list(shape), dt)
        return T(h, name)

    def ps(self, shape, dt=F32, name=None):
        self._n += 1
        name = name or f"ps{self._n}"
        h = self.nc.alloc_psum_tensor("P_" + name, list(shape), dt)
        return T(h, name)

    def dram(self, name, shape, dt=F32, kind="Internal"):
        h = self.nc.dram_tensor(name, list(shape), dt, kind=kind)
        return T(h, name)

    def rot(self, key, shape, dt, n, psum=False):
        if not hasattr(self, "_rot"):
            self._rot = {}
        if key not in self._rot:
            tiles = [(self.ps if psum else self.sb)(shape, dt, f"{key}_{i}") for i in range(n)]
            self._rot[key] = [tiles, 0]
        ent = self._rot[key]
        t = ent[0][ent[1] % len(ent[0])]
        ent[1] += 1
        return t

    def _semof(self, src):
        if src[0] == "e":
            return self.sem[src[1]]
        return self.dsem[src[1]][src[2]]

    def _wait(self, e, deps):
        best = {}
        for d in deps:
            if d is None:
                continue
            key = d[:-1]
            if key not in best or best[key] < d[-1]:
                best[key] = d[-1]
        for key, c in best.items():
            if key[0] == "e" and key[1] == e and not (self.same_eng_sync and e not in ("pe",)):
                continue
            if self.seen[e].get(key, 0) >= c:
                continue
            self.seen[e][key] = c
            self.engs[e].wait_ge(self._semof(key), c)
            self.n_wait += 1

    def _deps(self, reads, writes):
        deps = set()
        for v in reads:
            for b in v.bufs:
                if b.w is not None:
                    deps.add(b.w)
        for v in writes:
            for b in v.bufs:
                if b.w is not None:
                    deps.add(b.w)
                deps.update(b.r.values())
        return deps

    def _commit(self, tok, reads, writes):
        for v in writes:
            for b in v.bufs:
                b.w = tok
                b.r = {}
        wb = set()
        for v in writes:
            wb.update(id(b) for b in v.bufs)
        for v in reads:
            for b in v.bufs:
                if id(b) not in wb:
                    b.r[tok[:-1]] = tok

    def op(self, e, fn, reads=(), writes=()):
        self._wait(e, self._deps(reads, writes))
        ins = fn(self.engs[e])
        self.cnt[e] += 1
        ins.then_inc(self.sem[e], 1)
        self.n_ins += 1
        self._commit(("e", e, self.cnt[e]), reads, writes)
        return ins

    def dma(self, out, in_, q="sp", **kw):
        i = self.dnext[q]
        self.dnext[q] = (i + 1) % NDS
        deps = self._deps([in_], [out])
        if self.dcnt[q][i] > 0:
            deps.add(("d", q, i, 16 * self.dcnt[q][i]))
        self._wait(q, deps)
        ins = self.engs[q].dma_start(out=out.ap, in_=in_.ap, **kw)
        self.dcnt[q][i] += 1
        ins.then_inc(self.dsem[q][i], 16)
        self.n_ins += 1
        self._commit(("d", q, i, 16 * self.dcnt[q][i]), [in_], [out])
        return ins

    def finish(self, outs, e="sp"):
        deps = set()
        for t in outs:
            if t.buf.w is not None:
                deps.add(t.buf.w)
        for k in self.engs:
            if self.cnt[k] > 0:
                deps.add(("e", k, self.cnt[k]))
        for q in self.dsem:
            for i in range(NDS):
                if self.dcnt[q][i] > 0:
                    deps.add(("d", q, i, 16 * self.dcnt[q][i]))
        self._wait(e, deps)

    def mm(self, out, lhsT, rhs, start=True, stop=True):
        rd = [lhsT, rhs]
        return self.op("pe", lambda e: e.matmul(out.ap, lhsT.ap, rhs.ap, start=start, stop=stop), rd, [out])

    def tr(self, out, in_, ident):
        return self.op("pe", lambda e: e.transpose(out.ap, in_.ap, ident.ap), [in_, ident], [out])

    def act(self, out, in_, func, bias=None, scale=None, accum=None, e="act"):
        kw = {}
        rd = [in_]
        wr = [out]
        if bias is not None:
            if isinstance(bias, V):
                kw["bias"] = bias.ap
                rd.append(bias)
            else:
                kw["bias"] = bias
        if scale is not None:
            if isinstance(scale, V):
                kw["scale"] = scale.ap
                rd.append(scale)
            else:
                kw["scale"] = scale
        if accum is not None:
            kw["accum_out"] = accum.ap
            wr.append(accum)
        return self.op("act", lambda en: en.activation(out.ap, in_.ap, func, **kw), rd, wr)

    def tt(self, out, a, b, op, e="dve"):
        return self.op(e, lambda en: en.tensor_tensor(out.ap, a.ap, b.ap, op), [a, b], [out])

    def ts(self, out, a, s1, op0, s2=None, op1=None, accum=None, e="dve"):
        rd = [a]
        wr = [out]
        s1a = s1.ap if isinstance(s1, V) else s1
        s2a = s2.ap if isinstance(s2, V) else s2
        if isinstance(s1, V):
            rd.append(s1)
        if isinstance(s2, V):
            rd.append(s2)
        kw = {}
        if op1 is not None:
            kw["op1"] = op1
        if accum is not None:
            kw["accum_out"] = accum.ap
            wr.append(accum)
        return self.op(e, lambda en: en.tensor_scalar(out.ap, a.ap, s1a, s2a, op0, **kw), rd, wr)

    def stt(self, out, a, s, b, op0, op1, e="dve"):
        rd = [a, b]
        sa = s.ap if isinstance(s, V) else s
        if isinstance(s, V):
            rd.append(s)
        return self.op(e, lambda en: en.scalar_tensor_tensor(out.ap, a.ap, sa, b.ap, op0, op1), rd, [out])

    def copy(self, out, in_, e="dve"):
        if e == "act":
            return self.op("act", lambda en: en.copy(out.ap, in_.ap), [in_], [out])
        return self.op(e, lambda en: en.tensor_copy(out.ap, in_.ap), [in_], [out])

    def memset(self, out, val, e="dve"):
        return self.op(e, lambda en: en.memset(out.ap, val), [], [out])

    def reduce(self, out, in_, op=ALU.add, axis=AX.X, e="dve"):
        return self.op(e, lambda en: en.tensor_reduce(out.ap, in_.ap, axis, op), [in_], [out])

    def recip(self, out, in_):
        return self.op("dve", lambda en: en.reciprocal(out.ap, in_.ap), [in_], [out])


D = 1024
SEQ = 16384
NCORE = 8
TOK = SEQ // NCORE
NT = TOK // 128
IN_COLS = 8208
EPS = 1e-6
DEBUG = False
SAME_ENG = False


def bcast_row(t, n, parts=128):
    return t.v(t.h[0:1, 0:n].to_broadcast([parts, n]))


def emit_mod(p, c_bc, wada, bada, col0, ncols, out_tile, ones1, plus_one=False):
    nblk = (ncols + 511) // 512
    for b in range(nblk):
        n = min(512, ncols - b * 512)
        ws = p.rot("modw", [128, 8, 512], F32, 1)
        p.dma(ws[:, :, 0:n], wada.v(wada.h[:, col0 + b * 512: col0 + b * 512 + n].rearrange("(k p) n -> p k n", p=128)))
        bs = p.rot("modb", [1, 512], F32, 2)
        p.dma(bs[0:1, 0:n], bada[0:1, col0 + b * 512: col0 + b * 512 + n])
        ps = p.rot("ps_mm", [128, 512], F32, 4, psum=True)
        for k in range(8):
            p.mm(ps[:, 0:n], c_bc[:, k, :], ws[:, k, 0:n], start=(k == 0), stop=False)
        p.mm(ps[:, 0:n], ones1[0:1, :], bs[0:1, 0:n], start=False, stop=True)
        if plus_one:
            p.ts(out_tile[:, b * 512: b * 512 + n], ps[:, 0:n], 1.0, ALU.add)
        else:
            p.copy(out_tile[:, b * 512: b * 512 + n], ps[:, 0:n])


def emit_cbc(p, c_in, ones_f):
    c_col = p.sb([128, 8], F32, "c_col")
    p.dma(c_col[:], c_in[:])
    c_act = p.sb([128, 8], F32, "c_act")
    p.act(c_act[:], c_col[:], AF.Silu)
    c_bc = p.sb([128, 8, 128], F32, "c_bc")
    for k in range(8):
        p.ts(c_bc[:, k, :], ones_f[:], c_act[:, k:k + 1], ALU.mult)
    return c_bc


def emit_rmsnorm_mod(p, x_t, gmod, shift, h_out, width=D):
    junk = p.rot("rn_junk", [128, width], F32, 1)
    ssq = p.rot("rn_ssq", [128, 1], F32, 2)
    p.act(junk[:], x_t, AF.Square, accum=ssq[:])
    rstd = p.rot("rn_rstd", [128, 1], F32, 2)
    p.act(rstd[:], ssq[:], AF.Sqrt, bias=EPS, scale=1.0 / width)
    p.recip(rstd[:], rstd[:])
    tmp = p.rot("rn_tmp", [128, width], F32, 1)
    p.stt(tmp[:], x_t, rstd[:], gmod, ALU.mult, ALU.mult)
    p.tt(h_out, tmp[:], shift, ALU.add)


def build_k1(ntiles=NT, ncols=IN_COLS):
    nc = bass.Bass("TRN2", target_bir_lowering=False)
    p = P(nc)
    ntok = ntiles * 128
    x = p.dram("x", [ntok, D], F32, kind="ExternalInput")
    c_in = p.dram("c_col", [128, 8], F32, kind="ExternalInput")
    wada = p.dram("wada", [D, 2 * D], F32, kind="ExternalInput")
    bada = p.dram("bada", [1, 2 * D], F32, kind="ExternalInput")
    ng = p.dram("norm_g", [1, D], F32, kind="ExternalInput")
    w_in = p.dram("w_in", [D, ncols], F32, kind="ExternalInput")
    ident_d = p.dram("ident_in", [128, 128], F32, kind="ExternalInput")
    out = p.dram("proj", [ntok, ncols], F32, kind="ExternalOutput")

    ident = p.sb([128, 128], F32, "ident")
    p.dma(ident[:], ident_d[:])
    identb = p.sb([128, 128], BF16, "identb")
    p.copy(identb[:], ident[:])
    ones_f = p.sb([128, 128], F32, "ones_f")
    p.memset(ones_f[:], 1.0)
    c_bc = emit_cbc(p, c_in, ones_f)
    shift = p.sb([128, D], F32, "shift")
    gmod = p.sb([128, D], F32, "gmod")
    emit_mod(p, c_bc, wada, bada, 0, D, shift, ones_f)
    emit_mod(p, c_bc, wada, bada, D, D, gmod, ones_f, plus_one=True)
    ngb = p.sb([128, D], F32, "ngb")
    p.dma(ngb[:], bcast_row(ng, D))
    p.tt(gmod[:], gmod[:], ngb[:], ALU.mult)

    HALF = (ncols + 1) // 2
    Wb = p.sb([128, 8, HALF], BF16, "Wb")
    PIECE = 2052
    cv = 0
    for half in range(2):
        h0 = half * HALF
        hn = min(HALF, ncols - h0)
        for k in range(8):
            p.dma(Wb[:, k, 0:hn], w_in[k * 128:(k + 1) * 128, h0:h0 + hn], q="pool")
        for t in range(ntiles):
            xt = p.rot("xt", [128, D], F32, 2)
            p.dma(xt[:], x[t * 128:(t + 1) * 128, :])
            hb = p.rot("hb", [128, D], BF16, 2)
            emit_rmsnorm_mod(p, xt[:], gmod[:], shift[:], hb[:])
            psT = p.rot("psT", [128, 8, 128], BF16, 1, psum=True)
            for k in range(8):
                p.tr(psT[:, k, :], hb[:, k * 128:(k + 1) * 128], identb[:])
            hT = p.rot("hT", [128, 8, 128], BF16, 2)
            p.copy(hT[:], psT[:], e="act")
            ot = p.rot("ot", [128, HALF], F32, 2)
            nb = (hn + 511) // 512
            for b in range(nb):
                n = min(512, hn - b * 512)
                ps = p.rot("ps_mm", [128, 512], F32, 4, psum=True)
                for k in range(8):
                    p.mm(ps[:, 0:n], hT[:, k, :], Wb[:, k, b * 512: b * 512 + n], start=(k == 0), stop=(k == 7))
                p.copy(ot[:, b * 512:b * 512 + n], ps[:, 0:n], e=("dve" if b % 2 == 0 else "act"))
            p.dma(out[t * 128:(t + 1) * 128, h0:h0 + hn], ot[:, 0:hn], q=("sp" if t % 2 == 0 else "pool"))
    if DEBUG:
        dbg = p.dram("dbg", [128, 4 * D], F32, kind="ExternalOutput")
        p.dma(dbg[:, 0:D], gmod[:])
        p.dma(dbg[:, D:2 * D], shift[:])
        hf = p.sb([128, D], F32, "hf")
        p.copy(hf[:], hb[:])
        p.dma(dbg[:, 2 * D:3 * D], hf[:])
        hf2 = p.sb([128, D], F32, "hf2")
        p.copy(hf2[:], hT[:])
        p.dma(dbg[:, 3 * D:4 * D], hf2[:])
        p.finish([out, dbg])
        return nc, p
    p.finish([out])
    return nc, p


def emit_qk_norm(p, src, nblk, gain_bc, identb, dstT, scale, neg_dstT=None):
    for b0 in range(0, nblk, 4):
        t = p.rot("qk_in", [128, 4, 128], F32, 2)
        p.dma(t[:], src.v(src.h[b0 * 128:(b0 + 4) * 128, :].rearrange("(b p) d -> p b d", p=128)), q=("sp" if (b0 // 4) % 2 == 0 else "pool"))
        sq = p.rot("qk_sq", [128, 4, 128], F32, 1)
        p.act(sq[:], t[:], AF.Square)
        ssq = p.rot("qk_ssq", [128, 4], F32, 2)
        p.reduce(ssq[:], sq[:])
        rstd = p.rot("qk_rstd", [128, 4], F32, 2)
        p.act(rstd[:], ssq[:], AF.Sqrt, bias=EPS, scale=1.0 / 128)
        p.recip(rstd[:], rstd[:])
        nb = p.rot("qk_nb", [128, 4, 128], BF16, 2)
        for i in range(4):
            p.stt(nb[:, i, :], t[:, i, :], rstd[:, i:i + 1], gain_bc[:], ALU.mult, ALU.mult)
        pt = p.rot("qk_pt", [128, 512], BF16, 1, psum=True)
        for i in range(4):
            p.tr(pt[:, i * 128:(i + 1) * 128], nb[:, i, :], identb[:])
        p.ts(dstT[:, b0 * 128:(b0 + 4) * 128], pt[:], scale, ALU.mult, e="pool" if False else "dve")
        if neg_dstT is not None:
            p.ts(neg_dstT[:, b0 * 128:(b0 + 4) * 128], pt[:], -scale, ALU.mult)


def build_sb(nblk=128):
    nc = bass.Bass("TRN2", target_bir_lowering=False)
    p = P(nc)
    S = nblk * 128
    ngrp = nblk // 8
    q_d = p.dram("q", [ngrp * 512, 128], F32, kind="ExternalInput")
    k_d = p.dram("k", [S, 128], F32, kind="ExternalInput")
    v_d = p.dram("v", [S, 128], F32, kind="ExternalInput")
    qg_d = p.dram("qg", [1, 128], F32, kind="ExternalInput")
    kg_d = p.dram("kg", [1, 128], F32, kind="ExternalInput")
    mask_d = p.dram("mask", [128, 8, 512], F32, kind="ExternalInput")
    mincl_d = p.dram("mincl", [128, 128], F32, kind="ExternalInput")
    ident_d = p.dram("ident_in", [128, 128], F32, kind="ExternalInput")
    out = p.dram("oT", [128, ngrp * 512], F32, kind="ExternalOutput")

    ident = p.sb([128, 128], F32, "ident")
    p.dma(ident[:], ident_d[:])
    identb = p.sb([128, 128], BF16, "identb")
    p.copy(identb[:], ident[:])
    mincl_f = p.sb([128, 128], F32, "mincl_f")
    p.dma(mincl_f[:], mincl_d[:])
    mincl = p.sb([128, 128], BF16, "mincl")
    p.copy(mincl[:], mincl_f[:])
    onesb = p.sb([128, 128], BF16, "onesb")
    p.memset(onesb[:], 1.0)
    maskb = p.sb([128, 8, 512], BF16, "maskb")
    for o in range(8):
        mt = p.rot("mask_st", [128, 512], F32, 2)
        p.dma(mt[:], mask_d[:, o, :])
        p.copy(maskb[:, o, :], mt[:])
    qg = p.sb([128, 128], F32, "qg")
    p.dma(qg[:], bcast_row(qg_d, 128))
    kg = p.sb([128, 128], F32, "kg")
    p.dma(kg[:], bcast_row(kg_d, 128))

    knT = p.sb([128, S], BF16, "knT")
    nknT = p.sb([128, S], BF16, "nknT")
    qnT = p.sb([128, ngrp * 512], BF16, "qnT")
    vb = p.sb([128, nblk, 128], BF16, "vb")
    emit_qk_norm(p, k_d, nblk, kg, identb, knT, 1.0, nknT)
    emit_qk_norm(p, q_d, ngrp * 4, qg, identb, qnT, 128 ** -0.5)
    for b0 in range(0, nblk, 4):
        t = p.rot("qk_in", [128, 4, 128], F32, 2)
        p.dma(t[:], v_d.v(v_d.h[b0 * 128:(b0 + 4) * 128, :].rearrange("(b p) d -> p b d", p=128)))
        p.copy(vb[:, b0:b0 + 4, :], t[:], e="pool")

    import os
    STOP = int(os.environ.get("SB_STOP", "0"))
    if STOP == 1:
        for m in range(ngrp):
            ot = p.rot("sbOT", [128, 512], F32, 2)
            p.copy(ot[:], qnT[:, m * 512:(m + 1) * 512])
            p.dma(out[:, m * 512:(m + 1) * 512], ot[:])
        p.finish([out])
        return nc, p
    for m in range(ngrp):
        O = p.rot("sbO", [128, 512], F32, 1, psum=True)
        CS = p.rot("sbCS", [128, 512], F32, 1, psum=True)
        qs = qnT[:, m * 512:(m + 1) * 512]
        kbs = list(range(8 * m + 7, -1, -1))
        nit = len(kbs)
        st = {}

        def phaseA(it):
            kb = kbs[it]
            off = kb - 8 * m
            Z = p.rot("sbZ", [128, 512], F32, 2, psum=True)
            U = p.rot("sbU", [128, 512], F32, 3, psum=True)
            p.mm(Z[:], knT[:, kb * 128:(kb + 1) * 128], qs)
            p.mm(U[:], nknT[:, kb * 128:(kb + 1) * 128], qs, start=True, stop=False)
            e = p.rot("sbE", [128, 512], F32, 3)
            p.act(e[:], Z[:], AF.Exp)
            st[("e", it)] = (U, e, off)

        def phaseA2(it):
            U, e, off = st.pop(("e", it))
            spb = p.rot("sbSP", [128, 512], BF16, 4)
            p.act(spb[:], e[:], AF.Ln, bias=1.0)
            if off >= 0:
                p.tt(spb[:], spb[:], maskb[:, off, :], ALU.mult)
            st[it] = (U, spb)

        def phaseB(it):
            kb = kbs[it]
            off = kb - 8 * m
            first = it == 0
            last = it == nit - 1
            U, spb = st.pop(it)
            p.mm(U[:], mincl[:], spb[:], start=False, stop=first)
            if not first:
                p.mm(U[:], identb[:], st["accb"][:], start=False, stop=True)
            p.mm(CS[:], onesb[:], spb[:], start=first, stop=last)
            att = p.rot("sbATT", [128, 512], BF16, 2)
            p.act(att[:], U[:], AF.Exp, scale=-1.0)
            if off >= 0:
                p.tt(att[:], att[:], maskb[:, off, :], ALU.mult)
            if not last:
                accb = p.rot("sbACC", [128, 512], BF16, 2)
                p.copy(accb[:], CS[:])
                st["accb"] = accb
            p.mm(O[:], vb[:, kb, :], att[:], start=first, stop=last)

        AH = 2
        for it in range(min(AH, nit)):
            phaseA(it)
            phaseA2(it)
        for it in range(nit):
            if it + AH < nit:
                phaseA(it + AH)
            phaseB(it)
            if it + AH < nit:
                phaseA2(it + AH)
        ot = p.rot("sbOT", [128, 512], F32, 2)
        p.copy(ot[:], O[:])
        p.dma(out[:, m * 512:(m + 1) * 512], ot[:])
    p.finish([out])
    return nc, p


def emit_conv_silu(p, src_d, nch, S, w_d, b_d, dst, dst_dt, name, CH=2048, bias=True):
    w = p.sb([nch, 4], F32, name + "_w")
    p.dma(w[:], w_d[:])
    if bias:
        b = p.sb([nch, 1], F32, name + "_b")
        p.dma(b[:], b_d[:])
    for c0 in range(0, S, CH):
        t = p.rot("cv_in", [128, CH + 3], F32, 2)
        p.dma(t[0:nch, :], src_d[:, c0:c0 + CH + 3])
        acc = p.rot("cv_acc", [128, CH], F32, 2)
        p.ts(acc[0:nch, :], t[0:nch, 0:CH], w[:, 0:1], ALU.mult)
        for i in range(1, 4):
            p.stt(acc[0:nch, :], t[0:nch, i:i + CH], w[:, i:i + 1], acc[0:nch, :], ALU.mult, ALU.add)
        if bias:
            p.act(dst[0:nch, c0:c0 + CH], acc[0:nch, :], AF.Silu, bias=b[:, 0:1])
        else:
            p.act(dst[0:nch, c0:c0 + CH], acc[0:nch, :], AF.Silu)


def build_ssd(nchunk=128):
    nc = bass.Bass("TRN2", target_bir_lowering=False)
    p = P(nc)
    S = nchunk * 128
    CH = min(2048, S)
    xT_d = p.dram("xT", [64, S + 3], F32, kind="ExternalInput")
    BT_d = p.dram("BT", [128, S + 3], F32, kind="ExternalInput")
    CT_d = p.dram("CT", [128, S + 3], F32, kind="ExternalInput")
    wx_d = p.dram("wx", [64, 4], F32, kind="ExternalInput")
    bx_d = p.dram("bx", [64, 1], F32, kind="ExternalInput")
    wB_d = p.dram("wB", [128, 4], F32, kind="ExternalInput")
    bB_d = p.dram("bB", [128, 1], F32, kind="ExternalInput")
    wC_d = p.dram("wC", [128, 4], F32, kind="ExternalInput")
    bC_d = p.dram("bC", [128, 1], F32, kind="ExternalInput")
    dt_d = p.dram("dt_col", [128, nchunk], F32, kind="ExternalInput")
    sc_d = p.dram("scal", [128, 3], F32, kind="ExternalInput")
    triu_d = p.dram("triu", [128, 128], F32, kind="ExternalInput")
    mneg_d = p.dram("mneg", [128, 128], F32, kind="ExternalInput")
    ident_d = p.dram("ident_in", [128, 128], F32, kind="ExternalInput")
    out = p.dram("y", [S, 64], F32, kind="ExternalOutput")

    ident = p.sb([128, 128], F32, "ident")
    p.dma(ident[:], ident_d[:])
    identb = p.sb([128, 128], BF16, "identb")
    p.copy(identb[:], ident[:])
    triu = p.sb([128, 128], F32, "triu")
    p.dma(triu[:], triu_d[:])
    mneg = p.sb([128, 128], F32, "mneg")
    p.dma(mneg[:], mneg_d[:])
    ones_f = p.sb([128, 128], F32, "ones_f")
    p.memset(ones_f[:], 1.0)
    scal = p.sb([128, 3], F32, "scal")
    p.dma(scal[:], sc_d[:])
    dtc = p.sb([128, nchunk], F32, "dtc")
    p.dma(dtc[:], dt_d[:])
    p.act(dtc[:], dtc[:], AF.Exp, bias=scal[:, 0:1])
    p.act(dtc[:], dtc[:], AF.Ln, bias=1.0)
    aneg = p.sb([128, 1], F32, "aneg")
    p.act(aneg[:], scal[:, 1:2], AF.Exp)
    p.ts(aneg[:], aneg[:], -1.0, ALU.mult)
    adt = p.sb([128, nchunk], F32, "adt")
    p.ts(adt[:], dtc[:], aneg[:, 0:1], ALU.mult)

    xT = p.sb([64, S], F32, "xTs")
    BT = p.sb([128, S], BF16, "BTs")
    CT = p.sb([128, S], BF16, "CTs")
    emit_conv_silu(p, xT_d, 64, S, wx_d, bx_d, xT, F32, "cx", CH)
    emit_conv_silu(p, BT_d, 128, S, wB_d, bB_d, BT, BF16, "cB", CH)
    emit_conv_silu(p, CT_d, 128, S, wC_d, bC_d, CT, BF16, "cC", CH)

    state_f = p.sb([128, 64], F32, "state_f")
    state_b = p.sb([128, 64], BF16, "state_b")
    p.memset(state_f[:], 0.0)
    p.memset(state_b[:], 0.0)
    OB = 8
    for c in range(nchunk):
        sl = slice(c * 128, (c + 1) * 128)
        px = p.rot("ssd_px", [128, 64], F32, 1, psum=True)
        p.tr(px[:], xT[0:64, sl], ident[0:64, 0:64])
        xtok = p.rot("ssd_xtok", [128, 64], F32, 2)
        p.copy(xtok[:], px[:])
        pB = p.rot("ssd_pB", [128, 128], BF16, 1, psum=True)
        p.tr(pB[:], BT[:, sl], identb[:])
        Btok = p.rot("ssd_Btok", [128, 128], BF16, 2)
        p.copy(Btok[:], pB[:], e="act")
        adt_c = adt[:, c:c + 1]
        pcol = p.rot("ssd_pcol", [128, 1], F32, 1, psum=True)
        p.mm(pcol[:], triu[:], adt_c)
        acol = p.rot("ssd_acol", [128, 1], F32, 2)
        p.copy(acol[:], pcol[:])
        adt_bc = p.rot("ssd_adtbc", [128, 128], F32, 2)
        p.ts(adt_bc[:], ones_f[:], adt_c, ALU.mult)
        prow = p.rot("ssd_prow", [128, 128], F32, 1, psum=True)
        p.mm(prow[:], adt_bc[:], triu[:])
        dsg = p.rot("ssd_dsg", [128, 128], F32, 2)
        p.stt(dsg[:], prow[:], acol[:, 0:1], mneg[:], ALU.subtract, ALU.add)
        segT = p.rot("ssd_segT", [128, 128], F32, 2)
        p.act(segT[:], dsg[:], AF.Exp)
        ea = p.rot("ssd_ea", [128, 128], F32, 2)
        p.act(ea[:], prow[:], AF.Exp)
        alast = p.rot("ssd_alast", [128, 1], F32, 2)
        p.copy(alast[:], prow[:, 127:128])
        dte = p.rot("ssd_dte", [128, 1], F32, 2)
        p.act(dte[:], acol[:], AF.Exp, bias=alast[:, 0:1], scale=-1.0)
        cdec = p.rot("ssd_cdec", [128, 1], F32, 2)
        p.act(cdec[:], alast[:], AF.Exp)
        pcb = p.rot("ssd_pcb", [128, 128], F32, 1, psum=True)
        p.mm(pcb[:], BT[:, sl], CT[:, sl])
        scT = p.rot("ssd_scT", [128, 128], BF16, 2)
        p.tt(scT[:], pcb[:], segT[:], ALU.mult)
        xdt = p.rot("ssd_xdt", [128, 64], BF16, 2)
        p.ts(xdt[:], xtok[:], dtc[:, c:c + 1], ALU.mult)
        cdT = p.rot("ssd_cdT", [128, 128], BF16, 2)
        p.tt(cdT[:], CT[:, sl], ea[:], ALU.mult)
        py = p.rot("ssd_py", [128, 64], F32, 2, psum=True)
        p.mm(py[:], scT[:], xdt[:], start=True, stop=False)
        p.mm(py[:], cdT[:], state_b[:], start=False, stop=True)
        if c % OB == 0:
            yo = p.rot("ssd_yo", [128, OB, 64], F32, 2)
        p.stt(yo[:, c % OB, :], xtok[:], scal[:, 2:3], py[:], ALU.mult, ALU.add)
        if c % OB == OB - 1:
            c0 = (c - OB + 1) * 128
            p.dma(out.v(out.h[c0:c0 + OB * 128, :].rearrange("(b p) d -> p b d", p=128)), yo[:])
        sc2 = p.rot("ssd_sc2", [128, 1], F32, 2)
        p.tt(sc2[:], dtc[:, c:c + 1], dte[:], ALU.mult)
        xdd = p.rot("ssd_xdd", [128, 64], BF16, 2)
        p.ts(xdd[:], xtok[:], sc2[:, 0:1], ALU.mult)
        pS = p.rot("ssd_pS", [128, 64], F32, 1, psum=True)
        p.mm(pS[:], Btok[:], xdd[:])
        p.stt(state_f[:], state_f[:], cdec[:, 0:1], pS[:], ALU.mult, ALU.add)
        p.copy(state_b[:], state_f[:])
    p.finish([out])
    return nc, p


def build_gdn(nchunk=256, NB=4):
    import os
    nc = bass.Bass("TRN2", target_bir_lowering=False)
    p = P(nc)
    S = nchunk * 64
    CH = min(1024, S)
    CPS = CH // 64
    qT_d = p.dram("qT", [128, S + 3], F32, kind="ExternalInput")
    kT_d = p.dram("kT", [128, S + 3], F32, kind="ExternalInput")
    vT_d = p.dram("vT", [64, S + 3], F32, kind="ExternalInput")
    wq_d = p.dram("wq", [128, 4], F32, kind="ExternalInput")
    wk_d = p.dram("wk", [128, 4], F32, kind="ExternalInput")
    wv_d = p.dram("wv", [64, 4], F32, kind="ExternalInput")
    a_d = p.dram("a_col", [64, nchunk], F32, kind="ExternalInput")
    b_d = p.dram("b_col", [64, nchunk], F32, kind="ExternalInput")
    sc_d = p.dram("scal", [128, 2], F32, kind="ExternalInput")
    triu_d = p.dram("triu", [64, 64], F32, kind="ExternalInput")
    mpos_d = p.dram("mpos", [64, 64], F32, kind="ExternalInput")
    mneg_d = p.dram("mneg", [64, 64], F32, kind="ExternalInput")
    st01_d = p.dram("st01", [64, 64], F32, kind="ExternalInput")
    ident_d = p.dram("ident_in", [128, 128], F32, kind="ExternalInput")
    out = p.dram("o", [S, 64], F32, kind="ExternalOutput")

    def ld(d, shape, name):
        t = p.sb(shape, F32, name)
        p.dma(t[:], d[:])
        return t
    ident = ld(ident_d, [128, 128], "ident")
    triu = ld(triu_d, [64, 64], "triu")
    mpos = ld(mpos_d, [64, 64], "mpos")
    mneg = ld(mneg_d, [64, 64], "mneg")
    st01 = ld(st01_d, [64, 64], "st01")
    scal = ld(sc_d, [128, 2], "scal")
    ones_f = p.sb([64, 128], F32, "ones_f")
    p.memset(ones_f[:], 1.0)
    beta = ld(b_d, [64, nchunk], "beta")
    p.act(beta[:], beta[:], AF.Sigmoid)
    gall = ld(a_d, [64, nchunk], "gall")
    p.act(gall[:], gall[:], AF.Exp, bias=scal[0:64, 1:2])
    p.act(gall[:], gall[:], AF.Ln, bias=1.0)
    aneg = p.sb([64, 1], F32, "aneg")
    p.act(aneg[:], scal[0:64, 0:1], AF.Exp)
    p.ts(aneg[:], aneg[:], -1.0, ALU.mult)
    p.ts(gall[:], gall[:], aneg[:, 0:1], ALU.mult)

    wq = ld(wq_d, [128, 4], "wq")
    wk = ld(wk_d, [128, 4], "wk")
    wv = ld(wv_d, [64, 4], "wv")
    state = p.sb([128, 64], F32, "state")
    p.memset(state[:], 0.0)
    OB = 8
    RSQ = 128 ** -0.5
    D1 = NB + 1
    D2 = 2 * NB + 1

    def conv(src_d, nch, w, c0, key):
        t = p.rot(key + "_in", [128, CH + 3], F32, 1)
        p.dma(t[0:nch, :], src_d[:, c0:c0 + CH + 3])
        acc = p.rot(key + "_acc", [128, CH], F32, 1)
        p.ts(acc[0:nch, :], t[0:nch, 0:CH], w[:, 0:1], ALU.mult)
        for i in range(1, 4):
            p.stt(acc[0:nch, :], t[0:nch, i:i + CH], w[:, i:i + 1], acc[0:nch, :], ALU.mult, ALU.add)
        o = p.rot(key + "_o", [128, CH], F32, 2)
        p.act(o[0:nch, :], acc[0:nch, :], AF.Silu)
        return o

    oo_box = [None]

    def pre(c, ci, qTs, kTs, vTs, ctx):
        sl = slice(ci * 64, (ci + 1) * 64)
        PB = p.rot("g_PB", [128, 512], F32, 2 * NB, psum=True)
        ctx["PB"] = PB
        ptq, ptk, ptv = PB[0:64, 0:128], PB[0:64, 128:256], PB[0:64, 256:320]
        p.tr(ptq, qTs[:, sl], ident[:])
        p.tr(ptk, kTs[:, sl], ident[:])
        p.tr(ptv, vTs[0:64, sl], ident[0:64, 0:64])
        g_c = gall[:, c:c + 1]
        b_c = beta[:, c:c + 1]
        pcol = PB[0:64, 320:321]
        p.mm(pcol, triu[:], g_c)
        g_bc = p.rot("g_gbc", [64, 128], F32, D1)
        p.ts(g_bc[:], ones_f[:], g_c, ALU.mult)
        yield
        junk = p.rot("g_junk", [64, 128], F32, 2)
        ssq = p.rot("g_ssq", [64, 2], F32, D1)
        p.act(junk[:], ptq, AF.Square, accum=ssq[:, 0:1])
        p.act(junk[:], ptk, AF.Square, accum=ssq[:, 1:2])
        if os.environ.get('GDN_SUB') == 'a':
            yield
            return
        gcol = p.rot("g_gcol", [64, 1], F32, D1)
        p.copy(gcol[:], pcol, e="act")
        if os.environ.get('GDN_SUB') == 'b':
            yield
            return
        prow, prow64 = PB[:, 384:448], PB[0:64, 384:448]
        p.mm(prow, g_bc[:], triu[:])
        yield
        rinv = p.rot("g_rinv", [64, 2], F32, D1)
        p.act(rinv[:], ssq[:], AF.Sqrt, bias=EPS)
        nd = p.rot("g_nd", [64, 64], F32, D1)
        p.stt(nd[:], prow64, gcol[:, 0:1], mpos[:], ALU.subtract, ALU.add)
        d2 = p.rot("g_d2", [64, 64], F32, D1)
        p.stt(d2[:], prow64, gcol[:, 0:1], mneg[:], ALU.subtract, ALU.add)
        glast = p.rot("g_glast", [128, 1], F32, D1)
        p.copy(glast[:], PB[:, 447:448])
        yield
        p.recip(rinv[:], rinv[:])
        dec_s = p.rot("g_decs", [64, 64], F32, D1)
        p.act(dec_s[:], nd[:], AF.Exp, scale=-1.0)
        decT = p.rot("g_decT", [64, 64], F32, D1)
        p.act(decT[:], d2[:], AF.Exp)
        egc = p.rot("g_egc", [64, 1], F32, D1)
        p.act(egc[:], gcol[:], AF.Exp)
        dlast = p.rot("g_dlast", [64, 1], F32, D1)
        p.act(dlast[:], gcol[:], AF.Exp, bias=glast[0:64, 0:1], scale=-1.0)
        cdec = p.rot("g_cdec", [128, 1], F32, D2)
        p.act(cdec[:], glast[:], AF.Exp)
        ctx["cdec"] = cdec
        yield
        decTs = p.rot("g_decTs", [64, 64], F32, D1)
        p.tt(decTs[:], decT[:], st01[:], ALU.mult)
        k_n = p.rot("g_kn", [64, 128], F32, D1)
        p.ts(k_n[:], ptk, rinv[:, 1:2], ALU.mult)
        q_n = p.rot("g_qn", [64, 128], F32, D1)
        p.ts(q_n[:], ptq, rinv[:, 0:1], ALU.mult, RSQ, ALU.mult)
        R = p.rot("g_R0", [64, 192], F32, D1)
        p.ts(R[:, 0:64], ptv, b_c, ALU.mult)
        yield
        kb = p.rot("g_kb", [64, 128], F32, D1)
        p.ts(kb[:], k_n[:], b_c, ALU.mult)
        qd = p.rot("g_qd", [64, 128], F32, D1)
        p.ts(qd[:], q_n[:], egc[:, 0:1], ALU.mult)
        kd = p.rot("g_kd", [64, 128], F32, D2)
        p.ts(kd[:], k_n[:], dlast[:, 0:1], ALU.mult)
        ctx["kd"] = kd
        yield
        p.ts(R[:, 64:192], kb[:], egc[:, 0:1], ALU.mult)
        p.tr(PB[:, 0:64], k_n[:], ident[0:64, 0:64])
        p.tr(PB[:, 64:128], kb[:], ident[0:64, 0:64])
        p.tr(PB[:, 128:192], q_n[:], ident[0:64, 0:64])
        p.tr(PB[:, 192:256], qd[:], ident[0:64, 0:64])
        yield
        fT = p.rot("g_fT", [128, 256], F32, D2)
        p.copy(fT[:], PB[:, 0:256])
        knT, kbT, qnT, qdT = fT[:, 0:64], fT[:, 64:128], fT[:, 128:192], fT[:, 192:256]
        ctx["qdT"] = qdT
        yield
        pG0, pG1, pG2 = PB[0:64, 256:320], PB[0:64, 320:384], PB[0:64, 384:448]
        p.mm(pG0, kbT, knT)
        p.mm(pG1, knT, kbT)
        p.mm(pG2, knT, qnT)
        yield
        A = p.rot("g_A", [64, 64], F32, 2 * NB)
        p.tt(A[:], pG0, dec_s[:], ALU.mult)
        At = p.rot("g_At", [64, 64], F32, 2 * NB)
        p.tt(At[:], pG1, decTs[:], ALU.mult)
        attnT = p.rot("g_attnT", [64, 64], F32, D2)
        p.tt(attnT[:], pG2, decT[:], ALU.mult)
        ctx["attnT"] = attnT
        yield
        for lvl in range(6):
            pR = PB[0:64, 0:192]
            p.mm(pR, At[:], R[:])
            if lvl < 5:
                pP0, pP1 = PB[0:64, 192:256], PB[0:64, 256:320]
                p.mm(pP0, At[:], A[:])
                p.mm(pP1, A[:], At[:])
            yield
            Rn = p.rot("g_Rf", [64, 192], F32, D2) if lvl == 5 else p.rot("g_Ri", [64, 192], F32, 2 * NB)
            if lvl == 0:
                p.tt(Rn[:], R[:], pR, ALU.subtract)
            else:
                p.tt(Rn[:], R[:], pR, ALU.add)
            R = Rn
            if lvl < 5:
                A2 = p.rot("g_A", [64, 64], F32, 2 * NB)
                At2 = p.rot("g_At", [64, 64], F32, 2 * NB)
                p.copy(A2[:], pP0)
                p.copy(At2[:], pP1)
                A, At = A2, At2
            yield
        ctx["R"] = R
        pW = PB[:, 320:384]
        p.tr(pW, R[:, 64:192], ident[0:64, 0:64])
        yield
        wT = p.rot("g_wT", [128, 64], F32, D2)
        p.copy(wT[:], pW)
        ctx["wT"] = wT
        yield

    def scan(c, ctx):
        PB = ctx["PB"]
        R = ctx["R"]
        pv = PB[0:64, 384:448]
        p.mm(pv, ctx["wT"][:], state[:])
        vnew = p.rot("g_vnew", [64, 64], F32, 3)
        p.tt(vnew[:], R[:, 0:64], pv, ALU.subtract)
        po = PB[0:64, 448:512]
        p.mm(po, ctx["qdT"], state[:], start=True, stop=False)
        p.mm(po, ctx["attnT"][:], vnew[:], start=False, stop=True)
        pS = PB[:, 192:256]
        p.mm(pS, ctx["kd"][:], vnew[:])
        p.stt(state[:], state[:], ctx["cdec"][:, 0:1], pS, ALU.mult, ALU.add)
        if c % OB == 0:
            oo_box[0] = p.rot("g_oo", [64, OB, 64], F32, 2)
        oo = oo_box[0]
        p.copy(oo[:, c % OB, :], po)
        if c % OB == OB - 1:
            c0 = (c - OB + 1) * 64
            p.dma(out.v(out.h[c0:c0 + OB * 64, :].rearrange("(b p) d -> p b d", p=64)), oo[:])

    import os
    MAXST = int(os.environ.get('GDN_MAXST', '999'))
    pending = []
    for sc in range(S // CH):
        qTs = conv(qT_d, 128, wq, sc * CH, "gq")
        kTs = conv(kT_d, 128, wk, sc * CH, "gk")
        vTs = conv(vT_d, 64, wv, sc * CH, "gv")
        for b0 in range(0, CPS, NB):
            ctxs = [dict() for _ in range(NB)]
            gens = [pre(sc * CPS + b0 + i, b0 + i, qTs, kTs, vTs, ctxs[i]) for i in range(NB)]
            stage = 0
            alive = True
            while alive:
                alive = False
                for g in gens:
                    try:
                        next(g)
                        alive = True
                    except StopIteration:
                        pass
                stage += 1
                if stage >= MAXST:
                    break
                if pending and stage % 4 == 0:
                    cc, cx = pending.pop(0)
                    scan(cc, cx)
            while pending:
                cc, cx = pending.pop(0)
                scan(cc, cx)
            if MAXST < 99:
                continue
            pending = [(sc * CPS + b0 + i, ctxs[i]) for i in range(NB)]
    while pending:
        cc, cx = pending.pop(0)
        scan(cc, cx)
    p.finish([out])
    return nc, p


def load_w_bf16(p, w_d, rows, cols, dst, r0=0):
    for k in range(rows // 128):
        p.dma(dst[:, k, 0:cols], w_d[r0 + k * 128:r0 + (k + 1) * 128, 0:cols], q="pool")


def build_k3a(ntiles=NT):
    nc = bass.Bass("TRN2", target_bir_lowering=False)
    p = P(nc)
    ntok = ntiles * 128
    di = lambda n, s: p.dram(n, s, F32, kind="ExternalInput")
    x = di("x", [ntok, D]); z_d = di("z", [ntok, 512]); gg_d = di("ggate", [ntok, 512]); brg_d = di("brg", [ntok, 3072])
    ya_d = di("ya", [ntok, 512]); yb_d = di("yb", [ntok, 512]); yc_d = di("yc", [ntok, 512])
    c_in = di("c_col", [128, 8]); wada = di("wada", [D, 4 * D]); bada = di("bada", [1, 4 * D])
    ssmg_d = di("ssm_g", [1, 512]); gdng_d = di("gdn_g", [1, 128]); ffng_d = di("ffn_g", [1, D])
    wbr_d = di("w_branch", [1536, D]); wout_d = di("w_out", [D, D]); wgr_d = di("wgr", [D, 36]); bgr_d = di("bgr", [1, 36])
    ident_d = di("ident_in", [128, 128])
    x1_o = p.dram("x1", [ntok, D], F32, kind="ExternalOutput")
    h2_o = p.dram("h2", [ntok, D], F32, kind="ExternalOutput")
    wt_o = p.dram("wt", [ntok, 32], F32, kind="ExternalOutput")
    gf_o = p.dram("gf", [128, D], F32, kind="ExternalOutput")

    ident = p.sb([128, 128], F32, "ident")
    p.dma(ident[:], ident_d[:])
    identb = p.sb([128, 128], BF16, "identb")
    p.copy(identb[:], ident[:])
    ones_f = p.sb([128, 128], F32, "ones_f")
    p.memset(ones_f[:], 1.0)
    c_bc = emit_cbc(p, c_in, ones_f)
    gate_m = p.sb([128, D], F32, "gate_m"); shift_f = p.sb([128, D], F32, "shift_f")
    gmod_f = p.sb([128, D], F32, "gmod_f"); gate_f = p.sb([128, D], F32, "gate_f")
    emit_mod(p, c_bc, wada, bada, 0, D, gate_m, ones_f)
    emit_mod(p, c_bc, wada, bada, D, D, shift_f, ones_f)
    emit_mod(p, c_bc, wada, bada, 2 * D, D, gmod_f, ones_f, plus_one=True)
    emit_mod(p, c_bc, wada, bada, 3 * D, D, gate_f, ones_f)
    p.dma(gf_o[:], gate_f[:])
    ffng = p.sb([128, D], F32, "ffng")
    p.dma(ffng[:], bcast_row(ffng_d, D))
    p.tt(gmod_f[:], gmod_f[:], ffng[:], ALU.mult)
    ssmg = p.sb([128, 512], F32, "ssmg")
    p.dma(ssmg[:], bcast_row(ssmg_d, 512))
    gdng = p.sb([128, 128], F32, "gdng")
    p.dma(gdng[:], bcast_row(gdng_d, 128))
    wgr = p.sb([128, 8, 36], F32, "wgr")
    p.dma(wgr[:], wgr_d.v(wgr_d.h[:, :].rearrange("(k p) n -> p k n", p=128)))
    bgr = p.sb([1, 36], F32, "bgr")
    p.dma(bgr[:], bgr_d[:])
    Wbr = p.sb([128, 12, D], BF16, "Wbr")
    load_w_bf16(p, wbr_d, 1536, D, Wbr)
    Wout = p.sb([128, 8, D], BF16, "Wout")
    load_w_bf16(p, wout_d, D, D, Wout)

    for t in range(ntiles):
        rs = slice(t * 128, (t + 1) * 128)
        xt = p.rot("xt", [128, D], F32, 2)
        p.dma(xt[:], x[rs, :])
        zt = p.rot("zt", [128, 512], F32, 2); p.dma(zt[:], z_d[rs, :], q="pool")
        gt = p.rot("gt", [128, 512], F32, 2); p.dma(gt[:], gg_d[rs, :])
        bt = p.rot("bt", [128, 3072], F32, 1); p.dma(bt[:], brg_d[rs, :], q="pool")
        ya = p.rot("ya", [128, 512], F32, 2); p.dma(ya[:], ya_d[rs, :])
        yb = p.rot("yb", [128, 512], F32, 2); p.dma(yb[:], yb_d[rs, :], q="pool")
        yc = p.rot("yc", [128, 512], F32, 2); p.dma(yc[:], yc_d[rs, :])
        brn = p.rot("brn", [128, 1536], BF16, 2)
        p.act(zt[:], zt[:], AF.Silu)
        p.tt(ya[:], ya[:], zt[:], ALU.mult)
        junk = p.rot("junk", [128, 512], F32, 1)
        ssq = p.rot("ssq", [128, 8], F32, 2)
        for g in range(2):
            p.act(junk[:, 0:256], ya[:, g * 256:(g + 1) * 256], AF.Square, accum=ssq[:, g:g + 1])
        for hd in range(4):
            p.act(junk[:, 0:128], yc[:, hd * 128:(hd + 1) * 128], AF.Square, accum=ssq[:, 2 + hd:3 + hd])
        rstd = p.rot("rstd", [128, 8], F32, 2)
        p.act(rstd[:, 0:2], ssq[:, 0:2], AF.Sqrt, bias=EPS, scale=1.0 / 256)
        p.act(rstd[:, 2:6], ssq[:, 2:6], AF.Sqrt, bias=EPS, scale=1.0 / 128)
        p.recip(rstd[:, 0:6], rstd[:, 0:6])
        for g in range(2):
            sl = slice(g * 256, (g + 1) * 256)
            p.stt(brn[:, sl], ya[:, sl], rstd[:, g:g + 1], ssmg[:, sl], ALU.mult, ALU.mult)
        p.copy(brn[:, 512:1024], yb[:], e="pool")
        p.act(gt[:], gt[:], AF.Silu)
        for hd in range(4):
            sl = slice(hd * 128, (hd + 1) * 128)
            p.stt(yc[:, sl], yc[:, sl], rstd[:, 2 + hd:3 + hd], gdng[:], ALU.mult, ALU.mult)
        p.tt(brn[:, 1024:1536], yc[:], gt[:], ALU.mult)
        brT = p.rot("brT", [128, 12, 128], BF16, 2)
        for q4 in range(3):
            pt = p.rot("psT", [128, 4, 128], BF16, 2, psum=True)
            for i in range(4):
                kk = q4 * 4 + i
                p.tr(pt[:, i, :], brn[:, kk * 128:(kk + 1) * 128], identb[:])
            p.copy(brT[:, q4 * 4:q4 * 4 + 4, :], pt[:])
        p.act(bt[:], bt[:], AF.Sigmoid)
        merged = p.rot("merged", [128, D], F32, 1)
        for nb in range(2):
            cs = slice(nb * 512, (nb + 1) * 512)
            for i in range(3):
                ps = p.rot("ps_mm", [128, 512], F32, 4, psum=True)
                for k in range(4):
                    p.mm(ps[:], brT[:, 4 * i + k, :], Wbr[:, 4 * i + k, cs], start=(k == 0), stop=(k == 3))
                if i == 0:
                    p.tt(merged[:, cs], ps[:], bt[:, i * D + nb * 512: i * D + (nb + 1) * 512], ALU.mult)
                else:
                    tmp = p.rot("mtmp", [128, 512], F32, 2)
                    p.tt(tmp[:], ps[:], bt[:, i * D + nb * 512: i * D + (nb + 1) * 512], ALU.mult)
                    p.tt(merged[:, cs], merged[:, cs], tmp[:], ALU.add)
        mb = p.rot("mb", [128, D], BF16, 1)
        p.copy(mb[:], merged[:], e="pool")
        mT = p.rot("mT", [128, 8, 128], BF16, 1)
        for q4 in range(2):
            pt = p.rot("psT", [128, 4, 128], BF16, 2, psum=True)
            for i in range(4):
                kk = q4 * 4 + i
                p.tr(pt[:, i, :], mb[:, kk * 128:(kk + 1) * 128], identb[:])
            p.copy(mT[:, q4 * 4:q4 * 4 + 4, :], pt[:])
        x1 = p.rot("x1", [128, D], F32, 2)
        for nb in range(2):
            cs = slice(nb * 512, (nb + 1) * 512)
            ps = p.rot("ps_mm", [128, 512], F32, 4, psum=True)
            for k in range(8):
                p.mm(ps[:], mT[:, k, :], Wout[:, k, cs], start=(k == 0), stop=(k == 7))
            tmp = p.rot("mtmp", [128, 512], F32, 2)
            p.tt(tmp[:], ps[:], gate_m[:, cs], ALU.mult)
            p.tt(x1[:, cs], tmp[:], xt[:, cs], ALU.add)
        p.dma(x1_o[rs, :], x1[:])
        h2 = p.rot("h2", [128, D], F32, 2)
        emit_rmsnorm_mod(p, x1[:], gmod_f[:], shift_f[:], h2[:])
        p.dma(h2_o[rs, :], h2[:], q="pool")
        h2T = p.rot("h2Tf", [128, 8, 128], F32, 1)
        for q4 in range(2):
            pt = p.rot("psTf", [128, 4, 128], F32, 1, psum=True)
            for i in range(4):
                kk = q4 * 4 + i
                p.tr(pt[:, i, :], h2[:, kk * 128:(kk + 1) * 128], ident[:])
            p.copy(h2T[:, q4 * 4:q4 * 4 + 4, :], pt[:])
        pr = p.rot("ps_r", [128, 36], F32, 1, psum=True)
        for k in range(8):
            p.mm(pr[:], h2T[:, k, :], wgr[:, k, :], start=(k == 0), stop=False)
        p.mm(pr[:], ones_f[0:1, :], bgr[0:1, :], start=False, stop=True)
        lg = p.rot("lg", [128, 36], F32, 2)
        p.copy(lg[:], pr[:])
        sm = p.rot("rt_sm", [128, 16], F32, 2)
        gmax, ngmax, sg, pgrp, m1, m2, e2, rden, w1, w2 = [sm[:, i:i + 1] for i in range(10)]
        p.reduce(gmax, lg[:, 0:4], op=ALU.max)
        oh = p.rot("rt_oh", [128, 4], F32, 2)
        p.ts(oh[:], lg[:, 0:4], gmax, ALU.is_equal)
        p.ts(ngmax, gmax, -1.0, ALU.mult)
        eg = p.rot("rt_eg", [128, 4], F32, 2)
        p.act(eg[:], lg[:, 0:4], AF.Exp, bias=ngmax)
        p.reduce(sg, eg[:])
        p.recip(pgrp, sg)
        el = p.rot("rt_el", [128, 8], F32, 2)
        p.ts(el[:], lg[:, 4:12], oh[:, 0:1], ALU.mult)
        for g in range(1, 4):
            p.stt(el[:], lg[:, 4 + 8 * g:12 + 8 * g], oh[:, g:g + 1], el[:], ALU.mult, ALU.add)
        p.reduce(m1, el[:], op=ALU.max)
        mk1 = p.rot("rt_mk1", [128, 8], F32, 2)
        p.ts(mk1[:], el[:], m1, ALU.is_equal)
        el2 = p.rot("rt_el2", [128, 8], F32, 2)
        p.ts(el2[:], mk1[:], -1e30, ALU.mult)
        p.tt(el2[:], el2[:], el[:], ALU.add)
        p.reduce(m2, el2[:], op=ALU.max)
        mk2 = p.rot("rt_mk2", [128, 8], F32, 2)
        p.ts(mk2[:], el2[:], m2, ALU.is_equal)
        p.tt(e2, m2, m1, ALU.subtract)
        p.act(e2, e2, AF.Exp)
        p.ts(rden, e2, 1.0, ALU.add)
        p.recip(rden, rden)
        p.tt(w1, rden, pgrp, ALU.mult)
        p.tt(w2, w1, e2, ALU.mult)
        wexp = p.rot("rt_wexp", [128, 8], F32, 2)
        p.ts(wexp[:], mk1[:], w1, ALU.mult)
        p.stt(wexp[:], mk2[:], w2, wexp[:], ALU.mult, ALU.add)
        wt = p.rot("rt_wt", [128, 32], F32, 2)
        for g in range(4):
            p.ts(wt[:, 8 * g:8 * g + 8], wexp[:], oh[:, g:g + 1], ALU.mult)
        p.dma(wt_o[rs, :], wt[:])
    p.finish([x1_o, h2_o, wt_o, gf_o])
    return nc, p


def build_k3b(ntiles=NT, nexp=32):
    nc = bass.Bass("TRN2", target_bir_lowering=False)
    p = P(nc)
    ntok = ntiles * 128
    di = lambda n, s: p.dram(n, s, F32, kind="ExternalInput")
    x1_d = di("x1", [ntok, D]); h2_d = di("h2", [ntok, D]); wt_d = di("wt", [ntok, 32]); gf_d = di("gf", [128, D])
    wg_d = di("w_gate", [nexp * D, 512]); wu_d = di("w_up", [nexp * D, 512]); wd_d = di("w_down", [nexp * 512, D])
    ident_d = di("ident_in", [128, 128])
    xo = p.dram("xo", [ntok, D], F32, kind="ExternalOutput")
    ident = p.sb([128, 128], F32, "ident")
    p.dma(ident[:], ident_d[:])
    identb = p.sb([128, 128], BF16, "identb")
    p.copy(identb[:], ident[:])
    gf = p.sb([128, D], F32, "gf")
    p.dma(gf[:], gf_d[:])
    x1 = p.sb([128, ntiles, D], F32, "x1")
    h2T = p.sb([128, ntiles, 8, 128], BF16, "h2T")
    wt = p.sb([128, ntiles, 32], F32, "wt")
    for t in range(ntiles):
        rs = slice(t * 128, (t + 1) * 128)
        p.dma(x1[:, t, :], x1_d[rs, :])
        p.dma(wt[:, t, :], wt_d[rs, :], q="pool")
        ht = p.rot("ht", [128, D], F32, 2)
        p.dma(ht[:], h2_d[rs, :], q="pool")
        hb = p.rot("hb", [128, D], BF16, 2)
        p.copy(hb[:], ht[:])
        for q4 in range(2):
            pt = p.rot("psT", [128, 4, 128], BF16, 2, psum=True)
            for i in range(4):
                kk = q4 * 4 + i
                p.tr(pt[:, i, :], hb[:, kk * 128:(kk + 1) * 128], identb[:])
            p.copy(h2T[:, t, q4 * 4:q4 * 4 + 4, :], pt[:])
    def load_expert(e):
        Wg = p.rot("Wg", [128, 8, 512], BF16, 2)
        Wu = p.rot("Wu", [128, 8, 512], BF16, 2)
        Wd = p.rot("Wd", [128, 4, D], BF16, 2)
        for k in range(8):
            p.dma(Wg[:, k, :], wg_d[e * D + k * 128:e * D + (k + 1) * 128, :], q="pool")
            p.dma(Wu[:, k, :], wu_d[e * D + k * 128:e * D + (k + 1) * 128, :], q="pool")
        return Wg, Wu, Wd

    def load_wd_piece(e, Wd, k):
        st = p.rot("wstage", [128, 1024], F32, 4)
        p.dma(st[:], wd_d[e * 512 + k * 128:e * 512 + (k + 1) * 128, :])
        p.tt(Wd[:, k, :], st[:], gf[:], ALU.mult)

    nxt = load_expert(0)
    for k in range(4):
        load_wd_piece(0, nxt[2], k)
    for e in range(nexp):
        Wg, Wu, Wd = nxt
        stb = {}

        def partA(t):
            pg = p.rot("ps_mm", [128, 512], F32, 4, psum=True)
            pu = p.rot("ps_mm", [128, 512], F32, 4, psum=True)
            for k in range(8):
                p.mm(pg[:], h2T[:, t, k, :], Wg[:, k, :], start=(k == 0), stop=(k == 7))
                p.mm(pu[:], h2T[:, t, k, :], Wu[:, k, :], start=(k == 0), stop=(k == 7))
            sg = p.rot("sg", [128, 512], F32, 2)
            p.act(sg[:], pg[:], AF.Silu)
            hid = p.rot("hid", [128, 512], BF16, 3)
            p.stt(hid[:], sg[:], wt[:, t, e:e + 1], pu[:], ALU.mult, ALU.mult)
            stb[t] = hid

        def partB(t):
            hid = stb.pop(t)
            pt = p.rot("psT", [128, 4, 128], BF16, 2, psum=True)
            for i in range(4):
                p.tr(pt[:, i, :], hid[:, i * 128:(i + 1) * 128], identb[:])
            hT = p.rot("hidT", [128, 4, 128], BF16, 3)
            p.copy(hT[:], pt[:])
            stb[("T", t)] = hT

        def partC(t):
            hT = stb.pop(("T", t))
            for nb in range(2):
                cs = slice(nb * 512, (nb + 1) * 512)
                py = p.rot("ps_py", [128, 512], F32, 2, psum=True)
                for k in range(4):
                    p.mm(py[:], hT[:, k, :], Wd[:, k, cs], start=(k == 0), stop=(k == 3))
                p.tt(x1[:, t, cs], x1[:, t, cs], py[:], ALU.add)

        for step in range(ntiles + 2):
            if step == 0 and e + 1 < nexp:
                nxt = load_expert(e + 1)
            if e + 1 < nexp and ntiles >= 8 and 2 <= step < 6:
                load_wd_piece(e + 1, nxt[2], step - 2)
            if e + 1 < nexp and ntiles < 8 and step == 0:
                for k in range(4):
                    load_wd_piece(e + 1, nxt[2], k)
            if step < ntiles:
                partA(step)
            if 0 <= step - 1 < ntiles:
                partB(step - 1)
            if 0 <= step - 2 < ntiles:
                partC(step - 2)
    for t in range(ntiles):
        p.dma(xo[t * 128:(t + 1) * 128, :], x1[:, t, :], q=("sp" if t % 2 == 0 else "pool"))
    p.finish([xo])
    return nc, p


_PROGS = {}


def _prog(name, fn):
    if name not in _PROGS:
        _PROGS[name] = fn()[0]
    return _PROGS[name]


def _run(nc, in_maps):
    in_maps = [{k: np.ascontiguousarray(v, dtype=np.float32) for k, v in m.items()} for m in in_maps]
    return run_bass_kernel_spmd(nc, in_maps, core_ids=list(range(NCORE))).results


def _padT(a):
    return np.ascontiguousarray(np.concatenate([np.zeros((a.shape[1], 3), np.float32), a.T], 1))


def _sb_masks(j):
    kk = (np.arange(8)[:, None, None] * 128 + np.arange(128)[None, :, None])
    qq = j * 512 + np.arange(512)[None, None, :]
    return np.ascontiguousarray((kk < qq).astype(np.float32).transpose(1, 0, 2))


def kernel(x, c, w_ada, b_ada, norm_mix, norm_ffn, w_in, ssm_conv_w, ssm_conv_b, ssm_dt_bias, ssm_a_log, ssm_d,
           ssm_norm, sb_q_norm, sb_k_norm, gdn_conv_w, gdn_a_log, gdn_dt_bias, gdn_norm, w_branch, w_out,
           w_group, b_group, w_router, b_router, w_gate, w_up, w_down):
    f32 = lambda a: np.asarray(a, dtype=np.float32)
    x = f32(x)[0]
    c_col = np.ascontiguousarray(f32(c)[0].reshape(8, 128).T)
    I = np.eye(128, dtype=np.float32)
    i128 = np.arange(128)
    i64 = np.arange(64)
    triu128 = (i128[:, None] <= i128[None, :]).astype(np.float32)
    mneg128 = np.where(i128[:, None] <= i128[None, :], 0.0, -30000.0).astype(np.float32)
    mincl = (i128[:, None] >= i128[None, :]).astype(np.float32)
    triu64 = (i64[:, None] <= i64[None, :]).astype(np.float32)
    mpos64 = np.where(i64[:, None] > i64[None, :], 0.0, 30000.0).astype(np.float32)
    mneg64 = np.where(i64[:, None] <= i64[None, :], 0.0, -30000.0).astype(np.float32)
    st01 = (i64[:, None] < i64[None, :]).astype(np.float32)
    sbm = [_sb_masks(0), _sb_masks(1)]
    k1 = _prog("k1", build_k1)
    kssd = _prog("ssd", build_ssd)
    ksb = _prog("sb", build_sb)
    kgdn = _prog("gdn", build_gdn)
    k3a = _prog("k3a", build_k3a)
    k3b = _prog("k3b", build_k3b)
    for l in range(4):
        wa = f32(w_ada[l]); ba = f32(b_ada[l])
        r = _run(k1, [{"x": x[TOK * i:TOK * (i + 1)], "c_col": c_col, "wada": wa[:, 0:2 * D], "bada": ba[None, 0:2 * D],
                       "norm_g": f32(norm_mix[l])[None], "w_in": f32(w_in[l]), "ident_in": I} for i in range(NCORE)])
        proj = np.concatenate([r[i]["proj"] for i in range(NCORE)], 0)
        m_z = proj[:, 0:512]; m_xbc = proj[:, 512:1536]; m_dt = proj[:, 1536:1544]
        sbq = proj[:, 1544:3080]; gq = proj[:, 3080:4616]; g_a = proj[:, 4616:4620]; g_b = proj[:, 4620:4624]
        g_gate = proj[:, 4624:5136]; br_gate = proj[:, 5136:8208]
        cw = f32(ssm_conv_w[l]); cb = f32(ssm_conv_b[l])
        ims = []
        for i in range(NCORE):
            g = i // 4
            xs = slice(64 * i, 64 * i + 64); bs = slice(512 + 128 * g, 640 + 128 * g); cs = slice(768 + 128 * g, 896 + 128 * g)
            ims.append({"xT": _padT(m_xbc[:, xs]), "BT": _padT(m_xbc[:, bs]), "CT": _padT(m_xbc[:, cs]),
                        "wx": cw[:, xs].T, "bx": cb[xs, None], "wB": cw[:, bs].T, "bB": cb[bs, None], "wC": cw[:, cs].T, "bC": cb[cs, None],
                        "dt_col": m_dt[:, i].reshape(-1, 128).T,
                        "scal": np.tile(np.array([[f32(ssm_dt_bias[l])[i], f32(ssm_a_log[l])[i], f32(ssm_d[l])[i]]], np.float32), (128, 1)),
                        "triu": triu128, "mneg": mneg128, "ident_in": I})
        r = _run(kssd, ims)
        y_ssd = np.concatenate([r[i]["y"] for i in range(NCORE)], 1)
        ims = []
        for i in range(NCORE):
            h, j = i // 2, i % 2
            q = sbq[:, 128 * h:128 * h + 128]
            qsel = np.concatenate([q[(2 * m + j) * 512:(2 * m + j + 1) * 512] for m in range(16)], 0)
            ims.append({"q": qsel, "k": sbq[:, 512 + 128 * h:640 + 128 * h], "v": sbq[:, 1024 + 128 * h:1152 + 128 * h],
                        "qg": f32(sb_q_norm[l])[None], "kg": f32(sb_k_norm[l])[None], "mask": sbm[j], "mincl": mincl, "ident_in": I})
        r = _run(ksb, ims)
        y_sb = np.zeros((SEQ, 512), np.float32)
        for i in range(NCORE):
            h, j = i // 2, i % 2
            oT = r[i]["oT"]
            for m in range(16):
                g = 2 * m + j
                y_sb[g * 512:(g + 1) * 512, 128 * h:128 * h + 128] = oT[:, m * 512:(m + 1) * 512].T
        gw = f32(gdn_conv_w[l])
        ims = []
        for i in range(NCORE):
            h, e = i // 2, i % 2
            qs = slice(128 * h, 128 * h + 128); ks = slice(512 + 128 * h, 640 + 128 * h); vs = slice(1024 + 128 * h + 64 * e, 1024 + 128 * h + 64 * e + 64)
            ims.append({"qT": _padT(gq[:, qs]), "kT": _padT(gq[:, ks]), "vT": _padT(gq[:, vs]),
                        "wq": gw[:, qs].T, "wk": gw[:, ks].T, "wv": gw[:, vs].T,
                        "a_col": g_a[:, h].reshape(-1, 64).T, "b_col": g_b[:, h].reshape(-1, 64).T,
                        "scal": np.tile(np.array([[f32(gdn_a_log[l])[h], f32(gdn_dt_bias[l])[h]]], np.float32), (128, 1)),
                        "triu": triu64, "mpos": mpos64, "mneg": mneg64, "st01": st01, "ident_in": I})
        r = _run(kgdn, ims)
        o_gdn = np.zeros((SEQ, 512), np.float32)
        for i in range(NCORE):
            h, e = i // 2, i % 2
            o_gdn[:, 128 * h + 64 * e:128 * h + 64 * e + 64] = r[i]["o"]
        wgr = np.concatenate([f32(w_group[l]), f32(w_router[l])], 1)
        bgr = np.concatenate([f32(b_group[l]), f32(b_router[l])])[None]
        ims = []
        for i in range(NCORE):
            ts_ = slice(TOK * i, TOK * (i + 1))
            ims.append({"x": x[ts_], "z": m_z[ts_], "ggate": g_gate[ts_], "brg": br_gate[ts_], "ya": y_ssd[ts_], "yb": y_sb[ts_], "yc": o_gdn[ts_],
                        "c_col": c_col, "wada": wa[:, 2 * D:6 * D], "bada": ba[None, 2 * D:6 * D],
                        "ssm_g": f32(ssm_norm[l])[None], "gdn_g": f32(gdn_norm[l])[None], "ffn_g": f32(norm_ffn[l])[None],
                        "w_branch": f32(w_branch[l]).reshape(1536, D), "w_out": f32(w_out[l]), "wgr": wgr, "bgr": bgr, "ident_in": I})
        ra = _run(k3a, ims)
        wg = f32(w_gate[l]).reshape(-1, 512); wu = f32(w_up[l]).reshape(-1, 512); wd = f32(w_down[l]).reshape(-1, D)
        ims = [{"x1": ra[i]["x1"], "h2": ra[i]["h2"], "wt": ra[i]["wt"], "gf": ra[i]["gf"], "w_gate": wg, "w_up": wu, "w_down": wd, "ident_in": I}
               for i in range(NCORE)]
        rb = _run(k3b, ims)
        x = np.concatenate([rb[i]["xo"] for i in range(NCORE)], 0)
    return x[None].astype(np.float32)
```

```python
import numpy as np
import concourse.bass as bass
import concourse.mybir as mybir
from concourse.bass_utils import run_bass_kernel_spmd

F32 = mybir.dt.float32
BF16 = mybir.dt.bfloat16
I32 = mybir.dt.int32
U32 = mybir.dt.uint32
AF = mybir.ActivationFunctionType
ALU = mybir.AluOpType
AX = mybir.AxisListType

NDS = 12


class Buf:
    __slots__ = ("name", "w", "r")

    def __init__(self, name):
        self.name = name
        self.w = None
        self.r = {}


class V:
    __slots__ = ("ap", "bufs")

    def __init__(self, ap, bufs):
        self.ap = ap
        self.bufs = bufs


class T:
    def __init__(self, h, name, nbuf_axis=None, nbuf=1):
        self.h = h
        self.name = name
        self.buf = Buf(name)

    def __getitem__(self, idx):
        return V(self.h[idx], (self.buf,))

    def v(self, ap):
        return V(ap, (self.buf,))


class P:
    def __init__(self, nc, same_eng_sync=True):
        self.nc = nc
        self.engs = {"pe": nc.tensor, "dve": nc.vector, "act": nc.scalar, "pool": nc.gpsimd, "sp": nc.sync}
        self.sem = {k: nc.alloc_semaphore(name=f"s_{k}") for k in self.engs}
        self.cnt = {k: 0 for k in self.engs}
        self.seen = {k: {} for k in self.engs}
        self.dsem = {}
        self.dcnt = {}
        self.dnext = {}
        for q in ("sp", "pool", "act"):
            self.dsem[q] = [nc.alloc_semaphore(name=f"d_{q}{i}") for i in range(NDS)]
            self.dcnt[q] = [0] * NDS
            self.dnext[q] = 0
        self.same_eng_sync = same_eng_sync
        self.n_wait = 0
        self.n_ins = 0
        self._n = 0

    def sb(self, shape, dt=F32, name=None):
        self._n += 1
        name = name or f"sb{self._n}"
        h = self.nc.alloc_sbuf_tensor("S_" + name, list(shape), dt)
        return T(h, name)

    def ps(self, shape, dt=F32, name=None):
        self._n += 1
        name = name or f"ps{self._n}"
        h = self.nc.alloc_psum_tensor("P_" + name, list(shape), dt)
        return T(h, name)

    def dram(self, name, shape, dt=F32, kind="Internal"):
        h = self.nc.dram_tensor(name, list(shape), dt, kind=kind)
        return T(h, name)

    def rot(self, key, shape, dt, n, psum=False):
        if not hasattr(self, "_rot"):
            self._rot = {}
        if key not in self._rot:
            tiles = [(self.ps if psum else self.sb)(shape, dt, f"{key}_{i}") for i in range(n)]
            self._rot[key] = [tiles, 0]
        ent = self._rot[key]
        t = ent[0][ent[1] % len(ent[0])]
        ent[1] += 1
        return t

    def _semof(self, src):
        if src[0] == "e":
            return self.sem[src[1]]
        return self.dsem[src[1]][src[2]]

    def _wait(self, e, deps):
        best = {}
        for d in deps:
            if d is None:
                continue
            key = d[:-1]
            if key not in best or best[key] < d[-1]:
                best[key] = d[-1]
        for key, c in best.items():
            if key[0] == "e" and key[1] == e and not (self.same_eng_sync and e not in ("pe",)):
                continue
            if self.seen[e].get(key, 0) >= c:
                continue
            self.seen[e][key] = c
            self.engs[e].wait_ge(self._semof(key), c)
            self.n_wait += 1

    def _deps(self, reads, writes):
        deps = set()
        for v in reads:
            for b in v.bufs:
                if b.w is not None:
                    deps.add(b.w)
        for v in writes:
            for b in v.bufs:
                if b.w is not None:
                    deps.add(b.w)
                deps.update(b.r.values())
        return deps

    def _commit(self, tok, reads, writes):
        for v in writes:
            for b in v.bufs:
                b.w = tok
                b.r = {}
        wb = set()
        for v in writes:
            wb.update(id(b) for b in v.bufs)
        for v in reads:
            for b in v.bufs:
                if id(b) not in wb:
                    b.r[tok[:-1]] = tok

    def op(self, e, fn, reads=(), writes=()):
        self._wait(e, self._deps(reads, writes))
        ins = fn(self.engs[e])
        self.cnt[e] += 1
        ins.then_inc(self.sem[e], 1)
        self.n_ins += 1
        self._commit(("e", e, self.cnt[e]), reads, writes)
        return ins

    def dma(self, out, in_, q="sp", **kw):
        i = self.dnext[q]
        self.dnext[q] = (i + 1) % NDS
        deps = self._deps([in_], [out])
        if self.dcnt[q][i] > 0:
            deps.add(("d", q, i, 16 * self.dcnt[q][i]))
        self._wait(q, deps)
        ins = self.engs[q].dma_start(out=out.ap, in_=in_.ap, **kw)
        self.dcnt[q][i] += 1
        ins.then_inc(self.dsem[q][i], 16)
        self.n_ins += 1
        self._commit(("d", q, i, 16 * self.dcnt[q][i]), [in_], [out])
        return ins

    def finish(self, outs, e="sp"):
        deps = set()
        for t in outs:
            if t.buf.w is not None:
                deps.add(t.buf.w)
        for k in self.engs:
            if self.cnt[k] > 0:
                deps.add(("e", k, self.cnt[k]))
        for q in self.dsem:
            for i in range(NDS):
                if self.dcnt[q][i] > 0:
                    deps.add(("d", q, i, 16 * self.dcnt[q][i]))
        self._wait(e, deps)

    def mm(self, out, lhsT, rhs, start=True, stop=True):
        rd = [lhsT, rhs]
        return self.op("pe", lambda e: e.matmul(out.ap, lhsT.ap, rhs.ap, start=start, stop=stop), rd, [out])

    def tr(self, out, in_, ident):
        return self.op("pe", lambda e: e.transpose(out.ap, in_.ap, ident.ap), [in_, ident], [out])

    def act(self, out, in_, func, bias=None, scale=None, accum=None, e="act"):
        kw = {}
        rd = [in_]
        wr = [out]
        if bias is not None:
            if isinstance(bias, V):
                kw["bias"] = bias.ap
                rd.append(bias)
            else:
                kw["bias"] = bias
        if scale is not None:
            if isinstance(scale, V):
                kw["scale"] = scale.ap
                rd.append(scale)
            else:
                kw["scale"] = scale
        if accum is not None:
            kw["accum_out"] = accum.ap
            wr.append(accum)
        return self.op("act", lambda en: en.activation(out.ap, in_.ap, func, **kw), rd, wr)

    def tt(self, out, a, b, op, e="dve"):
        return self.op(e, lambda en: en.tensor_tensor(out.ap, a.ap, b.ap, op), [a, b], [out])

    def ts(self, out, a, s1, op0, s2=None, op1=None, accum=None, e="dve"):
        rd = [a]
        wr = [out]
        s1a = s1.ap if isinstance(s1, V) else s1
        s2a = s2.ap if isinstance(s2, V) else s2
        if isinstance(s1, V):
            rd.append(s1)
        if isinstance(s2, V):
            rd.append(s2)
        kw = {}
        if op1 is not None:
            kw["op1"] = op1
        if accum is not None:
            kw["accum_out"] = accum.ap
            wr.append(accum)
        return self.op(e, lambda en: en.tensor_scalar(out.ap, a.ap, s1a, s2a, op0, **kw), rd, wr)

    def stt(self, out, a, s, b, op0, op1, e="dve"):
        rd = [a, b]
        sa = s.ap if isinstance(s, V) else s
        if isinstance(s, V):
            rd.append(s)
        return self.op(e, lambda en: en.scalar_tensor_tensor(out.ap, a.ap, sa, b.ap, op0, op1), rd, [out])

    def copy(self, out, in_, e="dve"):
        if e == "act":
            return self.op("act", lambda en: en.copy(out.ap, in_.ap), [in_], [out])
        return self.op(e, lambda en: en.tensor_copy(out.ap, in_.ap), [in_], [out])

    def memset(self, out, val, e="dve"):
        return self.op(e, lambda en: en.memset(out.ap, val), [], [out])

    def reduce(self, out, in_, op=ALU.add, axis=AX.X, e="dve"):
        return self.op(e, lambda en: en.tensor_reduce(out.ap, in_.ap, axis, op), [in_], [out])

    def recip(self, out, in_):
        return self.op("dve", lambda en: en.reciprocal(out.ap, in_.ap), [in_], [out])


D = 1024
SEQ = 16384
NCORE = 8
TOK = SEQ // NCORE
NT = TOK // 128
IN_COLS = 8208
EPS = 1e-6
DEBUG = False
SAME_ENG = False


def bcast_row(t, n, parts=128):
    return t.v(t.h[0:1, 0:n].to_broadcast([parts, n]))


def emit_mod(p, c_bc, wada, bada, col0, ncols, out_tile, ones1, plus_one=False):
    nblk = (ncols + 511) // 512
    for b in range(nblk):
        n = min(512, ncols - b * 512)
        ws = p.rot("modw", [128, 8, 512], F32, 1)
        p.dma(ws[:, :, 0:n], wada.v(wada.h[:, col0 + b * 512: col0 + b * 512 + n].rearrange("(k p) n -> p k n", p=128)))
        bs = p.rot("modb", [1, 512], F32, 2)
        p.dma(bs[0:1, 0:n], bada[0:1, col0 + b * 512: col0 + b * 512 + n])
        ps = p.rot("ps_mm", [128, 512], F32, 4, psum=True)
        for k in range(8):
            p.mm(ps[:, 0:n], c_bc[:, k, :], ws[:, k, 0:n], start=(k == 0), stop=False)
        p.mm(ps[:, 0:n], ones1[0:1, :], bs[0:1, 0:n], start=False, stop=True)
        if plus_one:
            p.ts(out_tile[:, b * 512: b * 512 + n], ps[:, 0:n], 1.0, ALU.add)
        else:
            p.copy(out_tile[:, b * 512: b * 512 + n], ps[:, 0:n])


def emit_cbc(p, c_in, ones_f):
    c_col = p.sb([128, 8], F32, "c_col")
    p.dma(c_col[:], c_in[:])
    c_act = p.sb([128, 8], F32, "c_act")
    p.act(c_act[:], c_col[:], AF.Silu)
    c_bc = p.sb([128, 8, 128], F32, "c_bc")
    for k in range(8):
        p.ts(c_bc[:, k, :], ones_f[:], c_act[:, k:k + 1], ALU.mult)
    return c_bc


def emit_rmsnorm_mod(p, x_t, gmod, shift, h_out, width=D):
    junk = p.rot("rn_junk", [128, width], F32, 1)
    ssq = p.rot("rn_ssq", [128, 1], F32, 2)
    p.act(junk[:], x_t, AF.Square, accum=ssq[:])
    rstd = p.rot("rn_rstd", [128, 1], F32, 2)
    p.act(rstd[:], ssq[:], AF.Sqrt, bias=EPS, scale=1.0 / width)
    p.recip(rstd[:], rstd[:])
    tmp = p.rot("rn_tmp", [128, width], F32, 1)
    p.stt(tmp[:], x_t, rstd[:], gmod, ALU.mult, ALU.mult)
    p.tt(h_out, tmp[:], shift, ALU.add)


def build_k1(ntiles=NT, ncols=IN_COLS):
    nc = bass.Bass("TRN2", target_bir_lowering=False)
    p = P(nc)
    ntok = ntiles * 128
    x = p.dram("x", [ntok, D], F32, kind="ExternalInput")
    c_in = p.dram("c_col", [128, 8], F32, kind="ExternalInput")
    wada = p.dram("wada", [D, 2 * D], F32, kind="ExternalInput")
    bada = p.dram("bada", [1, 2 * D], F32, kind="ExternalInput")
    ng = p.dram("norm_g", [1, D], F32, kind="ExternalInput")
    w_in = p.dram("w_in", [D, ncols], F32, kind="ExternalInput")
    ident_d = p.dram("ident_in", [128, 128], F32, kind="ExternalInput")
    out = p.dram("proj", [ntok, ncols], F32, kind="ExternalOutput")

    ident = p.sb([128, 128], F32, "ident")
    p.dma(ident[:], ident_d[:])
    identb = p.sb([128, 128], BF16, "identb")
    p.copy(identb[:], ident[:])
    ones_f = p.sb([128, 128], F32, "ones_f")
    p.memset(ones_f[:], 1.0)
    c_bc = emit_cbc(p, c_in, ones_f)
    shift = p.sb([128, D], F32, "shift")
    gmod = p.sb([128, D], F32, "gmod")
    emit_mod(p, c_bc, wada, bada, 0, D, shift, ones_f)
    emit_mod(p, c_bc, wada, bada, D, D, gmod, ones_f, plus_one=True)
    ngb = p.sb([128, D], F32, "ngb")
    p.dma(ngb[:], bcast_row(ng, D))
    p.tt(gmod[:], gmod[:], ngb[:], ALU.mult)

    HALF = (ncols + 1) // 2
    Wb = p.sb([128, 8, HALF], BF16, "Wb")
    PIECE = 2052
    cv = 0
    for half in range(2):
        h0 = half * HALF
        hn = min(HALF, ncols - h0)
        for k in range(8):
            p.dma(Wb[:, k, 0:hn], w_in[k * 128:(k + 1) * 128, h0:h0 + hn], q="pool")
        def prep(t):
            xt = p.rot("xt", [128, D], F32, 2)
            p.dma(xt[:], x[t * 128:(t + 1) * 128, :])
            hb = p.rot("hb", [128, D], BF16, 2)
            emit_rmsnorm_mod(p, xt[:], gmod[:], shift[:], hb[:])
            psT = p.rot("psT", [128, 8, 128], BF16, 1, psum=True)
            for k in range(8):
                p.tr(psT[:, k, :], hb[:, k * 128:(k + 1) * 128], identb[:])
            hT = p.rot("hT", [128, 8, 128], BF16, 3)
            p.copy(hT[:], psT[:], e="act")
            return hb, hT

        nxt_prep = prep(0)
        for t in range(ntiles):
            hb, hT = nxt_prep
            if t + 1 < ntiles:
                nxt_prep = prep(t + 1)
            ot = p.rot("ot", [128, HALF], F32, 2)
            nb = (hn + 511) // 512
            for b in range(nb):
                n = min(512, hn - b * 512)
                ps = p.rot("ps_mm", [128, 512], F32, 4, psum=True)
                for k in range(8):
                    p.mm(ps[:, 0:n], hT[:, k, :], Wb[:, k, b * 512: b * 512 + n], start=(k == 0), stop=(k == 7))
                p.copy(ot[:, b * 512:b * 512 + n], ps[:, 0:n], e=("dve" if b % 2 == 0 else "act"))
            p.dma(out[t * 128:(t + 1) * 128, h0:h0 + hn], ot[:, 0:hn], q=("sp" if t % 2 == 0 else "pool"))
    if DEBUG:
        dbg = p.dram("dbg", [128, 4 * D], F32, kind="ExternalOutput")
        p.dma(dbg[:, 0:D], gmod[:])
        p.dma(dbg[:, D:2 * D], shift[:])
        hf = p.sb([128, D], F32, "hf")
        p.copy(hf[:], hb[:])
        p.dma(dbg[:, 2 * D:3 * D], hf[:])
        hf2 = p.sb([128, D], F32, "hf2")
        p.copy(hf2[:], hT[:])
        p.dma(dbg[:, 3 * D:4 * D], hf2[:])
        p.finish([out, dbg])
        return nc, p
    p.finish([out])
    return nc, p


def emit_qk_norm(p, src, nblk, gain_bc, identb, dstT, scale, neg_dstT=None):
    for b0 in range(0, nblk, 4):
        t = p.rot("qk_in", [128, 4, 128], F32, 2)
        p.dma(t[:], src.v(src.h[b0 * 128:(b0 + 4) * 128, :].rearrange("(b p) d -> p b d", p=128)), q=("sp" if (b0 // 4) % 2 == 0 else "pool"))
        sq = p.rot("qk_sq", [128, 4, 128], F32, 1)
        p.act(sq[:], t[:], AF.Square)
        ssq = p.rot("qk_ssq", [128, 4], F32, 2)
        p.reduce(ssq[:], sq[:])
        rstd = p.rot("qk_rstd", [128, 4], F32, 2)
        p.act(rstd[:], ssq[:], AF.Sqrt, bias=EPS, scale=1.0 / 128)
        p.recip(rstd[:], rstd[:])
        nb = p.rot("qk_nb", [128, 4, 128], BF16, 2)
        for i in range(4):
            p.stt(nb[:, i, :], t[:, i, :], rstd[:, i:i + 1], gain_bc[:], ALU.mult, ALU.mult)
        pt = p.rot("qk_pt", [128, 512], BF16, 1, psum=True)
        for i in range(4):
            p.tr(pt[:, i * 128:(i + 1) * 128], nb[:, i, :], identb[:])
        p.ts(dstT[:, b0 * 128:(b0 + 4) * 128], pt[:], scale, ALU.mult, e="pool" if False else "dve")
        if neg_dstT is not None:
            p.ts(neg_dstT[:, b0 * 128:(b0 + 4) * 128], pt[:], -scale, ALU.mult)


def build_sb(nblk=128):
    nc = bass.Bass("TRN2", target_bir_lowering=False)
    p = P(nc)
    S = nblk * 128
    ngrp = nblk // 8
    q_d = p.dram("q", [ngrp * 512, 128], F32, kind="ExternalInput")
    k_d = p.dram("k", [S, 128], F32, kind="ExternalInput")
    v_d = p.dram("v", [S, 128], F32, kind="ExternalInput")
    qg_d = p.dram("qg", [1, 128], F32, kind="ExternalInput")
    kg_d = p.dram("kg", [1, 128], F32, kind="ExternalInput")
    mask_d = p.dram("mask", [128, 8, 512], F32, kind="ExternalInput")
    mincl_d = p.dram("mincl", [128, 128], F32, kind="ExternalInput")
    ident_d = p.dram("ident_in", [128, 128], F32, kind="ExternalInput")
    out = p.dram("oT", [128, ngrp * 512], F32, kind="ExternalOutput")

    ident = p.sb([128, 128], F32, "ident")
    p.dma(ident[:], ident_d[:])
    identb = p.sb([128, 128], BF16, "identb")
    p.copy(identb[:], ident[:])
    mincl_f = p.sb([128, 128], F32, "mincl_f")
    p.dma(mincl_f[:], mincl_d[:])
    mincl = p.sb([128, 128], BF16, "mincl")
    p.copy(mincl[:], mincl_f[:])
    onesb = p.sb([128, 128], BF16, "onesb")
    p.memset(onesb[:], 1.0)
    maskb = p.sb([128, 8, 512], BF16, "maskb")
    for o in range(8):
        mt = p.rot("mask_st", [128, 512], F32, 2)
        p.dma(mt[:], mask_d[:, o, :])
        p.copy(maskb[:, o, :], mt[:])
    qg = p.sb([128, 128], F32, "qg")
    p.dma(qg[:], bcast_row(qg_d, 128))
    kg = p.sb([128, 128], F32, "kg")
    p.dma(kg[:], bcast_row(kg_d, 128))

    knT = p.sb([128, S], BF16, "knT")
    nknT = p.sb([128, S], BF16, "nknT")
    qnT = p.sb([128, ngrp * 512], BF16, "qnT")
    vb = p.sb([128, nblk, 128], BF16, "vb")
    emit_qk_norm(p, k_d, nblk, kg, identb, knT, 1.0, nknT)
    emit_qk_norm(p, q_d, ngrp * 4, qg, identb, qnT, 128 ** -0.5)
    for b0 in range(0, nblk, 4):
        t = p.rot("qk_in", [128, 4, 128], F32, 2)
        p.dma(t[:], v_d.v(v_d.h[b0 * 128:(b0 + 4) * 128, :].rearrange("(b p) d -> p b d", p=128)))
        p.copy(vb[:, b0:b0 + 4, :], t[:], e="pool")

    import os
    STOP = int(os.environ.get("SB_STOP", "0"))
    if STOP == 1:
        for m in range(ngrp):
            ot = p.rot("sbOT", [128, 512], F32, 2)
            p.copy(ot[:], qnT[:, m * 512:(m + 1) * 512])
            p.dma(out[:, m * 512:(m + 1) * 512], ot[:])
        p.finish([out])
        return nc, p
    for m in range(ngrp):
        O = p.rot("sbO", [128, 512], F32, 1, psum=True)
        CS = p.rot("sbCS", [128, 512], F32, 1, psum=True)
        qs = qnT[:, m * 512:(m + 1) * 512]
        kbs = list(range(8 * m + 7, -1, -1))
        nit = len(kbs)
        st = {}

        def phaseA(it):
            kb = kbs[it]
            off = kb - 8 * m
            Z = p.rot("sbZ", [128, 512], F32, 2, psum=True)
            U = p.rot("sbU", [128, 512], F32, 3, psum=True)
            p.mm(Z[:], knT[:, kb * 128:(kb + 1) * 128], qs)
            p.mm(U[:], nknT[:, kb * 128:(kb + 1) * 128], qs, start=True, stop=False)
            e = p.rot("sbE", [128, 512], F32, 3)
            p.act(e[:], Z[:], AF.Exp)
            st[("e", it)] = (U, e, off)

        def phaseA2(it):
            U, e, off = st.pop(("e", it))
            spb = p.rot("sbSP", [128, 512], BF16, 4)
            p.act(spb[:], e[:], AF.Ln, bias=1.0)
            if off >= 0:
                p.tt(spb[:], spb[:], maskb[:, off, :], ALU.mult)
            st[it] = (U, spb)

        def phaseB(it):
            kb = kbs[it]
            off = kb - 8 * m
            first = it == 0
            last = it == nit - 1
            U, spb = st.pop(it)
            p.mm(U[:], mincl[:], spb[:], start=False, stop=first)
            if not first:
                p.mm(U[:], identb[:], st["accb"][:], start=False, stop=True)
            p.mm(CS[:], onesb[:], spb[:], start=first, stop=last)
            att = p.rot("sbATT", [128, 512], BF16, 2)
            p.act(att[:], U[:], AF.Exp, scale=-1.0)
            if off >= 0:
                p.tt(att[:], att[:], maskb[:, off, :], ALU.mult)
            if not last:
                accb = p.rot("sbACC", [128, 512], BF16, 2)
                p.copy(accb[:], CS[:])
                st["accb"] = accb
            p.mm(O[:], vb[:, kb, :], att[:], start=first, stop=last)

        AH = 2
        for it in range(min(AH, nit)):
            phaseA(it)
            phaseA2(it)
        for it in range(nit):
            if it + AH < nit:
                phaseA(it + AH)
            phaseB(it)
            if it + AH < nit:
                phaseA2(it + AH)
        ot = p.rot("sbOT", [128, 512], F32, 2)
        p.copy(ot[:], O[:])
        p.dma(out[:, m * 512:(m + 1) * 512], ot[:])
    p.finish([out])
    return nc, p


def emit_conv_silu(p, src_d, nch, S, w_d, b_d, dst, dst_dt, name, CH=2048, bias=True):
    w = p.sb([nch, 4], F32, name + "_w")
    p.dma(w[:], w_d[:])
    if bias:
        b = p.sb([nch, 1], F32, name + "_b")
        p.dma(b[:], b_d[:])
    for c0 in range(0, S, CH):
        t = p.rot("cv_in", [128, CH + 3], F32, 2)
        p.dma(t[0:nch, :], src_d[:, c0:c0 + CH + 3])
        acc = p.rot("cv_acc", [128, CH], F32, 2)
        p.ts(acc[0:nch, :], t[0:nch, 0:CH], w[:, 0:1], ALU.mult)
        for i in range(1, 4):
            p.stt(acc[0:nch, :], t[0:nch, i:i + CH], w[:, i:i + 1], acc[0:nch, :], ALU.mult, ALU.add)
        if bias:
            p.act(dst[0:nch, c0:c0 + CH], acc[0:nch, :], AF.Silu, bias=b[:, 0:1])
        else:
            p.act(dst[0:nch, c0:c0 + CH], acc[0:nch, :], AF.Silu)


def build_ssd(nchunk=128):
    nc = bass.Bass("TRN2", target_bir_lowering=False)
    p = P(nc)
    S = nchunk * 128
    CH = min(2048, S)
    xT_d = p.dram("xT", [64, S + 3], F32, kind="ExternalInput")
    BT_d = p.dram("BT", [128, S + 3], F32, kind="ExternalInput")
    CT_d = p.dram("CT", [128, S + 3], F32, kind="ExternalInput")
    wx_d = p.dram("wx", [64, 4], F32, kind="ExternalInput")
    bx_d = p.dram("bx", [64, 1], F32, kind="ExternalInput")
    wB_d = p.dram("wB", [128, 4], F32, kind="ExternalInput")
    bB_d = p.dram("bB", [128, 1], F32, kind="ExternalInput")
    wC_d = p.dram("wC", [128, 4], F32, kind="ExternalInput")
    bC_d = p.dram("bC", [128, 1], F32, kind="ExternalInput")
    dt_d = p.dram("dt_col", [128, nchunk], F32, kind="ExternalInput")
    sc_d = p.dram("scal", [128, 3], F32, kind="ExternalInput")
    triu_d = p.dram("triu", [128, 128], F32, kind="ExternalInput")
    mneg_d = p.dram("mneg", [128, 128], F32, kind="ExternalInput")
    ident_d = p.dram("ident_in", [128, 128], F32, kind="ExternalInput")
    out = p.dram("y", [S, 64], F32, kind="ExternalOutput")

    ident = p.sb([128, 128], F32, "ident")
    p.dma(ident[:], ident_d[:])
    identb = p.sb([128, 128], BF16, "identb")
    p.copy(identb[:], ident[:])
    triu = p.sb([128, 128], F32, "triu")
    p.dma(triu[:], triu_d[:])
    mneg = p.sb([128, 128], F32, "mneg")
    p.dma(mneg[:], mneg_d[:])
    ones_f = p.sb([128, 128], F32, "ones_f")
    p.memset(ones_f[:], 1.0)
    scal = p.sb([128, 3], F32, "scal")
    p.dma(scal[:], sc_d[:])
    dtc = p.sb([128, nchunk], F32, "dtc")
    p.dma(dtc[:], dt_d[:])
    p.act(dtc[:], dtc[:], AF.Exp, bias=scal[:, 0:1])
    p.act(dtc[:], dtc[:], AF.Ln, bias=1.0)
    aneg = p.sb([128, 1], F32, "aneg")
    p.act(aneg[:], scal[:, 1:2], AF.Exp)
    p.ts(aneg[:], aneg[:], -1.0, ALU.mult)
    adt = p.sb([128, nchunk], F32, "adt")
    p.ts(adt[:], dtc[:], aneg[:, 0:1], ALU.mult)

    xT = p.sb([64, S], F32, "xTs")
    BT = p.sb([128, S], BF16, "BTs")
    CT = p.sb([128, S], BF16, "CTs")
    emit_conv_silu(p, xT_d, 64, S, wx_d, bx_d, xT, F32, "cx", CH)
    emit_conv_silu(p, BT_d, 128, S, wB_d, bB_d, BT, BF16, "cB", CH)
    emit_conv_silu(p, CT_d, 128, S, wC_d, bC_d, CT, BF16, "cC", CH)

    state_f = p.sb([128, 64], F32, "state_f")
    state_b = p.sb([128, 64], BF16, "state_b")
    p.memset(state_f[:], 0.0)
    p.memset(state_b[:], 0.0)
    OB = 8
    for c in range(nchunk):
        sl = slice(c * 128, (c + 1) * 128)
        px = p.rot("ssd_px", [128, 64], F32, 1, psum=True)
        p.tr(px[:], xT[0:64, sl], ident[0:64, 0:64])
        xtok = p.rot("ssd_xtok", [128, 64], F32, 2)
        p.copy(xtok[:], px[:])
        pB = p.rot("ssd_pB", [128, 128], BF16, 1, psum=True)
        p.tr(pB[:], BT[:, sl], identb[:])
        Btok = p.rot("ssd_Btok", [128, 128], BF16, 2)
        p.copy(Btok[:], pB[:], e="act")
        adt_c = adt[:, c:c + 1]
        pcol = p.rot("ssd_pcol", [128, 1], F32, 1, psum=True)
        p.mm(pcol[:], triu[:], adt_c)
        acol = p.rot("ssd_acol", [128, 1], F32, 2)
        p.copy(acol[:], pcol[:])
        adt_bc = p.rot("ssd_adtbc", [128, 128], F32, 2)
        p.ts(adt_bc[:], ones_f[:], adt_c, ALU.mult)
        prow = p.rot("ssd_prow", [128, 128], F32, 1, psum=True)
        p.mm(prow[:], adt_bc[:], triu[:])
        dsg = p.rot("ssd_dsg", [128, 128], F32, 2)
        p.stt(dsg[:], prow[:], acol[:, 0:1], mneg[:], ALU.subtract, ALU.add)
        segT = p.rot("ssd_segT", [128, 128], F32, 2)
        p.act(segT[:], dsg[:], AF.Exp)
        ea = p.rot("ssd_ea", [128, 128], F32, 2)
        p.act(ea[:], prow[:], AF.Exp)
        alast = p.rot("ssd_alast", [128, 1], F32, 2)
        p.copy(alast[:], prow[:, 127:128])
        dte = p.rot("ssd_dte", [128, 1], F32, 2)
        p.act(dte[:], acol[:], AF.Exp, bias=alast[:, 0:1], scale=-1.0)
        cdec = p.rot("ssd_cdec", [128, 1], F32, 2)
        p.act(cdec[:], alast[:], AF.Exp)
        pcb = p.rot("ssd_pcb", [128, 128], F32, 1, psum=True)
        p.mm(pcb[:], BT[:, sl], CT[:, sl])
        scT = p.rot("ssd_scT", [128, 128], BF16, 2)
        p.tt(scT[:], pcb[:], segT[:], ALU.mult)
        xdt = p.rot("ssd_xdt", [128, 64], BF16, 2)
        p.ts(xdt[:], xtok[:], dtc[:, c:c + 1], ALU.mult)
        cdT = p.rot("ssd_cdT", [128, 128], BF16, 2)
        p.tt(cdT[:], CT[:, sl], ea[:], ALU.mult)
        py = p.rot("ssd_py", [128, 64], F32, 2, psum=True)
        p.mm(py[:], scT[:], xdt[:], start=True, stop=False)
        p.mm(py[:], cdT[:], state_b[:], start=False, stop=True)
        if c % OB == 0:
            yo = p.rot("ssd_yo", [128, OB, 64], F32, 2)
        p.stt(yo[:, c % OB, :], xtok[:], scal[:, 2:3], py[:], ALU.mult, ALU.add)
        if c % OB == OB - 1:
            c0 = (c - OB + 1) * 128
            p.dma(out.v(out.h[c0:c0 + OB * 128, :].rearrange("(b p) d -> p b d", p=128)), yo[:])
        sc2 = p.rot("ssd_sc2", [128, 1], F32, 2)
        p.tt(sc2[:], dtc[:, c:c + 1], dte[:], ALU.mult)
        xdd = p.rot("ssd_xdd", [128, 64], BF16, 2)
        p.ts(xdd[:], xtok[:], sc2[:, 0:1], ALU.mult)
        pS = p.rot("ssd_pS", [128, 64], F32, 1, psum=True)
        p.mm(pS[:], Btok[:], xdd[:])
        p.stt(state_f[:], state_f[:], cdec[:, 0:1], pS[:], ALU.mult, ALU.add)
        p.copy(state_b[:], state_f[:])
    p.finish([out])
    return nc, p


def build_gdn(nchunk=256, NB=4):
    import os
    nc = bass.Bass("TRN2", target_bir_lowering=False)
    p = P(nc)
    S = nchunk * 64
    CH = min(1024, S)
    CPS = CH // 64
    qT_d = p.dram("qT", [128, S + 3], F32, kind="ExternalInput")
    kT_d = p.dram("kT", [128, S + 3], F32, kind="ExternalInput")
    vT_d = p.dram("vT", [64, S + 3], F32, kind="ExternalInput")
    wq_d = p.dram("wq", [128, 4], F32, kind="ExternalInput")
    wk_d = p.dram("wk", [128, 4], F32, kind="ExternalInput")
    wv_d = p.dram("wv", [64, 4], F32, kind="ExternalInput")
    a_d = p.dram("a_col", [64, nchunk], F32, kind="ExternalInput")
    b_d = p.dram("b_col", [64, nchunk], F32, kind="ExternalInput")
    sc_d = p.dram("scal", [128, 2], F32, kind="ExternalInput")
    triu_d = p.dram("triu", [64, 64], F32, kind="ExternalInput")
    mpos_d = p.dram("mpos", [64, 64], F32, kind="ExternalInput")
    mneg_d = p.dram("mneg", [64, 64], F32, kind="ExternalInput")
    st01_d = p.dram("st01", [64, 64], F32, kind="ExternalInput")
    ident_d = p.dram("ident_in", [128, 128], F32, kind="ExternalInput")
    out = p.dram("o", [S, 64], F32, kind="ExternalOutput")

    def ld(d, shape, name):
        t = p.sb(shape, F32, name)
        p.dma(t[:], d[:])
        return t
    ident = ld(ident_d, [128, 128], "ident")
    triu = ld(triu_d, [64, 64], "triu")
    mpos = ld(mpos_d, [64, 64], "mpos")
    mneg = ld(mneg_d, [64, 64], "mneg")
    st01 = ld(st01_d, [64, 64], "st01")
    scal = ld(sc_d, [128, 2], "scal")
    ones_f = p.sb([64, 128], F32, "ones_f")
    p.memset(ones_f[:], 1.0)
    beta = ld(b_d, [64, nchunk], "beta")
    p.act(beta[:], beta[:], AF.Sigmoid)
    gall = ld(a_d, [64, nchunk], "gall")
    p.act(gall[:], gall[:], AF.Exp, bias=scal[0:64, 1:2])
    p.act(gall[:], gall[:], AF.Ln, bias=1.0)
    aneg = p.sb([64, 1], F32, "aneg")
    p.act(aneg[:], scal[0:64, 0:1], AF.Exp)
    p.ts(aneg[:], aneg[:], -1.0, ALU.mult)
    p.ts(gall[:], gall[:], aneg[:, 0:1], ALU.mult)

    wq = ld(wq_d, [128, 4], "wq")
    wk = ld(wk_d, [128, 4], "wk")
    wv = ld(wv_d, [64, 4], "wv")
    state = p.sb([128, 64], F32, "state")
    p.memset(state[:], 0.0)
    OB = 8
    RSQ = 128 ** -0.5
    D1 = NB + 1
    D2 = 2 * NB + 1

    def conv(src_d, nch, w, c0, key):
        t = p.rot(key + "_in", [128, CH + 3], F32, 1)
        p.dma(t[0:nch, :], src_d[:, c0:c0 + CH + 3])
        acc = p.rot(key + "_acc", [128, CH], F32, 1)
        p.ts(acc[0:nch, :], t[0:nch, 0:CH], w[:, 0:1], ALU.mult)
        for i in range(1, 4):
            p.stt(acc[0:nch, :], t[0:nch, i:i + CH], w[:, i:i + 1], acc[0:nch, :], ALU.mult, ALU.add)
        o = p.rot(key + "_o", [128, CH], F32, 2)
        p.act(o[0:nch, :], acc[0:nch, :], AF.Silu)
        return o

    oo_box = [None]

    def pre(c, ci, qTs, kTs, vTs, ctx):
        sl = slice(ci * 64, (ci + 1) * 64)
        PB = p.rot("g_PB", [128, 512], F32, 2 * NB, psum=True)
        ctx["PB"] = PB
        ptq, ptk, ptv = PB[0:64, 0:128], PB[0:64, 128:256], PB[0:64, 256:320]
        p.tr(ptq, qTs[:, sl], ident[:])
        p.tr(ptk, kTs[:, sl], ident[:])
        p.tr(ptv, vTs[0:64, sl], ident[0:64, 0:64])
        g_c = gall[:, c:c + 1]
        b_c = beta[:, c:c + 1]
        pcol = PB[0:64, 320:321]
        p.mm(pcol, triu[:], g_c)
        g_bc = p.rot("g_gbc", [64, 128], F32, D1)
        p.ts(g_bc[:], ones_f[:], g_c, ALU.mult)
        yield
        junk = p.rot("g_junk", [64, 128], F32, 2)
        ssq = p.rot("g_ssq", [64, 2], F32, D1)
        p.act(junk[:], ptq, AF.Square, accum=ssq[:, 0:1])
        p.act(junk[:], ptk, AF.Square, accum=ssq[:, 1:2])
        if os.environ.get('GDN_SUB') == 'a':
            yield
            return
        gcol = p.rot("g_gcol", [64, 1], F32, D1)
        p.copy(gcol[:], pcol, e="act")
        if os.environ.get('GDN_SUB') == 'b':
            yield
            return
        prow, prow64 = PB[:, 384:448], PB[0:64, 384:448]
        p.mm(prow, g_bc[:], triu[:])
        yield
        rinv = p.rot("g_rinv", [64, 2], F32, D1)
        p.act(rinv[:], ssq[:], AF.Sqrt, bias=EPS)
        nd = p.rot("g_nd", [64, 64], F32, D1)
        p.stt(nd[:], prow64, gcol[:, 0:1], mpos[:], ALU.subtract, ALU.add)
        d2 = p.rot("g_d2", [64, 64], F32, D1)
        p.stt(d2[:], prow64, gcol[:, 0:1], mneg[:], ALU.subtract, ALU.add)
        glast = p.rot("g_glast", [128, 1], F32, D1)
        p.copy(glast[:], PB[:, 447:448])
        yield
        p.recip(rinv[:], rinv[:])
        dec_s = p.rot("g_decs", [64, 64], F32, D1)
        p.act(dec_s[:], nd[:], AF.Exp, scale=-1.0)
        decT = p.rot("g_decT", [64, 64], F32, D1)
        p.act(decT[:], d2[:], AF.Exp)
        egc = p.rot("g_egc", [64, 1], F32, D1)
        p.act(egc[:], gcol[:], AF.Exp)
        dlast = p.rot("g_dlast", [64, 1], F32, D1)
        p.act(dlast[:], gcol[:], AF.Exp, bias=glast[0:64, 0:1], scale=-1.0)
        cdec = p.rot("g_cdec", [128, 1], F32, D2)
        p.act(cdec[:], glast[:], AF.Exp)
        ctx["cdec"] = cdec
        yield
        decTs = p.rot("g_decTs", [64, 64], F32, D1)
        p.tt(decTs[:], decT[:], st01[:], ALU.mult)
        k_n = p.rot("g_kn", [64, 128], F32, D1)
        p.ts(k_n[:], ptk, rinv[:, 1:2], ALU.mult)
        q_n = p.rot("g_qn", [64, 128], F32, D1)
        p.ts(q_n[:], ptq, rinv[:, 0:1], ALU.mult, RSQ, ALU.mult)
        R = p.rot("g_R0", [64, 192], F32, D1)
        p.ts(R[:, 0:64], ptv, b_c, ALU.mult)
        yield
        kb = p.rot("g_kb", [64, 128], F32, D1)
        p.ts(kb[:], k_n[:], b_c, ALU.mult)
        qd = p.rot("g_qd", [64, 128], F32, D1)
        p.ts(qd[:], q_n[:], egc[:, 0:1], ALU.mult)
        kd = p.rot("g_kd", [64, 128], F32, D2)
        p.ts(kd[:], k_n[:], dlast[:, 0:1], ALU.mult)
        ctx["kd"] = kd
        yield
        p.ts(R[:, 64:192], kb[:], egc[:, 0:1], ALU.mult)
        p.tr(PB[:, 0:64], k_n[:], ident[0:64, 0:64])
        p.tr(PB[:, 64:128], kb[:], ident[0:64, 0:64])
        p.tr(PB[:, 128:192], q_n[:], ident[0:64, 0:64])
        p.tr(PB[:, 192:256], qd[:], ident[0:64, 0:64])
        yield
        fT = p.rot("g_fT", [128, 256], F32, D2)
        p.copy(fT[:], PB[:, 0:256])
        knT, kbT, qnT, qdT = fT[:, 0:64], fT[:, 64:128], fT[:, 128:192], fT[:, 192:256]
        ctx["qdT"] = qdT
        yield
        pG0, pG1, pG2 = PB[0:64, 256:320], PB[0:64, 320:384], PB[0:64, 384:448]
        p.mm(pG0, kbT, knT)
        p.mm(pG1, knT, kbT)
        p.mm(pG2, knT, qnT)
        yield
        A = p.rot("g_A", [64, 64], F32, 2 * NB)
        p.tt(A[:], pG0, dec_s[:], ALU.mult)
        At = p.rot("g_At", [64, 64], F32, 2 * NB)
        p.tt(At[:], pG1, decTs[:], ALU.mult)
        attnT = p.rot("g_attnT", [64, 64], F32, D2)
        p.tt(attnT[:], pG2, decT[:], ALU.mult)
        ctx["attnT"] = attnT
        yield
        for lvl in range(6):
            pR = PB[0:64, 0:192]
            p.mm(pR, At[:], R[:])
            if lvl < 5:
                pP0, pP1 = PB[0:64, 192:256], PB[0:64, 256:320]
                p.mm(pP0, At[:], A[:])
                p.mm(pP1, A[:], At[:])
            yield
            Rn = p.rot("g_Rf", [64, 192], F32, D2) if lvl == 5 else p.rot("g_Ri", [64, 192], F32, 2 * NB)
            if lvl == 0:
                p.tt(Rn[:], R[:], pR, ALU.subtract)
            else:
                p.tt(Rn[:], R[:], pR, ALU.add)
            R = Rn
            if lvl < 5:
                A2 = p.rot("g_A", [64, 64], F32, 2 * NB)
                At2 = p.rot("g_At", [64, 64], F32, 2 * NB)
                p.copy(A2[:], pP0)
                p.copy(At2[:], pP1)
                A, At = A2, At2
            yield
        ctx["R"] = R
        pW = PB[:, 320:384]
        p.tr(pW, R[:, 64:192], ident[0:64, 0:64])
        yield
        wT = p.rot("g_wT", [128, 64], F32, D2)
        p.copy(wT[:], pW)
        ctx["wT"] = wT
        yield

    def scan(c, ctx):
        PB = ctx["PB"]
        R = ctx["R"]
        pv = PB[0:64, 384:448]
        p.mm(pv, ctx["wT"][:], state[:])
        vnew = p.rot("g_vnew", [64, 64], F32, 3)
        p.tt(vnew[:], R[:, 0:64], pv, ALU.subtract)
        po = PB[0:64, 448:512]
        p.mm(po, ctx["qdT"], state[:], start=True, stop=False)
        p.mm(po, ctx["attnT"][:], vnew[:], start=False, stop=True)
        pS = PB[:, 192:256]
        p.mm(pS, ctx["kd"][:], vnew[:])
        p.stt(state[:], state[:], ctx["cdec"][:, 0:1], pS, ALU.mult, ALU.add)
        if c % OB == 0:
            oo_box[0] = p.rot("g_oo", [64, OB, 64], F32, 2)
        oo = oo_box[0]
        p.copy(oo[:, c % OB, :], po)
        if c % OB == OB - 1:
            c0 = (c - OB + 1) * 64
            p.dma(out.v(out.h[c0:c0 + OB * 64, :].rearrange("(b p) d -> p b d", p=64)), oo[:])

    import os
    MAXST = int(os.environ.get('GDN_MAXST', '999'))
    pending = []
    for sc in range(S // CH):
        qTs = conv(qT_d, 128, wq, sc * CH, "gq")
        kTs = conv(kT_d, 128, wk, sc * CH, "gk")
        vTs = conv(vT_d, 64, wv, sc * CH, "gv")
        for b0 in range(0, CPS, NB):
            ctxs = [dict() for _ in range(NB)]
            gens = [pre(sc * CPS + b0 + i, b0 + i, qTs, kTs, vTs, ctxs[i]) for i in range(NB)]
            stage = 0
            alive = True
            while alive:
                alive = False
                for g in gens:
                    try:
                        next(g)
                        alive = True
                    except StopIteration:
                        pass
                stage += 1
                if stage >= MAXST:
                    break
                if pending and stage % 4 == 0:
                    cc, cx = pending.pop(0)
                    scan(cc, cx)
            while pending:
                cc, cx = pending.pop(0)
                scan(cc, cx)
            if MAXST < 99:
                continue
            pending = [(sc * CPS + b0 + i, ctxs[i]) for i in range(NB)]
    while pending:
        cc, cx = pending.pop(0)
        scan(cc, cx)
    p.finish([out])
    return nc, p


def load_w_bf16(p, w_d, rows, cols, dst, r0=0):
    for k in range(rows // 128):
        p.dma(dst[:, k, 0:cols], w_d[r0 + k * 128:r0 + (k + 1) * 128, 0:cols], q="pool")


def build_k3a(ntiles=NT):
    nc = bass.Bass("TRN2", target_bir_lowering=False)
    p = P(nc)
    ntok = ntiles * 128
    di = lambda n, s: p.dram(n, s, F32, kind="ExternalInput")
    x = di("x", [ntok, D]); z_d = di("z", [ntok, 512]); gg_d = di("ggate", [ntok, 512]); brg_d = di("brg", [ntok, 3072])
    ya_d = di("ya", [ntok, 512]); yb_d = di("yb", [ntok, 512]); yc_d = di("yc", [ntok, 512])
    c_in = di("c_col", [128, 8]); wada = di("wada", [D, 4 * D]); bada = di("bada", [1, 4 * D])
    ssmg_d = di("ssm_g", [1, 512]); gdng_d = di("gdn_g", [1, 128]); ffng_d = di("ffn_g", [1, D])
    wbr_d = di("w_branch", [1536, D]); wout_d = di("w_out", [D, D]); wgr_d = di("wgr", [D, 36]); bgr_d = di("bgr", [1, 36])
    ident_d = di("ident_in", [128, 128])
    x1_o = p.dram("x1", [ntok, D], F32, kind="ExternalOutput")
    h2_o = p.dram("h2", [ntok, D], F32, kind="ExternalOutput")
    wt_o = p.dram("wt", [ntok, 32], F32, kind="ExternalOutput")
    gf_o = p.dram("gf", [128, D], F32, kind="ExternalOutput")

    ident = p.sb([128, 128], F32, "ident")
    p.dma(ident[:], ident_d[:])
    identb = p.sb([128, 128], BF16, "identb")
    p.copy(identb[:], ident[:])
    ones_f = p.sb([128, 128], F32, "ones_f")
    p.memset(ones_f[:], 1.0)
    c_bc = emit_cbc(p, c_in, ones_f)
    gate_m = p.sb([128, D], F32, "gate_m"); shift_f = p.sb([128, D], F32, "shift_f")
    gmod_f = p.sb([128, D], F32, "gmod_f"); gate_f = p.sb([128, D], F32, "gate_f")
    emit_mod(p, c_bc, wada, bada, 0, D, gate_m, ones_f)
    emit_mod(p, c_bc, wada, bada, D, D, shift_f, ones_f)
    emit_mod(p, c_bc, wada, bada, 2 * D, D, gmod_f, ones_f, plus_one=True)
    emit_mod(p, c_bc, wada, bada, 3 * D, D, gate_f, ones_f)
    p.dma(gf_o[:], gate_f[:])
    ffng = p.sb([128, D], F32, "ffng")
    p.dma(ffng[:], bcast_row(ffng_d, D))
    p.tt(gmod_f[:], gmod_f[:], ffng[:], ALU.mult)
    ssmg = p.sb([128, 512], F32, "ssmg")
    p.dma(ssmg[:], bcast_row(ssmg_d, 512))
    gdng = p.sb([128, 128], F32, "gdng")
    p.dma(gdng[:], bcast_row(gdng_d, 128))
    wgr = p.sb([128, 8, 36], F32, "wgr")
    p.dma(wgr[:], wgr_d.v(wgr_d.h[:, :].rearrange("(k p) n -> p k n", p=128)))
    bgr = p.sb([1, 36], F32, "bgr")
    p.dma(bgr[:], bgr_d[:])
    Wbr = p.sb([128, 12, D], BF16, "Wbr")
    load_w_bf16(p, wbr_d, 1536, D, Wbr)
    Wout = p.sb([128, 8, D], BF16, "Wout")
    load_w_bf16(p, wout_d, D, D, Wout)

    for t in range(ntiles):
        rs = slice(t * 128, (t + 1) * 128)
        xt = p.rot("xt", [128, D], F32, 2)
        p.dma(xt[:], x[rs, :])
        zt = p.rot("zt", [128, 512], F32, 2); p.dma(zt[:], z_d[rs, :], q="pool")
        gt = p.rot("gt", [128, 512], F32, 2); p.dma(gt[:], gg_d[rs, :])
        bt = p.rot("bt", [128, 3072], F32, 1); p.dma(bt[:], brg_d[rs, :], q="pool")
        ya = p.rot("ya", [128, 512], F32, 2); p.dma(ya[:], ya_d[rs, :])
        yb = p.rot("yb", [128, 512], F32, 2); p.dma(yb[:], yb_d[rs, :], q="pool")
        yc = p.rot("yc", [128, 512], F32, 2); p.dma(yc[:], yc_d[rs, :])
        brn = p.rot("brn", [128, 1536], BF16, 2)
        p.act(zt[:], zt[:], AF.Silu)
        p.tt(ya[:], ya[:], zt[:], ALU.mult)
        junk = p.rot("junk", [128, 512], F32, 1)
        ssq = p.rot("ssq", [128, 8], F32, 2)
        for g in range(2):
            p.act(junk[:, 0:256], ya[:, g * 256:(g + 1) * 256], AF.Square, accum=ssq[:, g:g + 1])
        for hd in range(4):
            p.act(junk[:, 0:128], yc[:, hd * 128:(hd + 1) * 128], AF.Square, accum=ssq[:, 2 + hd:3 + hd])
        rstd = p.rot("rstd", [128, 8], F32, 2)
        p.act(rstd[:, 0:2], ssq[:, 0:2], AF.Sqrt, bias=EPS, scale=1.0 / 256)
        p.act(rstd[:, 2:6], ssq[:, 2:6], AF.Sqrt, bias=EPS, scale=1.0 / 128)
        p.recip(rstd[:, 0:6], rstd[:, 0:6])
        for g in range(2):
            sl = slice(g * 256, (g + 1) * 256)
            p.stt(brn[:, sl], ya[:, sl], rstd[:, g:g + 1], ssmg[:, sl], ALU.mult, ALU.mult)
        p.copy(brn[:, 512:1024], yb[:], e="pool")
        p.act(gt[:], gt[:], AF.Silu)
        for hd in range(4):
            sl = slice(hd * 128, (hd + 1) * 128)
            p.stt(yc[:, sl], yc[:, sl], rstd[:, 2 + hd:3 + hd], gdng[:], ALU.mult, ALU.mult)
        p.tt(brn[:, 1024:1536], yc[:], gt[:], ALU.mult)
        brT = p.rot("brT", [128, 12, 128], BF16, 2)
        for q4 in range(3):
            pt = p.rot("psT", [128, 4, 128], BF16, 2, psum=True)
            for i in range(4):
                kk = q4 * 4 + i
                p.tr(pt[:, i, :], brn[:, kk * 128:(kk + 1) * 128], identb[:])
            p.copy(brT[:, q4 * 4:q4 * 4 + 4, :], pt[:])
        p.act(bt[:], bt[:], AF.Sigmoid)
        merged = p.rot("merged", [128, D], F32, 1)
        for nb in range(2):
            cs = slice(nb * 512, (nb + 1) * 512)
            for i in range(3):
                ps = p.rot("ps_mm", [128, 512], F32, 4, psum=True)
                for k in range(4):
                    p.mm(ps[:], brT[:, 4 * i + k, :], Wbr[:, 4 * i + k, cs], start=(k == 0), stop=(k == 3))
                if i == 0:
                    p.tt(merged[:, cs], ps[:], bt[:, i * D + nb * 512: i * D + (nb + 1) * 512], ALU.mult)
                else:
                    tmp = p.rot("mtmp", [128, 512], F32, 2)
                    p.tt(tmp[:], ps[:], bt[:, i * D + nb * 512: i * D + (nb + 1) * 512], ALU.mult)
                    p.tt(merged[:, cs], merged[:, cs], tmp[:], ALU.add)
        mb = p.rot("mb", [128, D], BF16, 1)
        p.copy(mb[:], merged[:], e="pool")
        mT = p.rot("mT", [128, 8, 128], BF16, 1)
        for q4 in range(2):
            pt = p.rot("psT", [128, 4, 128], BF16, 2, psum=True)
            for i in range(4):
                kk = q4 * 4 + i
                p.tr(pt[:, i, :], mb[:, kk * 128:(kk + 1) * 128], identb[:])
            p.copy(mT[:, q4 * 4:q4 * 4 + 4, :], pt[:])
        x1 = p.rot("x1", [128, D], F32, 2)
        for nb in range(2):
            cs = slice(nb * 512, (nb + 1) * 512)
            ps = p.rot("ps_mm", [128, 512], F32, 4, psum=True)
            for k in range(8):
                p.mm(ps[:], mT[:, k, :], Wout[:, k, cs], start=(k == 0), stop=(k == 7))
            tmp = p.rot("mtmp", [128, 512], F32, 2)
            p.tt(tmp[:], ps[:], gate_m[:, cs], ALU.mult)
            p.tt(x1[:, cs], tmp[:], xt[:, cs], ALU.add)
        p.dma(x1_o[rs, :], x1[:])
        h2 = p.rot("h2", [128, D], F32, 2)
        emit_rmsnorm_mod(p, x1[:], gmod_f[:], shift_f[:], h2[:])
        p.dma(h2_o[rs, :], h2[:], q="pool")
        h2T = p.rot("h2Tf", [128, 8, 128], F32, 1)
        for q4 in range(2):
            pt = p.rot("psTf", [128, 4, 128], F32, 1, psum=True)
            for i in range(4):
                kk = q4 * 4 + i
                p.tr(pt[:, i, :], h2[:, kk * 128:(kk + 1) * 128], ident[:])
            p.copy(h2T[:, q4 * 4:q4 * 4 + 4, :], pt[:])
        pr = p.rot("ps_r", [128, 36], F32, 1, psum=True)
        for k in range(8):
            p.mm(pr[:], h2T[:, k, :], wgr[:, k, :], start=(k == 0), stop=False)
        p.mm(pr[:], ones_f[0:1, :], bgr[0:1, :], start=False, stop=True)
        lg = p.rot("lg", [128, 36], F32, 2)
        p.copy(lg[:], pr[:])
        sm = p.rot("rt_sm", [128, 16], F32, 2)
        gmax, ngmax, sg, pgrp, m1, m2, e2, rden, w1, w2 = [sm[:, i:i + 1] for i in range(10)]
        p.reduce(gmax, lg[:, 0:4], op=ALU.max)
        oh = p.rot("rt_oh", [128, 4], F32, 2)
        p.ts(oh[:], lg[:, 0:4], gmax, ALU.is_equal)
        p.ts(ngmax, gmax, -1.0, ALU.mult)
        eg = p.rot("rt_eg", [128, 4], F32, 2)
        p.act(eg[:], lg[:, 0:4], AF.Exp, bias=ngmax)
        p.reduce(sg, eg[:])
        p.recip(pgrp, sg)
        el = p.rot("rt_el", [128, 8], F32, 2)
        p.ts(el[:], lg[:, 4:12], oh[:, 0:1], ALU.mult)
        for g in range(1, 4):
            p.stt(el[:], lg[:, 4 + 8 * g:12 + 8 * g], oh[:, g:g + 1], el[:], ALU.mult, ALU.add)
        p.reduce(m1, el[:], op=ALU.max)
        mk1 = p.rot("rt_mk1", [128, 8], F32, 2)
        p.ts(mk1[:], el[:], m1, ALU.is_equal)
        el2 = p.rot("rt_el2", [128, 8], F32, 2)
        p.ts(el2[:], mk1[:], -1e30, ALU.mult)
        p.tt(el2[:], el2[:], el[:], ALU.add)
        p.reduce(m2, el2[:], op=ALU.max)
        mk2 = p.rot("rt_mk2", [128, 8], F32, 2)
        p.ts(mk2[:], el2[:], m2, ALU.is_equal)
        p.tt(e2, m2, m1, ALU.subtract)
        p.act(e2, e2, AF.Exp)
        p.ts(rden, e2, 1.0, ALU.add)
        p.recip(rden, rden)
        p.tt(w1, rden, pgrp, ALU.mult)
        p.tt(w2, w1, e2, ALU.mult)
        wexp = p.rot("rt_wexp", [128, 8], F32, 2)
        p.ts(wexp[:], mk1[:], w1, ALU.mult)
        p.stt(wexp[:], mk2[:], w2, wexp[:], ALU.mult, ALU.add)
        wt = p.rot("rt_wt", [128, 32], F32, 2)
        for g in range(4):
            p.ts(wt[:, 8 * g:8 * g + 8], wexp[:], oh[:, g:g + 1], ALU.mult)
        p.dma(wt_o[rs, :], wt[:])
    p.finish([x1_o, h2_o, wt_o, gf_o])
    return nc, p


def build_k3b(ntiles=NT, nexp=32):
    nc = bass.Bass("TRN2", target_bir_lowering=False)
    p = P(nc)
    ntok = ntiles * 128
    di = lambda n, s: p.dram(n, s, F32, kind="ExternalInput")
    x1_d = di("x1", [ntok, D]); h2_d = di("h2", [ntok, D]); wt_d = di("wt", [ntok, 32]); gf_d = di("gf", [128, D])
    wg_d = di("w_gate", [nexp * D, 512]); wu_d = di("w_up", [nexp * D, 512]); wd_d = di("w_down", [nexp * 512, D])
    ident_d = di("ident_in", [128, 128])
    xo = p.dram("xo", [ntok, D], F32, kind="ExternalOutput")
    ident = p.sb([128, 128], F32, "ident")
    p.dma(ident[:], ident_d[:])
    identb = p.sb([128, 128], BF16, "identb")
    p.copy(identb[:], ident[:])
    gf = p.sb([128, D], F32, "gf")
    p.dma(gf[:], gf_d[:])
    x1 = p.sb([128, ntiles, D], F32, "x1")
    h2T = p.sb([128, ntiles, 8, 128], BF16, "h2T")
    wt = p.sb([128, ntiles, 32], F32, "wt")
    for t in range(ntiles):
        rs = slice(t * 128, (t + 1) * 128)
        p.dma(x1[:, t, :], x1_d[rs, :])
        p.dma(wt[:, t, :], wt_d[rs, :], q="pool")
        ht = p.rot("ht", [128, D], F32, 2)
        p.dma(ht[:], h2_d[rs, :], q="pool")
        hb = p.rot("hb", [128, D], BF16, 2)
        p.copy(hb[:], ht[:])
        for q4 in range(2):
            pt = p.rot("psT", [128, 4, 128], BF16, 2, psum=True)
            for i in range(4):
                kk = q4 * 4 + i
                p.tr(pt[:, i, :], hb[:, kk * 128:(kk + 1) * 128], identb[:])
            p.copy(h2T[:, t, q4 * 4:q4 * 4 + 4, :], pt[:])
    def load_expert(e):
        Wg = p.rot("Wg", [128, 8, 512], BF16, 2)
        Wu = p.rot("Wu", [128, 8, 512], BF16, 2)
        Wd = p.rot("Wd", [128, 4, D], BF16, 2)
        for k in range(8):
            p.dma(Wg[:, k, :], wg_d[e * D + k * 128:e * D + (k + 1) * 128, :], q="pool")
            p.dma(Wu[:, k, :], wu_d[e * D + k * 128:e * D + (k + 1) * 128, :], q="pool")
        return Wg, Wu, Wd

    def load_wd_piece(e, Wd, k):
        st = p.rot("wstage", [128, 1024], F32, 4)
        p.dma(st[:], wd_d[e * 512 + k * 128:e * 512 + (k + 1) * 128, :])
        p.tt(Wd[:, k, :], st[:], gf[:], ALU.mult)

    nxt = load_expert(0)
    for k in range(4):
        load_wd_piece(0, nxt[2], k)
    for e in range(nexp):
        Wg, Wu, Wd = nxt
        stb = {}

        def partA(t):
            pg = p.rot("ps_mm", [128, 512], F32, 4, psum=True)
            pu = p.rot("ps_mm", [128, 512], F32, 4, psum=True)
            for k in range(8):
                p.mm(pg[:], h2T[:, t, k, :], Wg[:, k, :], start=(k == 0), stop=(k == 7))
                p.mm(pu[:], h2T[:, t, k, :], Wu[:, k, :], start=(k == 0), stop=(k == 7))
            sg = p.rot("sg", [128, 512], F32, 2)
            p.act(sg[:], pg[:], AF.Silu)
            hid = p.rot("hid", [128, 512], BF16, 3)
            p.stt(hid[:], sg[:], wt[:, t, e:e + 1], pu[:], ALU.mult, ALU.mult)
            stb[t] = hid

        def partB(t):
            hid = stb.pop(t)
            pt = p.rot("psT", [128, 4, 128], BF16, 2, psum=True)
            for i in range(4):
                p.tr(pt[:, i, :], hid[:, i * 128:(i + 1) * 128], identb[:])
            hT = p.rot("hidT", [128, 4, 128], BF16, 3)
            p.copy(hT[:], pt[:])
            stb[("T", t)] = hT

        def partC(t):
            hT = stb.pop(("T", t))
            for nb in range(2):
                cs = slice(nb * 512, (nb + 1) * 512)
                py = p.rot("ps_py", [128, 512], F32, 2, psum=True)
                for k in range(4):
                    p.mm(py[:], hT[:, k, :], Wd[:, k, cs], start=(k == 0), stop=(k == 3))
                p.tt(x1[:, t, cs], x1[:, t, cs], py[:], ALU.add)

        for step in range(ntiles + 2):
            if step == 0 and e + 1 < nexp:
                nxt = load_expert(e + 1)
            if e + 1 < nexp and ntiles >= 8 and 2 <= step < 6:
                load_wd_piece(e + 1, nxt[2], step - 2)
            if e + 1 < nexp and ntiles < 8 and step == 0:
                for k in range(4):
                    load_wd_piece(e + 1, nxt[2], k)
            if step < ntiles:
                partA(step)
            if 0 <= step - 1 < ntiles:
                partB(step - 1)
            if 0 <= step - 2 < ntiles:
                partC(step - 2)
    for t in range(ntiles):
        p.dma(xo[t * 128:(t + 1) * 128, :], x1[:, t, :], q=("sp" if t % 2 == 0 else "pool"))
    p.finish([xo])
    return nc, p


_PROGS = {}


def _prog(name, fn):
    if name not in _PROGS:
        _PROGS[name] = fn()[0]
    return _PROGS[name]


def _run(nc, in_maps):
    in_maps = [{k: np.ascontiguousarray(v, dtype=np.float32) for k, v in m.items()} for m in in_maps]
    return run_bass_kernel_spmd(nc, in_maps, core_ids=list(range(NCORE))).results


def _padT(a):
    return np.ascontiguousarray(np.concatenate([np.zeros((a.shape[1], 3), np.float32), a.T], 1))


def _sb_masks(j):
    kk = (np.arange(8)[:, None, None] * 128 + np.arange(128)[None, :, None])
    qq = j * 512 + np.arange(512)[None, None, :]
    return np.ascontiguousarray((kk < qq).astype(np.float32).transpose(1, 0, 2))


def kernel(x, c, w_ada, b_ada, norm_mix, norm_ffn, w_in, ssm_conv_w, ssm_conv_b, ssm_dt_bias, ssm_a_log, ssm_d,
           ssm_norm, sb_q_norm, sb_k_norm, gdn_conv_w, gdn_a_log, gdn_dt_bias, gdn_norm, w_branch, w_out,
           w_group, b_group, w_router, b_router, w_gate, w_up, w_down):
    f32 = lambda a: np.asarray(a, dtype=np.float32)
    x = f32(x)[0]
    c_col = np.ascontiguousarray(f32(c)[0].reshape(8, 128).T)
    I = np.eye(128, dtype=np.float32)
    i128 = np.arange(128)
    i64 = np.arange(64)
    triu128 = (i128[:, None] <= i128[None, :]).astype(np.float32)
    mneg128 = np.where(i128[:, None] <= i128[None, :], 0.0, -30000.0).astype(np.float32)
    mincl = (i128[:, None] >= i128[None, :]).astype(np.float32)
    triu64 = (i64[:, None] <= i64[None, :]).astype(np.float32)
    mpos64 = np.where(i64[:, None] > i64[None, :], 0.0, 30000.0).astype(np.float32)
    mneg64 = np.where(i64[:, None] <= i64[None, :], 0.0, -30000.0).astype(np.float32)
    st01 = (i64[:, None] < i64[None, :]).astype(np.float32)
    sbm = [_sb_masks(0), _sb_masks(1)]
    k1 = _prog("k1", build_k1)
    kssd = _prog("ssd", build_ssd)
    ksb = _prog("sb", build_sb)
    kgdn = _prog("gdn", build_gdn)
    k3a = _prog("k3a", build_k3a)
    k3b = _prog("k3b", build_k3b)
    for l in range(4):
        wa = f32(w_ada[l]); ba = f32(b_ada[l])
        r = _run(k1, [{"x": x[TOK * i:TOK * (i + 1)], "c_col": c_col, "wada": wa[:, 0:2 * D], "bada": ba[None, 0:2 * D],
                       "norm_g": f32(norm_mix[l])[None], "w_in": f32(w_in[l]), "ident_in": I} for i in range(NCORE)])
        proj = np.concatenate([r[i]["proj"] for i in range(NCORE)], 0)
        m_z = proj[:, 0:512]; m_xbc = proj[:, 512:1536]; m_dt = proj[:, 1536:1544]
        sbq = proj[:, 1544:3080]; gq = proj[:, 3080:4616]; g_a = proj[:, 4616:4620]; g_b = proj[:, 4620:4624]
        g_gate = proj[:, 4624:5136]; br_gate = proj[:, 5136:8208]
        cw = f32(ssm_conv_w[l]); cb = f32(ssm_conv_b[l])
        ims = []
        for i in range(NCORE):
            g = i // 4
            xs = slice(64 * i, 64 * i + 64); bs = slice(512 + 128 * g, 640 + 128 * g); cs = slice(768 + 128 * g, 896 + 128 * g)
            ims.append({"xT": _padT(m_xbc[:, xs]), "BT": _padT(m_xbc[:, bs]), "CT": _padT(m_xbc[:, cs]),
                        "wx": cw[:, xs].T, "bx": cb[xs, None], "wB": cw[:, bs].T, "bB": cb[bs, None], "wC": cw[:, cs].T, "bC": cb[cs, None],
                        "dt_col": m_dt[:, i].reshape(-1, 128).T,
                        "scal": np.tile(np.array([[f32(ssm_dt_bias[l])[i], f32(ssm_a_log[l])[i], f32(ssm_d[l])[i]]], np.float32), (128, 1)),
                        "triu": triu128, "mneg": mneg128, "ident_in": I})
        r = _run(kssd, ims)
        y_ssd = np.concatenate([r[i]["y"] for i in range(NCORE)], 1)
        ims = []
        for i in range(NCORE):
            h, j = i // 2, i % 2
            q = sbq[:, 128 * h:128 * h + 128]
            qsel = np.concatenate([q[(2 * m + j) * 512:(2 * m + j + 1) * 512] for m in range(16)], 0)
            ims.append({"q": qsel, "k": sbq[:, 512 + 128 * h:640 + 128 * h], "v": sbq[:, 1024 + 128 * h:1152 + 128 * h],
                        "qg": f32(sb_q_norm[l])[None], "kg": f32(sb_k_norm[l])[None], "mask": sbm[j], "mincl": mincl, "ident_in": I})
        r = _run(ksb, ims)
        y_sb = np.zeros((SEQ, 512), np.float32)
        for i in range(NCORE):
            h, j = i // 2, i % 2
            oT = r[i]["oT"]
            for m in range(16):
                g = 2 * m + j
                y_sb[g * 512:(g + 1) * 512, 128 * h:128 * h + 128] = oT[:, m * 512:(m + 1) * 512].T
        gw = f32(gdn_conv_w[l])
        ims = []
        for i in range(NCORE):
            h, e = i // 2, i % 2
            qs = slice(128 * h, 128 * h + 128); ks = slice(512 + 128 * h, 640 + 128 * h); vs = slice(1024 + 128 * h + 64 * e, 1024 + 128 * h + 64 * e + 64)
            ims.append({"qT": _padT(gq[:, qs]), "kT": _padT(gq[:, ks]), "vT": _padT(gq[:, vs]),
                        "wq": gw[:, qs].T, "wk": gw[:, ks].T, "wv": gw[:, vs].T,
                        "a_col": g_a[:, h].reshape(-1, 64).T, "b_col": g_b[:, h].reshape(-1, 64).T,
                        "scal": np.tile(np.array([[f32(gdn_a_log[l])[h], f32(gdn_dt_bias[l])[h]]], np.float32), (128, 1)),
                        "triu": triu64, "mpos": mpos64, "mneg": mneg64, "st01": st01, "ident_in": I})
        r = _run(kgdn, ims)
        o_gdn = np.zeros((SEQ, 512), np.float32)
        for i in range(NCORE):
            h, e = i // 2, i % 2
            o_gdn[:, 128 * h + 64 * e:128 * h + 64 * e + 64] = r[i]["o"]
        wgr = np.concatenate([f32(w_group[l]), f32(w_router[l])], 1)
        bgr = np.concatenate([f32(b_group[l]), f32(b_router[l])])[None]
        ims = []
        for i in range(NCORE):
            ts_ = slice(TOK * i, TOK * (i + 1))
            ims.append({"x": x[ts_], "z": m_z[ts_], "ggate": g_gate[ts_], "brg": br_gate[ts_], "ya": y_ssd[ts_], "yb": y_sb[ts_], "yc": o_gdn[ts_],
                        "c_col": c_col, "wada": wa[:, 2 * D:6 * D], "bada": ba[None, 2 * D:6 * D],
                        "ssm_g": f32(ssm_norm[l])[None], "gdn_g": f32(gdn_norm[l])[None], "ffn_g": f32(norm_ffn[l])[None],
                        "w_branch": f32(w_branch[l]).reshape(1536, D), "w_out": f32(w_out[l]), "wgr": wgr, "bgr": bgr, "ident_in": I})
        ra = _run(k3a, ims)
        wg = f32(w_gate[l]).reshape(-1, 512); wu = f32(w_up[l]).reshape(-1, 512); wd = f32(w_down[l]).reshape(-1, D)
        ims = [{"x1": ra[i]["x1"], "h2": ra[i]["h2"], "wt": ra[i]["wt"], "gf": ra[i]["gf"], "w_gate": wg, "w_up": wu, "w_down": wd, "ident_in": I}
               for i in range(NCORE)]
        rb = _run(k3b, ims)
        x = np.concatenate([rb[i]["xo"] for i in range(NCORE)], 0)
    return x[None].astype(np.float32)
```
